# Optimizing a Trainium2 kernel written in Bass

```python
import math
import jax, jax.numpy as jnp
from jax import lax
import numpy as np

D_MODEL = 1024
BATCH = 4
SEQ = 8192
DEPTH = 1

PLE_DIM = 256
EPS = 1e-6

LRU_WIDTH = 1024
LRU_BLOCKS = 8
LRU_BLOCK = LRU_WIDTH // LRU_BLOCKS
LRU_CONV = 4
LRU_C = 8.0

N_HEADS = 8
HEAD_DIM = 128
N_KV_HEADS = 2
GROUP = N_HEADS // N_KV_HEADS
ATTN_WIDTH = N_HEADS * HEAD_DIM
KV_WIDTH = N_KV_HEADS * HEAD_DIM
IDX_HEADS = 8
IDX_DIM = 64
TOPK_MAX = 256
Q_BLOCK = 128

N_BUCKETS = 32
MAX_DISTANCE = 128

D_FF = 3072
FFN_CONV = 3

IN_SPLITS = (LRU_WIDTH, LRU_WIDTH, ATTN_WIDTH, KV_WIDTH, KV_WIDTH,
             IDX_HEADS * IDX_DIM, IDX_DIM, IDX_HEADS, D_MODEL, D_MODEL)
IN_WIDTH = sum(IN_SPLITS)

kernel_name = 'hybrid_rglru_dsa_convglu_block'


def _rmsnorm(x, g):
    x32 = x.astype(jnp.float32)
    y = x32 * lax.rsqrt(jnp.mean(x32 * x32, axis=-1, keepdims=True) + EPS)
    return (y * g.astype(jnp.float32)).astype(x.dtype)


def _split_cols(h):
    parts = []
    start = 0
    for n in IN_SPLITS:
        parts.append(h[..., start:start + n])
        start += n
    return parts


def _causal_dwconv(x, w, b):
    width = w.shape[0]
    s = x.shape[1]
    xp = jnp.pad(x, ((0, 0), (width - 1, 0), (0, 0)))
    y = b
    for j in range(width):
        y = y + xp[:, j:j + s] * w[j]
    return y


def _rg_lru(x, wa, ba, wx, bx, lam):
    b, s, _ = x.shape
    xb = x.reshape(b, s, LRU_BLOCKS, LRU_BLOCK)
    r = jax.nn.sigmoid(jnp.einsum('bsnc,ncd->bsnd', xb, wa).reshape(b, s, LRU_WIDTH) + ba)
    i = jax.nn.sigmoid(jnp.einsum('bsnc,ncd->bsnd', xb, wx).reshape(b, s, LRU_WIDTH) + bx)
    log_a = -LRU_C * r.astype(jnp.float32) * jax.nn.softplus(-lam.astype(jnp.float32))
    a = jnp.exp(log_a)
    mult = jnp.sqrt(-jnp.expm1(2.0 * log_a))
    u = mult * (i * x).astype(jnp.float32)

    def combine(left, right):
        a1, b1 = left
        a2, b2 = right
        return a1 * a2, a2 * b1 + b2

    _, h = lax.associative_scan(combine, (a, u), axis=1)
    return h.astype(x.dtype)


def _t5_bucket(dist):
    max_exact = N_BUCKETS // 2
    d_f = jnp.maximum(dist, 1).astype(jnp.float32)
    large = max_exact + (jnp.log(d_f / max_exact) / math.log(MAX_DISTANCE / max_exact)
                         * (N_BUCKETS - max_exact)).astype(jnp.int32)
    large = jnp.minimum(large, N_BUCKETS - 1)
    return jnp.where(dist < max_exact, dist, large)


def _dsa_attention(q, k, v, qi, ki, wi, rel_bias):
    b, s = q.shape[0], q.shape[1]
    n_blk = s // Q_BLOCK
    top_k = min(TOPK_MAX, s // 4)
    key_pos = jnp.arange(s, dtype=jnp.int32)
    q_pos = key_pos.reshape(n_blk, Q_BLOCK)
    ki32 = ki.astype(jnp.float32)

    def blockify(a):
        return jnp.moveaxis(a.reshape((b, n_blk, Q_BLOCK) + a.shape[2:]), 1, 0)

    def one_block(args):
        qb, qib, wib, tpos = args
        dots = jnp.einsum('bthd,bsd->bths', qib.astype(jnp.float32), ki32) * (IDX_DIM ** -0.5)
        score = jnp.einsum('bth,bths->bts', wib.astype(jnp.float32), jax.nn.relu(dots))
        causal = key_pos[None, :] <= tpos[:, None]
        score = jnp.where(causal[None], score, -jnp.inf)
        _, sel = lax.top_k(score, top_k)
        k_sel = jax.vmap(lambda kb, ib: kb[ib])(k, sel)
        v_sel = jax.vmap(lambda vb, ib: vb[ib])(v, sel)
        qg = qb.reshape(b, Q_BLOCK, N_KV_HEADS, GROUP, HEAD_DIM)
        logits = jnp.einsum('btngd,btknd->btngk', qg, k_sel).astype(jnp.float32) * (HEAD_DIM ** -0.5)
        dist = tpos[None, :, None] - sel
        bias = rel_bias[_t5_bucket(jnp.maximum(dist, 0))]
        bias = bias.reshape(b, Q_BLOCK, top_k, N_KV_HEADS, GROUP).transpose(0, 1, 3, 4, 2)
        logits = logits + bias.astype(jnp.float32)
        logits = jnp.where((dist >= 0)[:, :, None, None, :], logits, -jnp.inf)
        probs = jax.nn.softmax(logits, axis=-1).astype(v.dtype)
        o = jnp.einsum('btngk,btknd->btngd', probs, v_sel)
        return o.reshape(b, Q_BLOCK, N_HEADS, HEAD_DIM)

    out = lax.map(one_block, (blockify(q), blockify(qi), blockify(wi), q_pos))
    return jnp.moveaxis(out, 0, 1).reshape(b, s, ATTN_WIDTH)


def setup_inputs(seed: int = 0) -> dict:
    key = jax.random.key(seed)
    ks = jax.random.split(key, 32)
    f32 = jnp.float32

    def nrm(k, shape, fan_in):
        return jax.random.normal(k, shape, f32) * (fan_in ** -0.5)

    def gain(k, shape):
        return 1.0 + 0.05 * jax.random.normal(k, shape, f32)

    def small(k, shape):
        return 0.01 * jax.random.normal(k, shape, f32)

    u = jax.random.uniform(ks[10], (DEPTH, LRU_WIDTH), f32, 0.9, 0.999)
    sa = u ** (1.0 / LRU_C)
    lru_lambda = jnp.log(sa) - jnp.log1p(-sa)

    return {
        'x': jax.random.normal(ks[0], (BATCH, SEQ, D_MODEL), f32),
        'p': jax.random.normal(ks[1], (DEPTH, BATCH, SEQ, PLE_DIM), f32),
        'mix_norm': gain(ks[2], (DEPTH, D_MODEL)),
        'w_in': nrm(ks[3], (DEPTH, D_MODEL, IN_WIDTH), D_MODEL),
        'lru_conv_w': nrm(ks[4], (DEPTH, LRU_CONV, LRU_WIDTH), LRU_CONV),
        'lru_conv_b': small(ks[5], (DEPTH, LRU_WIDTH)),
        'lru_wa': nrm(ks[6], (DEPTH, LRU_BLOCKS, LRU_BLOCK, LRU_BLOCK), LRU_BLOCK),
        'lru_ba': small(ks[7], (DEPTH, LRU_WIDTH)),
        'lru_wx': nrm(ks[8], (DEPTH, LRU_BLOCKS, LRU_BLOCK, LRU_BLOCK), LRU_BLOCK),
        'lru_bx': small(ks[9], (DEPTH, LRU_WIDTH)),
        'lru_lambda': lru_lambda,
        'q_norm': gain(ks[11], (DEPTH, HEAD_DIM)),
        'k_norm': gain(ks[12], (DEPTH, HEAD_DIM)),
        'rel_bias': 0.5 * jax.random.normal(ks[13], (N_BUCKETS, N_HEADS), f32),
        'w_lru_br': nrm(ks[14], (DEPTH, LRU_WIDTH, D_MODEL), LRU_WIDTH),
        'w_attn_br': nrm(ks[15], (DEPTH, ATTN_WIDTH, D_MODEL), ATTN_WIDTH),
        'w_out': nrm(ks[16], (DEPTH, D_MODEL, D_MODEL), D_MODEL),
        'ffn_norm': gain(ks[17], (DEPTH, D_MODEL)),
        'w_up': nrm(ks[18], (DEPTH, D_MODEL, 2 * D_FF), D_MODEL),
        'ffn_conv_w': nrm(ks[19], (DEPTH, FFN_CONV, D_FF), FFN_CONV),
        'ffn_conv_b': small(ks[20], (DEPTH, D_FF)),
        'w_down': nrm(ks[21], (DEPTH, D_FF, D_MODEL), D_FF),
        'ple_norm': gain(ks[22], (DEPTH, D_MODEL)),
        'w_ple_gate': nrm(ks[23], (DEPTH, D_MODEL, D_MODEL), D_MODEL),
        'w_ple_proj': nrm(ks[24], (DEPTH, PLE_DIM, D_MODEL), PLE_DIM),
    }


def reference(x, p, mix_norm, w_in, lru_conv_w, lru_conv_b, lru_wa, lru_ba, lru_wx, lru_bx,
              lru_lambda, q_norm, k_norm, rel_bias, w_lru_br, w_attn_br, w_out, ffn_norm,
              w_up, ffn_conv_w, ffn_conv_b, w_down, ple_norm, w_ple_gate, w_ple_proj):
    b, s, _ = x.shape
    for l in range(DEPTH):
        xn = _rmsnorm(x, mix_norm[l])
        h = xn @ w_in[l]
        lru_x, lru_gate, q, k, v, qi, ki, wi, gate_a, gate_b = _split_cols(h)

        ua = _causal_dwconv(lru_x, lru_conv_w[l], lru_conv_b[l])
        ya = _rg_lru(ua, lru_wa[l], lru_ba[l], lru_wx[l], lru_bx[l], lru_lambda[l])
        ya = (ya * jax.nn.gelu(lru_gate)) @ w_lru_br[l]

        qh = _rmsnorm(q.reshape(b, s, N_HEADS, HEAD_DIM), q_norm[l])
        kh = _rmsnorm(k.reshape(b, s, N_KV_HEADS, HEAD_DIM), k_norm[l])
        vh = v.reshape(b, s, N_KV_HEADS, HEAD_DIM)
        qih = qi.reshape(b, s, IDX_HEADS, IDX_DIM)
        wih = wi * (IDX_HEADS ** -0.5)
        yb = _dsa_attention(qh, kh, vh, qih, ki, wih, rel_bias) @ w_attn_br[l]

        merged = jax.nn.sigmoid(gate_a) * ya + jax.nn.sigmoid(gate_b) * yb
        x = x + merged @ w_out[l]

        xn = _rmsnorm(x, ffn_norm[l])
        up = xn @ w_up[l]
        g_ffn = _causal_dwconv(up[..., :D_FF], ffn_conv_w[l], ffn_conv_b[l])
        x = x + (jax.nn.gelu(g_ffn) * up[..., D_FF:]) @ w_down[l]

        g_ple = jax.nn.sigmoid(_rmsnorm(x, ple_norm[l]) @ w_ple_gate[l])
        x = x + g_ple * (p[l] @ w_ple_proj[l])
    return x
```

```python
import math
from contextlib import ExitStack

import numpy as np
import concourse.bass as bass
import concourse.mybir as mybir
from concourse.bass_utils import run_bass_kernel_spmd

F32 = mybir.dt.float32
BF16 = mybir.dt.bfloat16
AF = mybir.ActivationFunctionType
ALU = mybir.AluOpType
AX = mybir.AxisListType

D = 1024
TB = 2
T = TB * 128
NSLOT = 4
EPS = 1e-6

C_MIXG, C_FFNG, C_PLEG = 0, 8, 16
C_LCW, C_LCB, C_LBA, C_LBX, C_LAM = 24, 56, 64, 72, 80
C_GQ, C_GK = 88, 89
C_FCW, C_FCB = 90, 162
C_CF = 186
C_EPS, C_ONE = 188, 189
C_POW = 190


class Sched:
    def __init__(self, nc, ndma=24):
        self.nc = nc
        self.eng = {"pe": nc.tensor, "act": nc.scalar, "dve": nc.vector, "pool": nc.gpsimd, "sp": nc.sync}
        self.sems = {}
        self.cnt = {}
        self.waited = {e: {} for e in self.eng}
        self.bufs = {}
        self.ndma = ndma
        self.dma_i = 0
        self.pool_i = 0
        self.nops = 0
        self.phase = None

    def setup(self, stack):
        for e in self.eng:
            self.sems[e] = stack.enter_context(self.nc.semaphore("s_" + e))
            self.cnt[e] = 0
        self.dsems = [stack.enter_context(self.nc.semaphore("d%d" % i)) for i in range(self.ndma)]
        self.dcnt = [0] * self.ndma
        self.semobj = dict(self.sems)
        for i, s in enumerate(self.dsems):
            self.semobj["d%d" % i] = s

    def _deps(self, reads, writes):
        deps = {}

        def add(k, v):
            if deps.get(k, 0) < v:
                deps[k] = v
        for b in reads:
            st = self.bufs.get(b)
            if st and st["w"]:
                add(*st["w"])
        for b in writes:
            st = self.bufs.get(b)
            if st:
                if st["w"]:
                    add(*st["w"])
                for k, v in st["r"].items():
                    add(k, v)
        return deps

    def _wait(self, e, deps, keep_one=False):
        eng = self.eng[e]
        need = [(k, v) for k, v in deps.items() if self.waited[e].get(k, 0) < v]
        inline = None
        if keep_one and need:
            inline = need.pop()
        for k, v in need:
            eng.wait_ge(self.semobj[k], v)
            self.waited[e][k] = v
        if inline is not None:
            self.waited[e][inline[0]] = inline[1]
            return (self.semobj[inline[0]], inline[1])
        return None

    def _update(self, ev, reads, writes):
        for b in reads:
            st = self.bufs.setdefault(b, {"w": None, "r": {}})
            if st["r"].get(ev[0], 0) < ev[1]:
                st["r"][ev[0]] = ev[1]
        for b in writes:
            self.bufs[b] = {"w": ev, "r": {}}

    def op(self, e, fn, reads=(), writes=()):
        deps = self._deps(reads, writes)
        if e == "pe":
            deps.pop("pe", None)
        inl = self._wait(e, deps, keep_one=True)
        ins = fn(self.eng[e])
        if inl is not None:
            ins._wait_ge(inl[0], inl[1])
        if self.phase is not None:
            ins.annotate(self.phase)
        self.cnt[e] += 1
        ins.then_inc(self.sems[e], 1)
        self._update((e, self.cnt[e]), reads, writes)
        self.nops += 1
        return ins

    def dma(self, e, out, in_, reads=(), writes=()):
        if e == "pool":
            i = self.pool_i % 3
            self.pool_i += 1
        else:
            i = 3 + self.dma_i % (self.ndma - 3)
            self.dma_i += 1
        key = "d%d" % i
        deps = self._deps(reads, writes)
        if self.dcnt[i] > 0:
            deps[key] = max(deps.get(key, 0), self.dcnt[i])
        self._wait(e, deps)
        ins = self.eng[e].dma_start(out=out, in_=in_)
        self.dcnt[i] += 16
        ins.then_inc(self.dsems[i], 16)
        self._update((key, self.dcnt[i]), reads, writes)
        self.nops += 1
        return ins

    def final_wait(self, e, bufs):
        self._wait(e, self._deps(bufs, bufs))


def build(NT, NIT=20, TOPK=256):
    NCT = NT // 2
    NTOK = NT * 128
    NBLK = NT // TB
    FIRST_OWN = NCT // TB
    FIRST_FULL = FIRST_OWN - 1
    CTXK = NCT * 128
    NOWN = (NT - NCT) * 128

    nc = bass.Bass("TRN2", target_bir_lowering=False)

    def din(name, shape, dt=F32):
        return nc.dram_tensor(name, shape, dt, kind="ExternalInput").ap()

    def dscr(name, shape, dt=BF16):
        return nc.dram_tensor(name, shape, dt, kind="Internal").ap()

    xbuf = din("xbuf", [NTOK, D])
    pbuf = din("pbuf", [NTOK, 256])
    w_in = din("w_in", [D, 6216])
    w_lbr = din("w_lru_br", [D, D])
    w_abr = din("w_attn_br", [D, D])
    w_out = din("w_out", [D, D])
    w_up = din("w_up", [D, 6144])
    w_down = din("w_down", [3072, D])
    w_pg = din("w_ple_gate", [D, D])
    w_pp = din("w_ple_proj", [256, D])
    lru_wa = din("lru_wa", [8, 128, 128])
    lru_wx = din("lru_wx", [8, 128, 128])
    cvec_d = din("cvec", [128, C_POW + NIT])
    tri_d = din("tri", [128, 128])
    ident_d = din("ident", [128, 128])
    bn_d = din("bias_near", [128, 2 * 8 * 128])
    b31_d = din("bias_far", [128, 2 * 8 * 128])
    out = nc.dram_tensor("out", [NOWN, D], F32, kind="ExternalOutput").ap()

    S = Sched(nc)
    with ExitStack() as st:
        S.setup(st)

        def sb(name, shape, dt=F32):
            return st.enter_context(nc.sbuf_tensor(name, shape, dt))

        kT = sb("kT", [128, 2, NTOK], BF16)
        Vt = sb("Vt", [128, NT, 2, 129], BF16)
        kiT = sb("kiT", [128, NTOK], BF16)
        wsl = sb("wsl", [128, NSLOT, 4096], BF16)
        lhalo = sb("lhalo", [128, 8, 3])
        cvec = sb("cvec_s", [128, C_POW + NIT])
        tri = sb("tri_s", [128, 128])
        ident_f = sb("ident_f", [128, 128])
        ident = sb("ident_b", [128, 128], BF16)
        ident4 = sb("ident4", [128, 4, 128], BF16)
        ones_b = sb("ones_b", [128, 128], BF16)
        biasn = sb("biasn", [128, 2, 8, 128], BF16)
        s8h = sb("s8h", [128, 8])
        s16h = sb("s16h", [128, 8])
        hba = sb("hba", [128, 8])
        hbx = sb("hbx", [128, 8])
        gqs = sb("gqs", [128, 1])
        hstate = sb("hstate", [128, 8])
        fhalo = sb("fhalo", [128, 24, 2])
        xres = sb("xres", [128, TB, D])
        xnT = sb("xnT", [128, 8, T], BF16)
        xn_tm = sb("xn_tm", [128, D], BF16)
        qT = sb("qT", [128, 8, T], BF16)
        qiT = sb("qiT", [128, 4, T], BF16)
        weff = sb("weff", [128, TB, 8])
        hgT = sb("hgT", [128, 8, T], BF16)
        attnT = sb("attnT", [128, 8, T], BF16)
        attn_tm = xn_tm
        mergedT = qT
        actT = hgT
        pT = sb("pT", [128, 2, T], BF16)
        p_tm = sb("p_tm", [128, 256], BF16)
        sm = sb("sm", [128, 64])
        hw = sb("hw", [128, NIT])
        cntb = sb("cntb", [128, 8])
        wv = sb("wv", [128, 8])
        den = sb("den", [128, 8])
        rden = sb("rden", [128, 8])
        uab = sb("uab", [128, 2, T], BF16)
        qsq = sb("qsq", [128, T], BF16)
        tmpf = sb("tmpf", [128, 2, 512])
        gbuf = sb("gbuf", [128, 2, T + 2])
        gcb = sb("gcb", [128, 2, T])
        rl = tmpf
        nm = sb("nm", [128, 2, 512], BF16)
        pTb = sb("pTb", [128, 2, 512], BF16)
        waxs = sb("waxs", [128, 8, 256], BF16)
        junk2 = sb("junk2", [128, 4096], BF16)
        ARN = max(NTOK, 32 * T + 24)
        score = sb("score", [128, ARN])
        LOFF = 24 * T
        lrux = score[:, LOFF:LOFF + 8 * (T + 3)].rearrange("p (c t) -> p c t", c=8)
        LXK = [("sc", c) for c in range(LOFF // 512, (LOFF + 8 * (T + 3) - 1) // 512 + 1)]
        pA = st.enter_context(nc.psum_tensor("pA", [128, 512], F32))
        pB = st.enter_context(nc.psum_tensor("pB", [128, 512], F32))
        pL0 = st.enter_context(nc.psum_tensor("pL0", [128, 512], F32))
        pL1 = st.enter_context(nc.psum_tensor("pL1", [128, 512], F32))
        pOA = st.enter_context(nc.psum_tensor("pOA", [128, 512], F32))
        pOB = st.enter_context(nc.psum_tensor("pOB", [128, 512], F32))
        pOC = st.enter_context(nc.psum_tensor("pOC", [128, 512], F32))
        pT_ps = st.enter_context(nc.psum_tensor("pTp", [128, 1024], BF16))
        st.enter_context(nc.Block())

        banks = {"A": pA, "B": pB, "OA": pOA, "OB": pOB, "OC": pOC, "L0": pL0, "L1": pL1}
        rot = {"n": 0, "i": 0}
        DENSE_ROT = ["A", "B"]

        ROT4 = ["A", "B", "L0", "L1"]

        def nextbank(rot4=False):
            r = ROT4 if rot4 else DENSE_ROT
            k = r[rot["n"] % len(r)]
            rot["n"] += 1
            return banks[k], ("ps", k)

        def nextbank_idx():
            k = "AB"[rot["i"] % 2]
            rot["i"] += 1
            return banks[k], ("ps", k)

        def cv(c0, n=1):
            return cvec[:, c0:c0 + n]

        def sck(a, b):
            return [("sc", c) for c in range(a // 512, (b - 1) // 512 + 1)]

        def arena(a, n):
            return score[:, a:a + n], sck(a, a + n)

        def act(out, in_, func, reads, writes, bias=None, scale=None, accum=None, e="act"):
            kw = {}
            if bias is not None:
                kw["bias"] = bias
            if scale is not None:
                kw["scale"] = scale
            if accum is not None:
                kw["accum_out"] = accum
            return S.op(e, lambda en: en.activation(out=out, in_=in_, func=func, **kw), reads, writes)

        def ts(out, in0, s1, s2, op0, op1, reads, writes, accum=None, e="dve"):
            kw = {}
            if accum is not None:
                kw["accum_out"] = accum
            if op1 is None:
                return S.op(e, lambda en: en.tensor_scalar(out=out, in0=in0, scalar1=s1, scalar2=s2, op0=op0, **kw), reads, writes)
            return S.op(e, lambda en: en.tensor_scalar(out=out, in0=in0, scalar1=s1, scalar2=s2, op0=op0, op1=op1, **kw), reads, writes)

        def stt(out, in0, scalar, in1, op0, op1, reads, writes, e="dve"):
            return S.op(e, lambda en: en.scalar_tensor_tensor(out=out, in0=in0, scalar=scalar, in1=in1, op0=op0, op1=op1), reads, writes)

        def tt(out, in0, in1, op, reads, writes, e="dve"):
            return S.op(e, lambda en: en.tensor_tensor(out=out, in0=in0, in1=in1, op=op), reads, writes)

        def cp(out, in_, reads, writes, e="act"):
            if e == "act":
                return S.op(e, lambda en: en.activation(out=out, in_=in_, func=AF.Copy), reads, writes)
            return S.op(e, lambda en: en.tensor_copy(out=out, in_=in_), reads, writes)

        def mm(out, lhsT, rhs, start, stop, reads, writes, skip=False):
            if skip:
                return S.op("pe", lambda en: en.matmul(out, lhsT=lhsT, rhs=rhs, start=start, stop=stop, skip_group_check=True), reads, writes)
            return S.op("pe", lambda en: en.matmul(out, lhsT=lhsT, rhs=rhs, start=start, stop=stop), reads, writes)

        def tr(out, in_, reads, writes):
            return S.op("pe", lambda en: en.transpose(out, in_, ident[:]), reads + ["ident"], writes)

        S.dma("sp", cvec[:], cvec_d, writes=["cvec"])
        S.dma("sp", tri[:], tri_d, writes=["tri"])
        S.dma("sp", ident_f[:], ident_d, writes=["identf"])
        bn_t, bn_k = arena(0, 2048)
        b31_t, b31_k = arena(2048, 2048)
        S.dma("sp", bn_t, bn_d, writes=bn_k)
        S.dma("sp", b31_t, b31_d, writes=b31_k)

        units = {}
        unit_order = []

        def unit(name, kc, ncols, parts):
            scr = nc.dram_tensor("u_" + name, [128, kc * ncols], BF16, kind="Internal").ap()
            units[name] = (scr, kc, ncols)
            unit_order.append(name)
            d3 = scr.rearrange("p (k n) -> p k n", k=kc)
            for (dc, src3) in parts:
                w = src3.shape[2]
                S.dma("pool", d3[:, :, dc:dc + w], src3, writes=[("u", name)])

        def kp(ap, r0, nr, c0, ncol):
            return ap[r0:r0 + nr, c0:c0 + ncol].rearrange("(k p) n -> p k n", p=128)

        unit("lrux0", 8, 512, [(0, kp(w_in, 0, D, 0, 512))])
        unit("lrux1", 8, 512, [(0, kp(w_in, 0, D, 512, 512))])
        unit("wax", 8, 256, [(0, lru_wa.rearrange("n c d -> c n d")), (128, lru_wx.rearrange("n c d -> c n d"))])
        unit("kk", 8, 384, [(0, kp(w_in, 0, D, 3072, 256)), (256, kp(w_in, 0, D, 4096, 64)), (320, kp(w_in, 0, D, 4096, 64))])
        unit("vw", 8, 264, [(0, kp(w_in, 0, D, 3328, 256)), (256, kp(w_in, 0, D, 4160, 8))])
        unit("q0", 8, 512, [(0, kp(w_in, 0, D, 2048, 512))])
        unit("q1", 8, 512, [(0, kp(w_in, 0, D, 2560, 512))])
        unit("qi", 8, 512, [(0, kp(w_in, 0, D, 3584, 512))])
        unit("lg0", 8, 512, [(0, kp(w_in, 0, D, 1024, 512))])
        unit("lg1", 8, 512, [(0, kp(w_in, 0, D, 1536, 512))])
        for hh in range(2):
            unit("lbr%d" % hh, 8, 512, [(0, kp(w_lbr, 0, D, hh * 512, 512))])
        for hh in range(2):
            unit("ga%d" % hh, 8, 512, [(0, kp(w_in, 0, D, 4168 + hh * 512, 512))])
        for hh in range(2):
            unit("abr%d" % hh, 8, 512, [(0, kp(w_abr, 0, D, hh * 512, 512))])
        for hh in range(2):
            unit("gb%d" % hh, 8, 512, [(0, kp(w_in, 0, D, 5192 + hh * 512, 512))])
        for hh in range(2):
            unit("wo%d" % hh, 8, 512, [(0, kp(w_out, 0, D, hh * 512, 512))])
        for fg in range(3):
            for hh, ab in enumerate("ab"):
                unit("upg%d%s" % (fg, ab), 8, 512, [(0, kp(w_up, 0, D, fg * 1024 + hh * 512, 512))])
            for hh, ab in enumerate("ab"):
                unit("upv%d%s" % (fg, ab), 8, 512, [(0, kp(w_up, 0, D, 3072 + fg * 1024 + hh * 512, 512))])
            for hh, ab in enumerate("ab"):
                unit("dn%d%s" % (fg, ab), 8, 512, [(0, kp(w_down, fg * 1024, 1024, hh * 512, 512))])
        unit("pp", 2, 1024, [(0, kp(w_pp, 0, 256, 0, 1024))])
        for hh in range(2):
            unit("pg%d" % hh, 8, 512, [(0, kp(w_pg, 0, D, hh * 512, 512))])

        cp(ident[:], ident_f[:], ["identf"], ["ident"], e="dve")
        for a4 in range(4):
            cp(ident4[:, a4, :], ident_f[:], ["identf"], ["ident4"], e="dve")
        S.op("dve", lambda en: en.memset(ones_b[:], 1.0), [], ["ones"])
        S.op("dve", lambda en: en.memset(Vt[:].rearrange("p n k d -> p (n k) d")[:, :, 128:129], 1.0), [], ["Vones"])
        S.op("dve", lambda en: en.memset(lhalo[:], 0.0), [], ["lhalo"])
        S.op("dve", lambda en: en.memset(hstate[:], 0.0), [], ["hstate"])
        S.op("dve", lambda en: en.memset(fhalo[:], 0.0), [], ["fhalo"])
        tt(biasn[:].rearrange("p a h t -> p (a h t)"), bn_t, b31_t, ALU.subtract, bn_k + b31_k, ["biasn"])
        act(sm[:, 0:8], cv(C_LAM, 8), AF.Exp, ["cvec"], ["sm0"], scale=-1.0)
        act(sm[:, 8:16], sm[:, 0:8], AF.Ln, ["sm0"], ["sm1"], bias=cv(C_ONE))
        ts(s8h[:], sm[:, 8:16], -4.0, None, ALU.mult, None, ["sm1"], ["s8"])
        ts(s16h[:], sm[:, 8:16], -8.0, None, ALU.mult, None, ["sm1"], ["s16"])
        ts(hba[:], cv(C_LBA, 8), 0.5, None, ALU.mult, None, ["cvec"], ["hb"])
        ts(hbx[:], cv(C_LBX, 8), 0.5, None, ALU.mult, None, ["cvec"], ["hb"])
        ts(gqs[:], cv(C_GQ), float(128 ** -0.5), None, ALU.mult, None, ["cvec"], ["gqs"])
        S.op("dve", lambda en: en.memset(wv[:], 0.0), [], ["wv"])
        S.op("dve", lambda en: en.memset(wv[:, 1:3], 1.0), ["wv"], ["wv"])
        ts(wv[:, 0:1], cv(C_CF), 1.0, None, ALU.mult, None, ["cvec", "wv"], ["wv"])
        ts(wv[:, 3:4], cv(C_CF), -0.5, None, ALU.mult, None, ["cvec", "wv"], ["wv"])
        ts(wv[:, 4:5], cv(C_CF), -0.5, None, ALU.mult, None, ["cvec", "wv"], ["wv"])
        ts(wv[:, 5:6], cv(C_CF), -0.5, None, ALU.mult, None, ["cvec", "wv"], ["wv"])
        ts(wv[:, 6:7], cv(C_CF), -0.5, None, ALU.mult, None, ["cvec", "wv"], ["wv"])
        ts(wv[:, 7:8], cv(C_CF), -0.5, None, ALU.mult, None, ["cvec", "wv"], ["wv"])

        def plan_block(full):
            pl = ["lrux0", "lrux1", "kk", "vw"]
            if not full:
                return pl
            pl += ["q0", "q1", "qi", "lg0", "lg1", "lbr0", "lbr1", "ga0", "ga1", "abr0", "abr1", "gb0", "gb1", "wo0", "wo1"]
            for fg in range(3):
                pl += ["upg%da" % fg, "upg%db" % fg, "upv%da" % fg, "upv%db" % fg, "dn%da" % fg, "dn%db" % fg]
            pl += ["pp", "pg0", "pg1"]
            return pl

        plan = []
        for b in range(NBLK):
            plan += plan_block(b >= FIRST_FULL)
        W = {"issued": 0, "consumed": 0, "hold": None}

        def slotview(i):
            scr, k, n = units[plan[i]]
            s = i % NSLOT
            return wsl[:, s, 0:k * n].rearrange("p (k n) -> p k n", k=k), ("ws", s)

        def wget(name):
            n = W["consumed"]
            assert plan[n] == name, (plan[n], name)
            retired = n if W["hold"] is None else min(n, W["hold"])
            while W["issued"] < min(len(plan), retired + NSLOT):
                i = W["issued"]
                v, key = slotview(i)
                scr, k_, n_ = units[plan[i]]
                S.dma("sp", wsl[:, i % NSLOT, 0:k_ * n_], scr, reads=[("u", plan[i])], writes=[key])
                W["issued"] += 1
            assert W["issued"] > n, name
            W["consumed"] += 1
            return slotview(n)

        def norm_transpose(j, gcol):
            src = xres[:, j, :]
            act(junk2[:, 0:D], src, AF.Square, [("xres", j)], ["junk2", "ssq"], accum=sm[:, 16:17])
            act(sm[:, 17:18], sm[:, 16:17], AF.Sqrt, ["ssq"], ["rstd0"], bias=cv(C_EPS), scale=1.0 / D)
            S.op("dve", lambda en: en.reciprocal(out=sm[:, 18:19], in_=sm[:, 17:18]), ["rstd0"], ["rstd"])
            ts(xn_tm[:], src, sm[:, 18:19], None, ALU.mult, None, [("xres", j), "rstd"], ["xn_tm"])
            for kc in range(8):
                tr(pT_ps[:, kc * 128:(kc + 1) * 128], xn_tm[:, kc * 128:(kc + 1) * 128], ["xn_tm"], [("ps", "T")])
            tt(xnT[:, :, j * 128:(j + 1) * 128], pT_ps[:, 0:1024].rearrange("p (k t) -> p k t", k=8),
               cv(gcol, 8).unsqueeze(2).to_broadcast([128, 8, 128]), ALU.mult, [("ps", "T"), "cvec"], [("xnT", j)])

        XNK = [("xnT", j) for j in range(TB)]

        def proj_fm(wname, ncols, evac, m=128, rhsT=None, rkeys=None, rot4=True):
            wv, wkey = wget(wname)
            rhsT = xnT if rhsT is None else rhsT
            rkeys = XNK if rkeys is None else rkeys
            for ci in range(ncols // m):
                bank, bkey = nextbank(rot4)
                for kc in range(8):
                    mm(bank[0:m, 0:T], wv[:, kc, ci * m:(ci + 1) * m], rhsT[:, kc, :], kc == 0, kc == 7,
                       [wkey] + rkeys, [bkey])
                evac(ci, bank, bkey)

        def proj_tasks(wname, ncols, evac, m=128):
            st8 = {}

            def mk(ci):
                def f():
                    if ci == 0:
                        st8["w"] = wget(wname)
                    wv, wkey = st8["w"]
                    bank, bkey = nextbank()
                    for kc in range(8):
                        mm(bank[0:m, 0:T], wv[:, kc, ci * m:(ci + 1) * m], xnT[:, kc, :], kc == 0, kc == 7, [wkey] + XNK, [bkey])
                    evac(ci, bank, bkey)
                return f
            return [mk(ci) for ci in range(ncols // m)]

        def headnorm(bank, bkey, gcol_ap, gkey, out_ap, okeys):
            act(qsq[:], bank[:, 0:T], AF.Square, [bkey], ["qsq"])
            mm(pL1[:, 0:T], ones_b[:], qsq[:], True, True, ["ones", "qsq"], [("ps", "L1")])
            sd0, sd0k = arena(22 * T, T)
            sd1, sd1k = arena(23 * T, T)
            act(sd0, pL1[:, 0:T], AF.Sqrt, [("ps", "L1"), "cvec"], sd0k, bias=cv(C_EPS), scale=1.0 / 128)
            S.op("dve", lambda en: en.reciprocal(out=sd1, in_=sd0), sd0k, sd1k)
            stt(out_ap, bank[:, 0:T], gcol_ap, sd1, ALU.mult, ALU.mult, [bkey, gkey] + sd1k, okeys)

        def block(b, full):
            t0 = b * T
            tiles = [b * TB + j for j in range(TB)]
            S.phase = "P1.norm"
            for j, g in enumerate(tiles):
                S.dma("sp", xres[:, j, :], xbuf[g * 128:(g + 1) * 128, :], writes=[("xres", j)])
            for j in range(TB):
                norm_transpose(j, C_MIXG)
            S.phase = "P1.lru"
            cp(lrux[:, :, 0:3], lhalo[:], ["lhalo"], LXK, e="pool")

            def ev_lrux(base):
                def f(ci, bank, bkey):
                    cp(lrux[:, base + ci, 3:3 + T], bank[:, 0:T], [bkey], LXK)
                return f
            proj_fm("lrux0", 512, ev_lrux(0), rot4=False)
            proj_fm("lrux1", 512, ev_lrux(4), rot4=False)
            S.dma("sp", waxs[:].rearrange("p n d -> p (n d)"), units["wax"][0], reads=[("u", "wax")], writes=["waxs"])
            wax, waxk = waxs, "waxs"
            side = []

            def ev_kk(ci, bank, bkey):
                if ci < 2:
                    headnorm(bank, bkey, cv(C_GK), "cvec", kT[:, ci, t0:t0 + T], [("kT", g) for g in tiles])
                else:
                    cp(kiT[:, t0:t0 + T], bank[:, 0:T], [bkey], [("ki", g) for g in tiles])
            side += proj_tasks("kk", 384, ev_kk)
            vst = {}

            def vw_task(j, g):
                def f():
                    if j == 0:
                        vst["w"] = wget("vw")
                    vw, vwk = vst["w"]
                    bank, bkey = nextbank()
                    for kc in range(8):
                        mm(bank[:, 0:264], xnT[:, kc, j * 128:(j + 1) * 128], vw[:, kc, :], kc == 0, kc == 7, [vwk, ("xnT", j)], [bkey])
                    cp(Vt[:, g, :, 0:128], bank[:, 0:256].rearrange("p (a d) -> p a d", a=2), [bkey], [("V", g)])
                    if full:
                        ts(weff[:, j, :], bank[:, 256:264], float(8 ** -0.5 * 64 ** -0.5), None, ALU.mult, None, [bkey], [("weff", j)])
                return f
            side += [vw_task(j, g) for j, g in enumerate(tiles)]
            if full:
                def ev_q(base):
                    def f(ci, bank, bkey):
                        headnorm(bank, bkey, gqs[:, 0:1], "gqs", qT[:, base + ci, :], [("qT", base + ci)])
                    return f
                side += proj_tasks("q0", 512, ev_q(0))
                side += proj_tasks("q1", 512, ev_q(4))

                def ev_qi(ci, bank, bkey):
                    cp(qiT[:, ci, :], bank[:, 0:T], [bkey], ["qiT"])
                side += proj_tasks("qi", 512, ev_qi)
            per = (len(side) + 3) // 4
            if b == FIRST_OWN:
                ts(hstate[:], hstate[:], cv(C_CF), None, ALU.mult, None, ["hstate", "cvec"], ["hstate"])
            def lru_bufs(c):
                pb = c % 2
                base = pb * 7 * T
                names = ["ua", "r", "ig", "a", "a2", "m", "u"]
                d = {n: arena(base + i * T, T) for i, n in enumerate(names)}
                d["h"] = arena(HOFF + c * T, T)
                return pb, d

            for c0 in range(0, 8, 2):
                pair = (c0, c0 + 1)
                for c in pair:
                    pb, d = lru_bufs(c)
                    ua, uak = d["ua"]
                    ts(ua, lrux[:, c, 0:T], cv(C_LCW + c * 4), cv(C_LCB + c), ALU.mult, ALU.add, LXK + ["cvec"], uak)
                    for jj in range(1, 4):
                        stt(ua, lrux[:, c, jj:jj + T], cv(C_LCW + c * 4 + jj), ua, ALU.mult, ALU.add, LXK + ["cvec"] + uak, uak)
                    cp(uab[:, pb, :], ua, uak, [("uab", pb)])
                for c in pair:
                    pb, d = lru_bufs(c)
                    (r_, rk), (ig, igk) = d["r"], d["ig"]
                    lb, lk = (pL0, ("ps", "L0")) if pb == 0 else (pL1, ("ps", "L1"))
                    mm(lb[:, 0:T], wax[:, c, 0:128], uab[:, pb, :], True, True, [waxk, ("uab", pb)], [lk])
                    act(r_, lb[:, 0:T], AF.Tanh, [lk, "hb"], rk, bias=hba[:, c:c + 1], scale=0.5)
                    mm(lb[:, 0:T], wax[:, c, 128:256], uab[:, pb, :], True, True, [waxk, ("uab", pb)], [lk])
                    act(ig, lb[:, 0:T], AF.Tanh, [lk, "hb"], igk, bias=hbx[:, c:c + 1], scale=0.5)
                for c in pair:
                    pb, d = lru_bufs(c)
                    (r_, rk), (a_, ak), (a2, a2k) = d["r"], d["a"], d["a2"]
                    act(a_, r_, AF.Exp, rk + ["s8"], ak, bias=s8h[:, c:c + 1], scale=s8h[:, c:c + 1])
                    act(a2, r_, AF.Exp, rk + ["s16"], a2k, bias=s16h[:, c:c + 1], scale=s16h[:, c:c + 1])
                for c in pair:
                    pb, d = lru_bufs(c)
                    (a2, a2k), (m_, mk) = d["a2"], d["m"]
                    act(m_, a2, AF.Sqrt, a2k + ["cvec"], mk, bias=cv(C_ONE), scale=-1.0)
                for c in pair:
                    pb, d = lru_bufs(c)
                    (ua, uak), (ig, igk), (m_, mk), (u_, uk), (a_, ak), (hc, hck) = d["ua"], d["ig"], d["m"], d["u"], d["a"], d["h"]
                    stt(u_, ig, 1.0, ua, ALU.add, ALU.mult, igk + uak, uk)
                    stt(u_, u_, 0.5, m_, ALU.mult, ALU.mult, uk + mk, uk)
                    S.op("dve", lambda en: en.tensor_tensor_scan(out=hc, data0=a_, data1=u_, initial=hstate[:, c:c + 1],
                                                                 op0=ALU.mult, op1=ALU.add), ak + uk + ["hstate"], hck)
                    cp(hstate[:, c:c + 1], hc[:, T - 1:T], hck, ["hstate"], e="dve")
                S.phase = "P1.side"
                for _ in range(per):
                    if side:
                        side.pop(0)()
                S.phase = "P1.lru"
            while side:
                side.pop(0)()
            cp(lhalo[:], lrux[:, :, T:T + 3], LXK, ["lhalo"], e="pool")

            if not full:
                return

            S.phase = "P1.q"
            def ev_lg(base):
                def f(ci, bank, bkey):
                    c = base + ci
                    hc, hck = arena(HOFF + c * T, T)
                    gt = tmpf[:, ci % 2, 0:T]
                    act(gt, bank[:, 0:T], AF.Gelu_apprx_tanh, [bkey], [("tmpf", ci % 2)])
                    tt(hgT[:, c, :], gt, hc, ALU.mult, [("tmpf", ci % 2)] + hck, [("hgT", c)])
                return f
            proj_fm("lg0", 512, ev_lg(0))
            proj_fm("lg1", 512, ev_lg(4))

            for j, g in enumerate(tiles):
                attention(j, g)

            S.phase = "M.merge"
            ya_t = lambda m: arena(m * T, T)
            yb_t = lambda m: arena(8 * T + m * T, T)

            def ev_ya(base):
                def f(ci, bank, bkey):
                    v, k = ya_t(base + ci)
                    act(v, bank[:, 0:T], AF.Copy, [bkey], k, scale=0.5)
                return f
            HGK = [("hgT", c) for c in range(8)]
            proj_fm("lbr0", 512, ev_ya(0), rhsT=hgT, rkeys=HGK)
            proj_fm("lbr1", 512, ev_ya(4), rhsT=hgT, rkeys=HGK)

            def ev_ga(base):
                def f(ci, bank, bkey):
                    v, k = ya_t(base + ci)
                    sgt = tmpf[:, ci % 2, 0:T]
                    act(sgt, bank[:, 0:T], AF.Tanh, [bkey], [("tmpf", ci % 2)], scale=0.5)
                    stt(v, sgt, 1.0, v, ALU.add, ALU.mult, k + [("tmpf", ci % 2)], k)
                return f
            proj_fm("ga0", 512, ev_ga(0))
            proj_fm("ga1", 512, ev_ga(4))

            def ev_yb(base):
                def f(ci, bank, bkey):
                    v, k = yb_t(base + ci)
                    act(v, bank[:, 0:T], AF.Copy, [bkey], k, scale=0.5)
                return f
            ATK = [("attnT", jj) for jj in range(TB)]
            proj_fm("abr0", 512, ev_yb(0), rhsT=attnT, rkeys=ATK)
            proj_fm("abr1", 512, ev_yb(4), rhsT=attnT, rkeys=ATK)

            def ev_gb(base):
                def f(ci, bank, bkey):
                    m = base + ci
                    va, ka = ya_t(m)
                    vb, kb = yb_t(m)
                    sgt = tmpf[:, ci % 2, 0:T]
                    act(sgt, bank[:, 0:T], AF.Tanh, [bkey], [("tmpf", ci % 2)], scale=0.5)
                    stt(vb, sgt, 1.0, vb, ALU.add, ALU.mult, kb + [("tmpf", ci % 2)], kb)
                    tt(mergedT[:, m, :], vb, va, ALU.add, ka + kb, [("qT", m)])
                return f
            proj_fm("gb0", 512, ev_gb(0))
            proj_fm("gb1", 512, ev_gb(4))

            def tm_accum(wname, lhs, lkeys):
                wv, wkey = wget(wname)
                n = int(wname[-1] in "1b")
                for j in range(TB):
                    bank, bkey = nextbank(True)
                    for kc in range(8):
                        mm(bank[:, 0:512], lhs[:, kc, j * 128:(j + 1) * 128], wv[:, kc, :], kc == 0, kc == 7, [wkey] + lkeys, [bkey])
                    tt(xres[:, j, n * 512:(n + 1) * 512], xres[:, j, n * 512:(n + 1) * 512], bank[:, 0:512], ALU.add,
                       [bkey, ("xres", j)], [("xres", j)])
            MK = [("qT", m) for m in range(8)]
            tm_accum("wo0", mergedT, MK)
            tm_accum("wo1", mergedT, MK)

            S.phase = "F.ffn"
            for j in range(TB):
                norm_transpose(j, C_FFNG)
            for fg in range(3):
                def ev_gate(base):
                    def f(ci, bank, bkey):
                        c = base + ci
                        fc = fg * 8 + c
                        pb = c % 2
                        if b == FIRST_OWN:
                            ts(gbuf[:, pb, 0:2], fhalo[:, fc, :], cv(C_CF), None, ALU.mult, None, ["fhalo", "cvec"], [("gbuf", pb)], e="pool")
                        else:
                            cp(gbuf[:, pb, 0:2], fhalo[:, fc, :], ["fhalo"], [("gbuf", pb)], e="pool")
                        cp(gbuf[:, pb, 2:2 + T], bank[:, 0:T], [bkey], [("gbuf", pb)])
                        cp(fhalo[:, fc, :], gbuf[:, pb, T:T + 2], [("gbuf", pb)], ["fhalo"], e="pool")
                        gc = gcb[:, pb, :]
                        ts(gc, gbuf[:, pb, 0:T], cv(C_FCW + fc * 3), cv(C_FCB + fc), ALU.mult, ALU.add, [("gbuf", pb), "cvec"], [("gcb", pb)])
                        for jj in (1, 2):
                            stt(gc, gbuf[:, pb, jj:jj + T], cv(C_FCW + fc * 3 + jj), gc, ALU.mult, ALU.add,
                                [("gbuf", pb), "cvec", ("gcb", pb)], [("gcb", pb)])
                        glv, glk = arena(c * T, T)
                        act(glv, gc, AF.Gelu_apprx_tanh, [("gcb", pb)], glk)
                    return f
                proj_fm("upg%da" % fg, 512, ev_gate(0))
                proj_fm("upg%db" % fg, 512, ev_gate(4))

                def ev_val(base):
                    def f(ci, bank, bkey):
                        c = base + ci
                        glv, glk = arena(c * T, T)
                        tt(actT[:, c, :], glv, bank[:, 0:T], ALU.mult, glk + [bkey], [("hgT", c)])
                    return f
                proj_fm("upv%da" % fg, 512, ev_val(0))
                proj_fm("upv%db" % fg, 512, ev_val(4))
                AK = [("hgT", c) for c in range(8)]
                tm_accum("dn%da" % fg, actT, AK)
                tm_accum("dn%db" % fg, actT, AK)

            S.phase = "E.ple"
            for j in range(TB):
                norm_transpose(j, C_PLEG)
            for j, g in enumerate(tiles):
                S.dma("pool", p_tm[:], pbuf[g * 128:(g + 1) * 128, :], writes=["p_tm"])
                for k2 in range(2):
                    tr(pT_ps[:, k2 * 128:(k2 + 1) * 128], p_tm[:, k2 * 128:(k2 + 1) * 128], ["p_tm"], [("ps", "T")])
                cp(pT[:, :, j * 128:(j + 1) * 128], pT_ps[:, 0:256].rearrange("p (k t) -> p k t", k=2), [("ps", "T")], [("pT", j)])
            pp, ppk = wget("pp")
            W["hold"] = W["consumed"] - 1
            for n in range(2):
                pg, pgk = wget("pg%d" % n)
                for j, g in enumerate(tiles):
                    bank, bkey = nextbank()
                    for kc in range(8):
                        mm(bank[:, 0:512], xnT[:, kc, j * 128:(j + 1) * 128], pg[:, kc, :], kc == 0, kc == 7, [pgk, ("xnT", j)], [bkey])
                    sg = tmpf[:, 0, :]
                    act(sg, bank[:, 0:512], AF.Tanh, [bkey], [("tmpf", 0)], scale=0.5)
                    for k2 in range(2):
                        mm(pL0[:, 0:512], pT[:, k2, j * 128:(j + 1) * 128], pp[:, k2, n * 512:(n + 1) * 512], k2 == 0, k2 == 1,
                           [ppk, ("pT", j)], [("ps", "L0")])
                    stt(sg, sg, 1.0, pL0[:, 0:512], ALU.add, ALU.mult, [("tmpf", 0), ("ps", "L0")], [("tmpf", 0)])
                    stt(xres[:, j, n * 512:(n + 1) * 512], sg, 0.5, xres[:, j, n * 512:(n + 1) * 512], ALU.mult, ALU.add,
                        [("tmpf", 0), ("xres", j)], [("xres", j)])
            W["hold"] = None
            if b >= FIRST_OWN:
                for j, g in enumerate(tiles):
                    S.dma("pool", out[(g - NCT) * 128:(g - NCT + 1) * 128, :], xres[:, j, :], reads=[("xres", j)], writes=[("out", g)])

        HOFF = 14 * T
        assert HOFF + 8 * T <= ARN

        OB = [(pOA, "OA", 0), (pOA, "OA", 1), (pOA, "OA", 2), (pOB, "OB", 0), (pOB, "OB", 1), (pOB, "OB", 2), (pOC, "OC", 0), (pOC, "OC", 1)]

        def attention(j, g):
            L = (g + 1) * 128
            nch = (L + 511) // 512
            js = slice(j * 128, (j + 1) * 128)
            S.phase = "A.idx"
            for ch in range(nch):
                n = min(512, L - ch * 512)
                sc = score[:, ch * 512:ch * 512 + n]
                sk = [("sc", ch)]
                kik = [("ki", kt) for kt in range(ch * 4, ch * 4 + n // 128)]
                for h in range(8):
                    bank, bkey = nextbank_idx()
                    p0 = 64 * (h % 2)
                    mm(bank[:, 0:n], qiT[p0:p0 + 64, h // 2, js], kiT[p0:p0 + 64, ch * 512:ch * 512 + n], True, True,
                       ["qiT"] + kik, [bkey])
                    act(rl[:, h % 2, 0:n], bank[:, 0:n], AF.Relu, [bkey], [("tmpf", h % 2)])
                    if h == 0:
                        ts(sc, rl[:, 0, 0:n], weff[:, j, 0:1], None, ALU.mult, None, [("tmpf", 0), ("weff", j)], sk)
                    else:
                        stt(sc, rl[:, h % 2, 0:n], weff[:, j, h:h + 1], sc, ALU.mult, ALU.add, [("tmpf", h % 2), ("weff", j)] + sk, sk)
            allk = sck(0, L)
            S.phase = "A.bis"
            S.op("dve", lambda en: en.tensor_reduce(out=sm[:, 20:21], in_=score[:, 0:L], axis=AX.X, op=ALU.max), allk, ["rmax"])
            S.op("dve", lambda en: en.tensor_reduce(out=sm[:, 21:22], in_=score[:, 0:L], axis=AX.X, op=ALU.min), allk, ["rmin"])
            tt(sm[:, 22:23], sm[:, 21:22], sm[:, 20:21], ALU.subtract, ["rmin", "rmax"], ["dd"])
            lo = sm[:, 23:24]
            stt(lo, sm[:, 22:23], 1.0 / 64, sm[:, 21:22], ALU.mult, ALU.add, ["dd", "rmin"], ["lo"])
            tt(sm[:, 24:25], sm[:, 20:21], lo, ALU.subtract, ["rmax", "lo"], ["w0"])
            ts(hw[:], cv(C_POW, NIT), sm[:, 24:25], None, ALU.mult, None, ["cvec", "w0"], ["hw"])
            dg = score[:, g * 128:(g + 1) * 128]
            tt(dg, dg, tri[:], ALU.add, [("sc", g // 4), "tri"], [("sc", g // 4)])
            cend = min(L, CTXK)
            nA = min(cend, 3072 if L < 6656 else 4096)
            act_pieces = []
            if nA > 0:
                n0 = min(nA, 2048)
                act_pieces.append((0, n0, junk2[:, 2048:2048 + n0], "junk2b"))
                for a_, (pb_, pk_) in zip(range(2048, nA, 512), ((pA, ("ps", "A")), (pB, ("ps", "B")),
                                                                  (pL0, ("ps", "L0")), (pL1, ("ps", "L1")))):
                    n_ = min(512, nA - a_)
                    act_pieces.append((a_, n_, pb_[:, 0:n_], pk_))
            assert len(act_pieces) <= 5 and sum(p[1] for p in act_pieces) == nA
            dve_pieces = []
            if cend > nA:
                assert cend - nA <= 2048
                dve_pieces.append((nA, cend - nA, 0))
            col = 1
            for a in range(CTXK, L, 2048):
                dve_pieces.append((a, min(2048, L - a), col))
                col += 1
            assert col <= 3
            mid, tot, stp, kadj = sm[:, 25:26], sm[:, 27:28], sm[:, 28:29], sm[:, 30:31]
            prod = sm[:, 40:48]
            CK = [("cnt", k) for k in range(8)]
            S.op("dve", lambda en: en.memset(cntb[:, 0:8], 0.0), [], CK)
            ts(kadj, cv(C_CF), -0.5 * nA, float(TOPK), ALU.mult, ALU.add, ["cvec"], ["kadj"])
            ts(mid, lo, hw[:, 0:1], None, ALU.add, None, ["lo", "hw"], ["mid"])
            for it in range(NIT):
                for k, (a, n, dst, dkey) in enumerate(act_pieces):
                    act(dst, score[:, a:a + n], AF.Sign, sck(a, a + n) + ["mid"], [dkey, ("cnt", 3 + k)],
                        bias=mid, scale=-1.0, accum=cntb[:, 3 + k:4 + k])
                for (a, n, c) in dve_pieces:
                    ts(junk2[:, 0:n], score[:, a:a + n], mid, None, ALU.is_gt, ALU.add,
                       sck(a, a + n) + ["mid"], ["junk2", ("cnt", c)], accum=cntb[:, c:c + 1])
                tt(prod, cntb[:, 0:8], wv[:, 0:8], ALU.mult, CK + ["wv"], ["prod"])
                S.op("dve", lambda en: en.tensor_reduce(out=tot, in_=prod, axis=AX.X, op=ALU.add), ["prod"], ["tot"])
                stt(stp, tot, kadj, hw[:, it:it + 1], ALU.is_ge, ALU.mult, ["tot", "hw", "kadj"], ["stp"])
                nx = min(it + 1, NIT - 1)
                dst, dk = (mid, "mid") if it < NIT - 1 else (lo, "lo")
                stt(dst, mid, hw[:, nx:nx + 1], stp, ALU.subtract, ALU.add, ["mid", "hw", "stp"], [dk])
            loc = sm[:, 29:30]
            ts(loc, lo, cv(C_CF), cv(C_CF + 1), ALU.mult, ALU.add, ["lo", "cvec"], ["loc"])
            S.phase = "A.att"
            steps = [(kt, kv) for kt in range(g + 1) for kv in range(2)]

            def qk(i):
                kt, kv = steps[i]
                ch, off = kt // 4, (kt % 4) * 128
                if kt % 4 == 0 and kv == 0:
                    n = min(512, L - ch * 512)
                    isctx = ch * 512 < CTXK
                    ts(nm[:, ch % 2, 0:n], score[:, ch * 512:ch * 512 + n], (loc if isctx else lo), -30000.0, ALU.is_le, ALU.mult,
                       [("sc", ch), "lo", "loc"], [("nm", ch % 2)])
                near = kt >= g - 1
                lgb, lgk = (pL0, ("ps", "L0")) if kv == 0 else (pL1, ("ps", "L1"))
                lg3 = lgb[:, 0:512].rearrange("p (a t) -> p a t", a=4)
                mm(lg3, kT[:, kv, kt * 128:(kt + 1) * 128], qT[:, 4 * kv:4 * kv + 4, js], True, False,
                   [("kT", kt)] + [("qT", 4 * kv + a) for a in range(4)], [lgk])
                mm(lg3, nm[:, ch % 2, off:off + 128], ident4[:], False, not near, [("nm", ch % 2), "ident4"], [lgk])
                if near:
                    mm(lg3, ident[:], biasn[:, g - kt, 4 * kv:4 * kv + 4, :], False, True, ["ident", "biasn"], [lgk])

            def pv(i):
                kt, kv = steps[i]
                lgb, lgk = (pL0, ("ps", "L0")) if kv == 0 else (pL1, ("ps", "L1"))
                pk = ("pTb", i % 2)
                pvv = pTb[:, i % 2, :]
                act(pvv, lgb[:, 0:512], AF.Exp, [lgk], [pk])
                for a in range(4):
                    hd = 4 * kv + a
                    ob, obk, sl = OB[hd]
                    mm(ob[:, sl * 129:(sl + 1) * 129], pvv[:, a * 128:(a + 1) * 128], Vt[:, kt, kv, :], (kt == 0 and sl == 0), kt == g,
                       [pk, ("V", kt), "Vones"], [("ps", obk)], skip=True)

            qk(0)
            qk(1)
            for i in range(len(steps)):
                pv(i)
                if i + 2 < len(steps):
                    qk(i + 2)
            S.phase = "A.fin"
            for (ob, obk, nh, h0) in ((pOA, "OA", 3, 0), (pOB, "OB", 3, 3), (pOC, "OC", 2, 6)):
                o3 = ob[:, 0:nh * 129].rearrange("p (h d) -> p h d", d=129)
                ts(den[:, h0:h0 + nh], o3[:, :, 128], 1e-30, None, ALU.max, None, [("ps", obk)], ["den"])
            S.op("dve", lambda en: en.reciprocal(out=rden[:], in_=den[:]), ["den"], ["rden"])
            for (ob, obk, nh, h0) in ((pOA, "OA", 3, 0), (pOB, "OB", 3, 3), (pOC, "OC", 2, 6)):
                o3 = ob[:, 0:nh * 129].rearrange("p (h d) -> p h d", d=129)
                tt(attn_tm[:, h0 * 128:(h0 + nh) * 128].rearrange("p (h d) -> p h d", d=128), o3[:, :, 0:128],
                   rden[:, h0:h0 + nh].unsqueeze(2).to_broadcast([128, nh, 128]), ALU.mult, [("ps", obk), "rden"], ["xn_tm"])
            for hd in range(8):
                tr(pT_ps[:, hd * 128:(hd + 1) * 128], attn_tm[:, hd * 128:(hd + 1) * 128], ["xn_tm"], [("ps", "T")])
            cp(attnT[:, :, js], pT_ps[:, 0:1024].rearrange("p (h t) -> p h t", h=8), [("ps", "T")], [("attnT", j)])

        for b in range(NBLK):
            block(b, b >= FIRST_FULL)
        S.final_wait("sp", [("out", g) for g in range(NCT, NT)])
        S.final_wait("pool", [("out", g) for g in range(NCT, NT)])
    print("ops", S.nops, {e: S.cnt[e] for e in S.cnt})
    return nc


def _bucket(dist):
    d_f = np.maximum(dist, 1).astype(np.float32)
    large = 16 + (np.log(d_f / np.float32(16)) / np.float32(math.log(128 / 16)) * np.float32(16)).astype(np.int32)
    large = np.minimum(large, 31)
    return np.where(dist < 16, dist, large)


def make_inputs(inp, NT, NIT=20):
    f32 = np.float32
    x = np.asarray(inp["x"], f32)
    p = np.asarray(inp["p"], f32)[0]
    Bn, SEQ, _ = x.shape
    half = SEQ // 2
    assert NT * 128 == SEQ

    def col(v, nch):
        return np.ascontiguousarray(np.asarray(v, f32).reshape(nch, 128).T)

    cvec = np.zeros((128, C_POW + NIT), f32)
    cvec[:, C_MIXG:C_MIXG + 8] = col(inp["mix_norm"][0], 8)
    cvec[:, C_FFNG:C_FFNG + 8] = col(inp["ffn_norm"][0], 8)
    cvec[:, C_PLEG:C_PLEG + 8] = col(inp["ple_norm"][0], 8)
    lcw = np.asarray(inp["lru_conv_w"], f32)[0]
    cvec[:, C_LCW:C_LCW + 32] = lcw.reshape(4, 8, 128).transpose(2, 1, 0).reshape(128, 32)
    cvec[:, C_LCB:C_LCB + 8] = col(inp["lru_conv_b"][0], 8)
    cvec[:, C_LBA:C_LBA + 8] = col(inp["lru_ba"][0], 8)
    cvec[:, C_LBX:C_LBX + 8] = col(inp["lru_bx"][0], 8)
    cvec[:, C_LAM:C_LAM + 8] = col(inp["lru_lambda"][0], 8)
    cvec[:, C_GQ] = np.asarray(inp["q_norm"], f32)[0]
    cvec[:, C_GK] = np.asarray(inp["k_norm"], f32)[0]
    fcw = np.asarray(inp["ffn_conv_w"], f32)[0]
    cvec[:, C_FCW:C_FCW + 72] = fcw.reshape(3, 24, 128).transpose(2, 1, 0).reshape(128, 72)
    cvec[:, C_FCB:C_FCB + 24] = col(inp["ffn_conv_b"][0], 24)
    cvec[:, C_EPS] = EPS
    cvec[:, C_ONE] = 1.0
    cvec[:, C_POW:C_POW + NIT] = (0.5 ** np.arange(1, NIT + 1, dtype=np.float64)).astype(f32)[None, :]

    tq = np.arange(128)[:, None]
    sk = np.arange(128)[None, :]
    tri = np.where(sk <= tq, 0.0, -1e30).astype(f32)
    ident = np.eye(128, dtype=f32)
    rb = np.asarray(inp["rel_bias"], f32)
    ss = np.arange(128)[:, None, None]
    oo = np.arange(2)[None, :, None]
    tt_ = np.arange(128)[None, None, :]
    dist = np.maximum(tt_ - ss + 128 * oo, 0).astype(np.int32)
    bidx = _bucket(dist)
    bn = rb[bidx]
    bn = np.ascontiguousarray(bn.transpose(0, 1, 3, 2)).reshape(128, 2 * 8 * 128)
    b31 = np.ascontiguousarray(rb[np.full_like(bidx, 31)].transpose(0, 1, 3, 2)).reshape(128, 2 * 8 * 128)

    shared = {
        "w_in": np.ascontiguousarray(inp["w_in"][0], f32), "w_lru_br": np.ascontiguousarray(inp["w_lru_br"][0], f32),
        "w_attn_br": np.ascontiguousarray(inp["w_attn_br"][0], f32), "w_out": np.ascontiguousarray(inp["w_out"][0], f32),
        "w_up": np.ascontiguousarray(inp["w_up"][0], f32), "w_down": np.ascontiguousarray(inp["w_down"][0], f32),
        "w_ple_gate": np.ascontiguousarray(inp["w_ple_gate"][0], f32), "w_ple_proj": np.ascontiguousarray(inp["w_ple_proj"][0], f32),
        "lru_wa": np.ascontiguousarray(inp["lru_wa"][0], f32), "lru_wx": np.ascontiguousarray(inp["lru_wx"][0], f32),
        "tri": tri, "ident": ident, "bias_near": bn, "bias_far": b31,
    }
    maps = []
    for c in range(2 * Bn):
        b, h = c // 2, c % 2
        cv_c = cvec.copy()
        if h == 1:
            xb, pb = x[b], p[b]
            cv_c[:, C_CF], cv_c[:, C_CF + 1] = 1.0, 0.0
        else:
            xb = np.concatenate([np.zeros((half, D), f32), x[b, :half]], axis=0)
            pb = np.concatenate([np.zeros((half, 256), f32), p[b, :half]], axis=0)
            cv_c[:, C_CF], cv_c[:, C_CF + 1] = 0.0, 1e30
        m = dict(shared)
        m["xbuf"] = np.ascontiguousarray(xb)
        m["pbuf"] = np.ascontiguousarray(pb)
        m["cvec"] = cv_c
        maps.append(m)
    return maps


def run(inp, NT, NIT=20):
    x = np.asarray(inp["x"])
    Bn, SEQ, _ = x.shape
    half = SEQ // 2
    nc = build(NT, NIT=NIT)
    maps = make_inputs(inp, NT, NIT=NIT)
    res = run_bass_kernel_spmd(nc, maps, core_ids=list(range(2 * Bn)))
    outp = np.empty((Bn, SEQ, D), np.float32)
    for c in range(2 * Bn):
        b, h = c // 2, c % 2
        outp[b, h * half:(h + 1) * half] = res.results[c]["out"]
    return outp


def kernel(**inputs):
    return run(inputs, 64, NIT=20)
```

```python
import math
from contextlib import ExitStack

import numpy as np
import concourse.bass as bass
import concourse.mybir as mybir
from concourse.bass_utils import run_bass_kernel_spmd

F32 = mybir.dt.float32
BF16 = mybir.dt.bfloat16
AF = mybir.ActivationFunctionType
ALU = mybir.AluOpType
AX = mybir.AxisListType

D = 1024
TB = 2
T = TB * 128
NSLOT = 4
EPS = 1e-6

C_MIXG, C_FFNG, C_PLEG = 0, 8, 16
C_LCW, C_LCB, C_LBA, C_LBX, C_LAM = 24, 56, 64, 72, 80
C_GQ, C_GK = 88, 89
C_FCW, C_FCB = 90, 162
C_CF = 186
C_EPS, C_ONE = 188, 189
C_POW = 190


class Sched:
    def __init__(self, nc, ndma=24):
        self.nc = nc
        self.eng = {"pe": nc.tensor, "act": nc.scalar, "dve": nc.vector, "pool": nc.gpsimd, "sp": nc.sync}
        self.sems = {}
        self.cnt = {}
        self.waited = {e: {} for e in self.eng}
        self.bufs = {}
        self.ndma = ndma
        self.dma_i = 0
        self.pool_i = 0
        self.nops = 0
        self.phase = None

    def setup(self, stack):
        for e in self.eng:
            self.sems[e] = stack.enter_context(self.nc.semaphore("s_" + e))
            self.cnt[e] = 0
        self.dsems = [stack.enter_context(self.nc.semaphore("d%d" % i)) for i in range(self.ndma)]
        self.dcnt = [0] * self.ndma
        self.semobj = dict(self.sems)
        for i, s in enumerate(self.dsems):
            self.semobj["d%d" % i] = s

    def _deps(self, reads, writes):
        deps = {}

        def add(k, v):
            if deps.get(k, 0) < v:
                deps[k] = v
        for b in reads:
            st = self.bufs.get(b)
            if st and st["w"]:
                add(*st["w"])
        for b in writes:
            st = self.bufs.get(b)
            if st:
                if st["w"]:
                    add(*st["w"])
                for k, v in st["r"].items():
                    add(k, v)
        return deps

    def _wait(self, e, deps, keep_one=False):
        eng = self.eng[e]
        need = [(k, v) for k, v in deps.items() if self.waited[e].get(k, 0) < v]
        inline = None
        if keep_one and need:
            inline = need.pop()
        for k, v in need:
            eng.wait_ge(self.semobj[k], v)
            self.waited[e][k] = v
        if inline is not None:
            self.waited[e][inline[0]] = inline[1]
            return (self.semobj[inline[0]], inline[1])
        return None

    def _update(self, ev, reads, writes):
        for b in reads:
            st = self.bufs.setdefault(b, {"w": None, "r": {}})
            if st["r"].get(ev[0], 0) < ev[1]:
                st["r"][ev[0]] = ev[1]
        for b in writes:
            self.bufs[b] = {"w": ev, "r": {}}

    def op(self, e, fn, reads=(), writes=()):
        deps = self._deps(reads, writes)
        if e == "pe":
            deps.pop("pe", None)
        inl = self._wait(e, deps, keep_one=True)
        ins = fn(self.eng[e])
        if inl is not None:
            ins._wait_ge(inl[0], inl[1])
        if self.phase is not None:
            ins.annotate(self.phase)
        self.cnt[e] += 1
        ins.then_inc(self.sems[e], 1)
        self._update((e, self.cnt[e]), reads, writes)
        self.nops += 1
        return ins

    def dma(self, e, out, in_, reads=(), writes=()):
        if e == "pool":
            i = self.pool_i % 3
            self.pool_i += 1
        else:
            i = 3 + self.dma_i % (self.ndma - 3)
            self.dma_i += 1
        key = "d%d" % i
        deps = self._deps(reads, writes)
        if self.dcnt[i] > 0:
            deps[key] = max(deps.get(key, 0), self.dcnt[i])
        self._wait(e, deps)
        ins = self.eng[e].dma_start(out=out, in_=in_)
        self.dcnt[i] += 16
        ins.then_inc(self.dsems[i], 16)
        self._update((key, self.dcnt[i]), reads, writes)
        self.nops += 1
        return ins

    def final_wait(self, e, bufs):
        self._wait(e, self._deps(bufs, bufs))


def build(NT, NIT=20, TOPK=256):
    NCT = NT // 2
    NTOK = NT * 128
    NBLK = NT // TB
    FIRST_OWN = NCT // TB
    FIRST_FULL = FIRST_OWN - 1
    CTXK = NCT * 128
    NOWN = (NT - NCT) * 128

    nc = bass.Bass("TRN2", target_bir_lowering=False)

    def din(name, shape, dt=F32):
        return nc.dram_tensor(name, shape, dt, kind="ExternalInput").ap()

    def dscr(name, shape, dt=BF16):
        return nc.dram_tensor(name, shape, dt, kind="Internal").ap()

    xbuf = din("xbuf", [NTOK, D])
    pbuf = din("pbuf", [NTOK, 256])
    w_in = din("w_in", [D, 6216])
    w_lbr = din("w_lru_br", [D, D])
    w_abr = din("w_attn_br", [D, D])
    w_out = din("w_out", [D, D])
    w_up = din("w_up", [D, 6144])
    w_down = din("w_down", [3072, D])
    w_pg = din("w_ple_gate", [D, D])
    w_pp = din("w_ple_proj", [256, D])
    lru_wa = din("lru_wa", [8, 128, 128])
    lru_wx = din("lru_wx", [8, 128, 128])
    cvec_d = din("cvec", [128, C_POW + NIT])
    tri_d = din("tri", [128, 128])
    ident_d = din("ident", [128, 128])
    bn_d = din("bias_near", [128, 2 * 8 * 128])
    b31_d = din("bias_far", [128, 2 * 8 * 128])
    out = nc.dram_tensor("out", [NOWN, D], F32, kind="ExternalOutput").ap()

    S = Sched(nc)
    with ExitStack() as st:
        S.setup(st)

        def sb(name, shape, dt=F32):
            return st.enter_context(nc.sbuf_tensor(name, shape, dt))

        kT = sb("kT", [128, 2, NTOK], BF16)
        Vt = sb("Vt", [128, NT, 2, 129], BF16)
        kiT = sb("kiT", [128, NTOK], BF16)
        wsl = sb("wsl", [128, NSLOT, 4096], BF16)
        lhalo = sb("lhalo", [128, 8, 3])
        cvec = sb("cvec_s", [128, C_POW + NIT])
        tri = sb("tri_s", [128, 128])
        ident_f = sb("ident_f", [128, 128])
        ident = sb("ident_b", [128, 128], BF16)
        ident4 = sb("ident4", [128, 4, 128], BF16)
        ones_b = sb("ones_b", [128, 128], BF16)
        biasn = sb("biasn", [128, 2, 8, 128], BF16)
        s8h = sb("s8h", [128, 8])
        s16h = sb("s16h", [128, 8])
        hba = sb("hba", [128, 8])
        hbx = sb("hbx", [128, 8])
        gqs = sb("gqs", [128, 1])
        hstate = sb("hstate", [128, 8])
        fhalo = sb("fhalo", [128, 24, 2])
        xres = sb("xres", [128, TB, D])
        xnT = sb("xnT", [128, 8, T], BF16)
        xn_tm = sb("xn_tm", [128, D], BF16)
        qT = sb("qT", [128, 8, T], BF16)
        qiT = sb("qiT", [128, 4, T], BF16)
        weff = sb("weff", [128, TB, 8])
        hgT = sb("hgT", [128, 8, T], BF16)
        attnT = sb("attnT", [128, 8, T], BF16)
        attn_tm = xn_tm
        mergedT = qT
        actT = hgT
        pT = sb("pT", [128, 2, T], BF16)
        p_tm = sb("p_tm", [128, 256], BF16)
        sm = sb("sm", [128, 64])
        hw = sb("hw", [128, NIT])
        cntb = sb("cntb", [128, 8])
        wv = sb("wv", [128, 8])
        den = sb("den", [128, 8])
        rden = sb("rden", [128, 8])
        uab = sb("uab", [128, 2, T], BF16)
        qsq = sb("qsq", [128, T], BF16)
        tmpf = sb("tmpf", [128, 2, 512])
        gbuf = sb("gbuf", [128, 2, T + 2])
        gcb = sb("gcb", [128, 2, T])
        rl = tmpf
        nm = sb("nm", [128, 2, 512], BF16)
        pTb = sb("pTb", [128, 2, 512], BF16)
        waxs = sb("waxs", [128, 8, 256], BF16)
        junk2 = sb("junk2", [128, 4096], BF16)
        ARN = max(NTOK, 32 * T + 24)
        score = sb("score", [128, ARN])
        LOFF = 24 * T
        lrux = score[:, LOFF:LOFF + 8 * (T + 3)].rearrange("p (c t) -> p c t", c=8)
        LXK = [("sc", c) for c in range(LOFF // 512, (LOFF + 8 * (T + 3) - 1) // 512 + 1)]
        pA = st.enter_context(nc.psum_tensor("pA", [128, 512], F32))
        pB = st.enter_context(nc.psum_tensor("pB", [128, 512], F32))
        pL0 = st.enter_context(nc.psum_tensor("pL0", [128, 512], F32))
        pL1 = st.enter_context(nc.psum_tensor("pL1", [128, 512], F32))
        pOA = st.enter_context(nc.psum_tensor("pOA", [128, 512], F32))
        pOB = st.enter_context(nc.psum_tensor("pOB", [128, 512], F32))
        pOC = st.enter_context(nc.psum_tensor("pOC", [128, 512], F32))
        pT_ps = st.enter_context(nc.psum_tensor("pTp", [128, 1024], BF16))
        st.enter_context(nc.Block())

        banks = {"A": pA, "B": pB, "OA": pOA, "OB": pOB, "OC": pOC, "L0": pL0, "L1": pL1}
        rot = {"n": 0, "i": 0}
        DENSE_ROT = ["A", "B"]

        ROT4 = ["A", "B", "L0", "L1"]

        def nextbank(rot4=False):
            r = ROT4 if rot4 else DENSE_ROT
            k = r[rot["n"] % len(r)]
            rot["n"] += 1
            return banks[k], ("ps", k)

        def nextbank_idx():
            k = "AB"[rot["i"] % 2]
            rot["i"] += 1
            return banks[k], ("ps", k)

        def cv(c0, n=1):
            return cvec[:, c0:c0 + n]

        def sck(a, b):
            return [("sc", c) for c in range(a // 512, (b - 1) // 512 + 1)]

        def arena(a, n):
            return score[:, a:a + n], sck(a, a + n)

        def act(out, in_, func, reads, writes, bias=None, scale=None, accum=None, e="act"):
            kw = {}
            if bias is not None:
                kw["bias"] = bias
            if scale is not None:
                kw["scale"] = scale
            if accum is not None:
                kw["accum_out"] = accum
            return S.op(e, lambda en: en.activation(out=out, in_=in_, func=func, **kw), reads, writes)

        def ts(out, in0, s1, s2, op0, op1, reads, writes, accum=None, e="dve"):
            kw = {}
            if accum is not None:
                kw["accum_out"] = accum
            if op1 is None:
                return S.op(e, lambda en: en.tensor_scalar(out=out, in0=in0, scalar1=s1, scalar2=s2, op0=op0, **kw), reads, writes)
            return S.op(e, lambda en: en.tensor_scalar(out=out, in0=in0, scalar1=s1, scalar2=s2, op0=op0, op1=op1, **kw), reads, writes)

        def stt(out, in0, scalar, in1, op0, op1, reads, writes, e="dve"):
            return S.op(e, lambda en: en.scalar_tensor_tensor(out=out, in0=in0, scalar=scalar, in1=in1, op0=op0, op1=op1), reads, writes)

        def tt(out, in0, in1, op, reads, writes, e="dve"):
            return S.op(e, lambda en: en.tensor_tensor(out=out, in0=in0, in1=in1, op=op), reads, writes)

        def cp(out, in_, reads, writes, e="act"):
            if e == "act":
                return S.op(e, lambda en: en.activation(out=out, in_=in_, func=AF.Copy), reads, writes)
            return S.op(e, lambda en: en.tensor_copy(out=out, in_=in_), reads, writes)

        def mm(out, lhsT, rhs, start, stop, reads, writes, skip=False):
            if skip:
                return S.op("pe", lambda en: en.matmul(out, lhsT=lhsT, rhs=rhs, start=start, stop=stop, skip_group_check=True), reads, writes)
            return S.op("pe", lambda en: en.matmul(out, lhsT=lhsT, rhs=rhs, start=start, stop=stop), reads, writes)

        def tr(out, in_, reads, writes):
            return S.op("pe", lambda en: en.transpose(out, in_, ident[:]), reads + ["ident"], writes)

        S.dma("sp", cvec[:], cvec_d, writes=["cvec"])
        S.dma("sp", tri[:], tri_d, writes=["tri"])
        S.dma("sp", ident_f[:], ident_d, writes=["identf"])
        bn_t, bn_k = arena(0, 2048)
        b31_t, b31_k = arena(2048, 2048)
        S.dma("sp", bn_t, bn_d, writes=bn_k)
        S.dma("sp", b31_t, b31_d, writes=b31_k)

        units = {}
        unit_order = []

        def unit(name, kc, ncols, parts):
            scr = nc.dram_tensor("u_" + name, [128, kc * ncols], BF16, kind="Internal").ap()
            units[name] = (scr, kc, ncols)
            unit_order.append(name)
            d3 = scr.rearrange("p (k n) -> p k n", k=kc)
            for (dc, src3) in parts:
                w = src3.shape[2]
                S.dma("pool", d3[:, :, dc:dc + w], src3, writes=[("u", name)])

        def kp(ap, r0, nr, c0, ncol):
            return ap[r0:r0 + nr, c0:c0 + ncol].rearrange("(k p) n -> p k n", p=128)

        unit("lrux0", 8, 512, [(0, kp(w_in, 0, D, 0, 512))])
        unit("lrux1", 8, 512, [(0, kp(w_in, 0, D, 512, 512))])
        unit("wax", 8, 256, [(0, lru_wa.rearrange("n c d -> c n d")), (128, lru_wx.rearrange("n c d -> c n d"))])
        unit("kk", 8, 384, [(0, kp(w_in, 0, D, 3072, 256)), (256, kp(w_in, 0, D, 4096, 64)), (320, kp(w_in, 0, D, 4096, 64))])
        unit("vw", 8, 264, [(0, kp(w_in, 0, D, 3328, 256)), (256, kp(w_in, 0, D, 4160, 8))])
        unit("q0", 8, 512, [(0, kp(w_in, 0, D, 2048, 512))])
        unit("q1", 8, 512, [(0, kp(w_in, 0, D, 2560, 512))])
        unit("qi", 8, 512, [(0, kp(w_in, 0, D, 3584, 512))])
        unit("lg0", 8, 512, [(0, kp(w_in, 0, D, 1024, 512))])
        unit("lg1", 8, 512, [(0, kp(w_in, 0, D, 1536, 512))])
        for hh in range(2):
            unit("lbr%d" % hh, 8, 512, [(0, kp(w_lbr, 0, D, hh * 512, 512))])
        for hh in range(2):
            unit("ga%d" % hh, 8, 512, [(0, kp(w_in, 0, D, 4168 + hh * 512, 512))])
        for hh in range(2):
            unit("abr%d" % hh, 8, 512, [(0, kp(w_abr, 0, D, hh * 512, 512))])
        for hh in range(2):
            unit("gb%d" % hh, 8, 512, [(0, kp(w_in, 0, D, 5192 + hh * 512, 512))])
        for hh in range(2):
            unit("wo%d" % hh, 8, 512, [(0, kp(w_out, 0, D, hh * 512, 512))])
        for fg in range(3):
            for hh, ab in enumerate("ab"):
                unit("upg%d%s" % (fg, ab), 8, 512, [(0, kp(w_up, 0, D, fg * 1024 + hh * 512, 512))])
            for hh, ab in enumerate("ab"):
                unit("upv%d%s" % (fg, ab), 8, 512, [(0, kp(w_up, 0, D, 3072 + fg * 1024 + hh * 512, 512))])
            for hh, ab in enumerate("ab"):
                unit("dn%d%s" % (fg, ab), 8, 512, [(0, kp(w_down, fg * 1024, 1024, hh * 512, 512))])
        unit("pp", 2, 1024, [(0, kp(w_pp, 0, 256, 0, 1024))])
        for hh in range(2):
            unit("pg%d" % hh, 8, 512, [(0, kp(w_pg, 0, D, hh * 512, 512))])

        cp(ident[:], ident_f[:], ["identf"], ["ident"], e="dve")
        for a4 in range(4):
            cp(ident4[:, a4, :], ident_f[:], ["identf"], ["ident4"], e="dve")
        S.op("dve", lambda en: en.memset(ones_b[:], 1.0), [], ["ones"])
        S.op("dve", lambda en: en.memset(Vt[:].rearrange("p n k d -> p (n k) d")[:, :, 128:129], 1.0), [], ["Vones"])
        S.op("dve", lambda en: en.memset(lhalo[:], 0.0), [], ["lhalo"])
        S.op("dve", lambda en: en.memset(hstate[:], 0.0), [], ["hstate"])
        S.op("dve", lambda en: en.memset(fhalo[:], 0.0), [], ["fhalo"])
        tt(biasn[:].rearrange("p a h t -> p (a h t)"), bn_t, b31_t, ALU.subtract, bn_k + b31_k, ["biasn"])
        act(sm[:, 0:8], cv(C_LAM, 8), AF.Exp, ["cvec"], ["sm0"], scale=-1.0)
        act(sm[:, 8:16], sm[:, 0:8], AF.Ln, ["sm0"], ["sm1"], bias=cv(C_ONE))
        ts(s8h[:], sm[:, 8:16], -4.0, None, ALU.mult, None, ["sm1"], ["s8"])
        ts(s16h[:], sm[:, 8:16], -8.0, None, ALU.mult, None, ["sm1"], ["s16"])
        ts(hba[:], cv(C_LBA, 8), 0.5, None, ALU.mult, None, ["cvec"], ["hb"])
        ts(hbx[:], cv(C_LBX, 8), 0.5, None, ALU.mult, None, ["cvec"], ["hb"])
        ts(gqs[:], cv(C_GQ), float(128 ** -0.5), None, ALU.mult, None, ["cvec"], ["gqs"])
        S.op("dve", lambda en: en.memset(wv[:], 0.0), [], ["wv"])
        S.op("dve", lambda en: en.memset(wv[:, 1:3], 1.0), ["wv"], ["wv"])
        ts(wv[:, 0:1], cv(C_CF), 1.0, None, ALU.mult, None, ["cvec", "wv"], ["wv"])
        ts(wv[:, 3:4], cv(C_CF), -0.5, None, ALU.mult, None, ["cvec", "wv"], ["wv"])
        ts(wv[:, 4:5], cv(C_CF), -0.5, None, ALU.mult, None, ["cvec", "wv"], ["wv"])
        ts(wv[:, 5:6], cv(C_CF), -0.5, None, ALU.mult, None, ["cvec", "wv"], ["wv"])

        def plan_block(full):
            pl = ["lrux0", "lrux1", "kk", "vw"]
            if not full:
                return pl
            pl += ["q0", "q1", "qi", "lg0", "lg1", "lbr0", "lbr1", "ga0", "ga1", "abr0", "abr1", "gb0", "gb1", "wo0", "wo1"]
            for fg in range(3):
                pl += ["upg%da" % fg, "upg%db" % fg, "upv%da" % fg, "upv%db" % fg, "dn%da" % fg, "dn%db" % fg]
            pl += ["pp", "pg0", "pg1"]
            return pl

        plan = []
        for b in range(NBLK):
            plan += plan_block(b >= FIRST_FULL)
        W = {"issued": 0, "consumed": 0, "hold": None}

        def slotview(i):
            scr, k, n = units[plan[i]]
            s = i % NSLOT
            return wsl[:, s, 0:k * n].rearrange("p (k n) -> p k n", k=k), ("ws", s)

        def wget(name):
            n = W["consumed"]
            assert plan[n] == name, (plan[n], name)
            retired = n if W["hold"] is None else min(n, W["hold"])
            while W["issued"] < min(len(plan), retired + NSLOT):
                i = W["issued"]
                v, key = slotview(i)
                scr, k_, n_ = units[plan[i]]
                S.dma("sp", wsl[:, i % NSLOT, 0:k_ * n_], scr, reads=[("u", plan[i])], writes=[key])
                W["issued"] += 1
            assert W["issued"] > n, name
            W["consumed"] += 1
            return slotview(n)

        def norm_transpose(j, gcol):
            src = xres[:, j, :]
            act(junk2[:, 0:D], src, AF.Square, [("xres", j)], ["junk2", "ssq"], accum=sm[:, 16:17])
            act(sm[:, 17:18], sm[:, 16:17], AF.Sqrt, ["ssq"], ["rstd0"], bias=cv(C_EPS), scale=1.0 / D)
            S.op("dve", lambda en: en.reciprocal(out=sm[:, 18:19], in_=sm[:, 17:18]), ["rstd0"], ["rstd"])
            ts(xn_tm[:], src, sm[:, 18:19], None, ALU.mult, None, [("xres", j), "rstd"], ["xn_tm"])
            for kc in range(8):
                tr(pT_ps[:, kc * 128:(kc + 1) * 128], xn_tm[:, kc * 128:(kc + 1) * 128], ["xn_tm"], [("ps", "T")])
            tt(xnT[:, :, j * 128:(j + 1) * 128], pT_ps[:, 0:1024].rearrange("p (k t) -> p k t", k=8),
               cv(gcol, 8).unsqueeze(2).to_broadcast([128, 8, 128]), ALU.mult, [("ps", "T"), "cvec"], [("xnT", j)])

        XNK = [("xnT", j) for j in range(TB)]

        def proj_fm(wname, ncols, evac, m=128, rhsT=None, rkeys=None, rot4=True):
            wv, wkey = wget(wname)
            rhsT = xnT if rhsT is None else rhsT
            rkeys = XNK if rkeys is None else rkeys
            for ci in range(ncols // m):
                bank, bkey = nextbank(rot4)
                for kc in range(8):
                    mm(bank[0:m, 0:T], wv[:, kc, ci * m:(ci + 1) * m], rhsT[:, kc, :], kc == 0, kc == 7,
                       [wkey] + rkeys, [bkey])
                evac(ci, bank, bkey)

        def proj_tasks(wname, ncols, evac, m=128):
            st8 = {}

            def mk(ci):
                def f():
                    if ci == 0:
                        st8["w"] = wget(wname)
                    wv, wkey = st8["w"]
                    bank, bkey = nextbank()
                    for kc in range(8):
                        mm(bank[0:m, 0:T], wv[:, kc, ci * m:(ci + 1) * m], xnT[:, kc, :], kc == 0, kc == 7, [wkey] + XNK, [bkey])
                    evac(ci, bank, bkey)
                return f
            return [mk(ci) for ci in range(ncols // m)]

        def headnorm(bank, bkey, gcol_ap, gkey, out_ap, okeys):
            act(qsq[:], bank[:, 0:T], AF.Square, [bkey], ["qsq"])
            mm(pL1[:, 0:T], ones_b[:], qsq[:], True, True, ["ones", "qsq"], [("ps", "L1")])
            sd0, sd0k = arena(22 * T, T)
            sd1, sd1k = arena(23 * T, T)
            act(sd0, pL1[:, 0:T], AF.Sqrt, [("ps", "L1"), "cvec"], sd0k, bias=cv(C_EPS), scale=1.0 / 128)
            S.op("dve", lambda en: en.reciprocal(out=sd1, in_=sd0), sd0k, sd1k)
            stt(out_ap, bank[:, 0:T], gcol_ap, sd1, ALU.mult, ALU.mult, [bkey, gkey] + sd1k, okeys)

        def block(b, full):
            t0 = b * T
            tiles = [b * TB + j for j in range(TB)]
            S.phase = "P1.norm"
            for j, g in enumerate(tiles):
                S.dma("sp", xres[:, j, :], xbuf[g * 128:(g + 1) * 128, :], writes=[("xres", j)])
            for j in range(TB):
                norm_transpose(j, C_MIXG)
            S.phase = "P1.lru"
            cp(lrux[:, :, 0:3], lhalo[:], ["lhalo"], LXK, e="pool")

            def ev_lrux(base):
                def f(ci, bank, bkey):
                    cp(lrux[:, base + ci, 3:3 + T], bank[:, 0:T], [bkey], LXK)
                return f
            proj_fm("lrux0", 512, ev_lrux(0), rot4=False)
            proj_fm("lrux1", 512, ev_lrux(4), rot4=False)
            S.dma("sp", waxs[:].rearrange("p n d -> p (n d)"), units["wax"][0], reads=[("u", "wax")], writes=["waxs"])
            wax, waxk = waxs, "waxs"
            side = []

            def ev_kk(ci, bank, bkey):
                if ci < 2:
                    headnorm(bank, bkey, cv(C_GK), "cvec", kT[:, ci, t0:t0 + T], [("kT", g) for g in tiles])
                else:
                    cp(kiT[:, t0:t0 + T], bank[:, 0:T], [bkey], [("ki", g) for g in tiles])
            side += proj_tasks("kk", 384, ev_kk)
            vst = {}

            def vw_task(j, g):
                def f():
                    if j == 0:
                        vst["w"] = wget("vw")
                    vw, vwk = vst["w"]
                    bank, bkey = nextbank()
                    for kc in range(8):
                        mm(bank[:, 0:264], xnT[:, kc, j * 128:(j + 1) * 128], vw[:, kc, :], kc == 0, kc == 7, [vwk, ("xnT", j)], [bkey])
                    cp(Vt[:, g, :, 0:128], bank[:, 0:256].rearrange("p (a d) -> p a d", a=2), [bkey], [("V", g)])
                    if full:
                        ts(weff[:, j, :], bank[:, 256:264], float(8 ** -0.5 * 64 ** -0.5), None, ALU.mult, None, [bkey], [("weff", j)])
                return f
            side += [vw_task(j, g) for j, g in enumerate(tiles)]
            if full:
                def ev_q(base):
                    def f(ci, bank, bkey):
                        headnorm(bank, bkey, gqs[:, 0:1], "gqs", qT[:, base + ci, :], [("qT", base + ci)])
                    return f
                side += proj_tasks("q0", 512, ev_q(0))
                side += proj_tasks("q1", 512, ev_q(4))

                def ev_qi(ci, bank, bkey):
                    cp(qiT[:, ci, :], bank[:, 0:T], [bkey], ["qiT"])
                side += proj_tasks("qi", 512, ev_qi)
            per = (len(side) + 3) // 4
            if b == FIRST_OWN:
                ts(hstate[:], hstate[:], cv(C_CF), None, ALU.mult, None, ["hstate", "cvec"], ["hstate"])
            def lru_bufs(c):
                pb = c % 2
                base = pb * 7 * T
                names = ["ua", "r", "ig", "a", "a2", "m", "u"]
                d = {n: arena(base + i * T, T) for i, n in enumerate(names)}
                d["h"] = arena(HOFF + c * T, T)
                return pb, d

            for c0 in range(0, 8, 2):
                pair = (c0, c0 + 1)
                for c in pair:
                    pb, d = lru_bufs(c)
                    ua, uak = d["ua"]
                    ts(ua, lrux[:, c, 0:T], cv(C_LCW + c * 4), cv(C_LCB + c), ALU.mult, ALU.add, LXK + ["cvec"], uak)
                    for jj in range(1, 4):
                        stt(ua, lrux[:, c, jj:jj + T], cv(C_LCW + c * 4 + jj), ua, ALU.mult, ALU.add, LXK + ["cvec"] + uak, uak)
                    cp(uab[:, pb, :], ua, uak, [("uab", pb)])
                for c in pair:
                    pb, d = lru_bufs(c)
                    (r_, rk), (ig, igk) = d["r"], d["ig"]
                    lb, lk = (pL0, ("ps", "L0")) if pb == 0 else (pL1, ("ps", "L1"))
                    mm(lb[:, 0:T], wax[:, c, 0:128], uab[:, pb, :], True, True, [waxk, ("uab", pb)], [lk])
                    act(r_, lb[:, 0:T], AF.Tanh, [lk, "hb"], rk, bias=hba[:, c:c + 1], scale=0.5)
                    mm(lb[:, 0:T], wax[:, c, 128:256], uab[:, pb, :], True, True, [waxk, ("uab", pb)], [lk])
                    act(ig, lb[:, 0:T], AF.Tanh, [lk, "hb"], igk, bias=hbx[:, c:c + 1], scale=0.5)
                for c in pair:
                    pb, d = lru_bufs(c)
                    (r_, rk), (a_, ak), (a2, a2k) = d["r"], d["a"], d["a2"]
                    act(a_, r_, AF.Exp, rk + ["s8"], ak, bias=s8h[:, c:c + 1], scale=s8h[:, c:c + 1])
                    act(a2, r_, AF.Exp, rk + ["s16"], a2k, bias=s16h[:, c:c + 1], scale=s16h[:, c:c + 1])
                for c in pair:
                    pb, d = lru_bufs(c)
                    (a2, a2k), (m_, mk) = d["a2"], d["m"]
                    act(m_, a2, AF.Sqrt, a2k + ["cvec"], mk, bias=cv(C_ONE), scale=-1.0)
                for c in pair:
                    pb, d = lru_bufs(c)
                    (ua, uak), (ig, igk), (m_, mk), (u_, uk), (a_, ak), (hc, hck) = d["ua"], d["ig"], d["m"], d["u"], d["a"], d["h"]
                    stt(u_, ig, 1.0, ua, ALU.add, ALU.mult, igk + uak, uk)
                    stt(u_, u_, 0.5, m_, ALU.mult, ALU.mult, uk + mk, uk)
                    S.op("dve", lambda en: en.tensor_tensor_scan(out=hc, data0=a_, data1=u_, initial=hstate[:, c:c + 1],
                                                                 op0=ALU.mult, op1=ALU.add), ak + uk + ["hstate"], hck)
                    cp(hstate[:, c:c + 1], hc[:, T - 1:T], hck, ["hstate"], e="dve")
                S.phase = "P1.side"
                for _ in range(per):
                    if side:
                        side.pop(0)()
                S.phase = "P1.lru"
            while side:
                side.pop(0)()
            cp(lhalo[:], lrux[:, :, T:T + 3], LXK, ["lhalo"], e="pool")

            if not full:
                return

            S.phase = "P1.q"
            def ev_lg(base):
                def f(ci, bank, bkey):
                    c = base + ci
                    hc, hck = arena(HOFF + c * T, T)
                    gt = tmpf[:, ci % 2, 0:T]
                    act(gt, bank[:, 0:T], AF.Gelu_apprx_tanh, [bkey], [("tmpf", ci % 2)])
                    tt(hgT[:, c, :], gt, hc, ALU.mult, [("tmpf", ci % 2)] + hck, [("hgT", c)])
                return f
            proj_fm("lg0", 512, ev_lg(0))
            proj_fm("lg1", 512, ev_lg(4))

            for j, g in enumerate(tiles):
                attention(j, g)

            S.phase = "M.merge"
            ya_t = lambda m: arena(m * T, T)
            yb_t = lambda m: arena(8 * T + m * T, T)

            def ev_ya(base):
                def f(ci, bank, bkey):
                    v, k = ya_t(base + ci)
                    act(v, bank[:, 0:T], AF.Copy, [bkey], k, scale=0.5)
                return f
            HGK = [("hgT", c) for c in range(8)]
            proj_fm("lbr0", 512, ev_ya(0), rhsT=hgT, rkeys=HGK)
            proj_fm("lbr1", 512, ev_ya(4), rhsT=hgT, rkeys=HGK)

            def ev_ga(base):
                def f(ci, bank, bkey):
                    v, k = ya_t(base + ci)
                    sgt = tmpf[:, ci % 2, 0:T]
                    act(sgt, bank[:, 0:T], AF.Tanh, [bkey], [("tmpf", ci % 2)], scale=0.5)
                    stt(v, sgt, 1.0, v, ALU.add, ALU.mult, k + [("tmpf", ci % 2)], k)
                return f
            proj_fm("ga0", 512, ev_ga(0))
            proj_fm("ga1", 512, ev_ga(4))

            def ev_yb(base):
                def f(ci, bank, bkey):
                    v, k = yb_t(base + ci)
                    act(v, bank[:, 0:T], AF.Copy, [bkey], k, scale=0.5)
                return f
            ATK = [("attnT", jj) for jj in range(TB)]
            proj_fm("abr0", 512, ev_yb(0), rhsT=attnT, rkeys=ATK)
            proj_fm("abr1", 512, ev_yb(4), rhsT=attnT, rkeys=ATK)

            def ev_gb(base):
                def f(ci, bank, bkey):
                    m = base + ci
                    va, ka = ya_t(m)
                    vb, kb = yb_t(m)
                    sgt = tmpf[:, ci % 2, 0:T]
                    act(sgt, bank[:, 0:T], AF.Tanh, [bkey], [("tmpf", ci % 2)], scale=0.5)
                    stt(vb, sgt, 1.0, vb, ALU.add, ALU.mult, kb + [("tmpf", ci % 2)], kb)
                    tt(mergedT[:, m, :], vb, va, ALU.add, ka + kb, [("qT", m)])
                return f
            proj_fm("gb0", 512, ev_gb(0))
            proj_fm("gb1", 512, ev_gb(4))

            def tm_accum(wname, lhs, lkeys):
                wv, wkey = wget(wname)
                n = int(wname[-1] in "1b")
                for j in range(TB):
                    bank, bkey = nextbank(True)
                    for kc in range(8):
                        mm(bank[:, 0:512], lhs[:, kc, j * 128:(j + 1) * 128], wv[:, kc, :], kc == 0, kc == 7, [wkey] + lkeys, [bkey])
                    tt(xres[:, j, n * 512:(n + 1) * 512], xres[:, j, n * 512:(n + 1) * 512], bank[:, 0:512], ALU.add,
                       [bkey, ("xres", j)], [("xres", j)])
            MK = [("qT", m) for m in range(8)]
            tm_accum("wo0", mergedT, MK)
            tm_accum("wo1", mergedT, MK)

            S.phase = "F.ffn"
            for j in range(TB):
                norm_transpose(j, C_FFNG)
            for fg in range(3):
                def ev_gate(base):
                    def f(ci, bank, bkey):
                        c = base + ci
                        fc = fg * 8 + c
                        pb = c % 2
                        if b == FIRST_OWN:
                            ts(gbuf[:, pb, 0:2], fhalo[:, fc, :], cv(C_CF), None, ALU.mult, None, ["fhalo", "cvec"], [("gbuf", pb)], e="pool")
                        else:
                            cp(gbuf[:, pb, 0:2], fhalo[:, fc, :], ["fhalo"], [("gbuf", pb)], e="pool")
                        cp(gbuf[:, pb, 2:2 + T], bank[:, 0:T], [bkey], [("gbuf", pb)])
                        cp(fhalo[:, fc, :], gbuf[:, pb, T:T + 2], [("gbuf", pb)], ["fhalo"], e="pool")
                        gc = gcb[:, pb, :]
                        ts(gc, gbuf[:, pb, 0:T], cv(C_FCW + fc * 3), cv(C_FCB + fc), ALU.mult, ALU.add, [("gbuf", pb), "cvec"], [("gcb", pb)])
                        for jj in (1, 2):
                            stt(gc, gbuf[:, pb, jj:jj + T], cv(C_FCW + fc * 3 + jj), gc, ALU.mult, ALU.add,
                                [("gbuf", pb), "cvec", ("gcb", pb)], [("gcb", pb)])
                        glv, glk = arena(c * T, T)
                        act(glv, gc, AF.Gelu_apprx_tanh, [("gcb", pb)], glk)
                    return f
                proj_fm("upg%da" % fg, 512, ev_gate(0))
                proj_fm("upg%db" % fg, 512, ev_gate(4))

                def ev_val(base):
                    def f(ci, bank, bkey):
                        c = base + ci
                        glv, glk = arena(c * T, T)
                        tt(actT[:, c, :], glv, bank[:, 0:T], ALU.mult, glk + [bkey], [("hgT", c)])
                    return f
                proj_fm("upv%da" % fg, 512, ev_val(0))
                proj_fm("upv%db" % fg, 512, ev_val(4))
                AK = [("hgT", c) for c in range(8)]
                tm_accum("dn%da" % fg, actT, AK)
                tm_accum("dn%db" % fg, actT, AK)

            S.phase = "E.ple"
            for j in range(TB):
                norm_transpose(j, C_PLEG)
            for j, g in enumerate(tiles):
                S.dma("pool", p_tm[:], pbuf[g * 128:(g + 1) * 128, :], writes=["p_tm"])
                for k2 in range(2):
                    tr(pT_ps[:, k2 * 128:(k2 + 1) * 128], p_tm[:, k2 * 128:(k2 + 1) * 128], ["p_tm"], [("ps", "T")])
                cp(pT[:, :, j * 128:(j + 1) * 128], pT_ps[:, 0:256].rearrange("p (k t) -> p k t", k=2), [("ps", "T")], [("pT", j)])
            pp, ppk = wget("pp")
            W["hold"] = W["consumed"] - 1
            for n in range(2):
                pg, pgk = wget("pg%d" % n)
                for j, g in enumerate(tiles):
                    bank, bkey = nextbank()
                    for kc in range(8):
                        mm(bank[:, 0:512], xnT[:, kc, j * 128:(j + 1) * 128], pg[:, kc, :], kc == 0, kc == 7, [pgk, ("xnT", j)], [bkey])
                    sg = tmpf[:, 0, :]
                    act(sg, bank[:, 0:512], AF.Tanh, [bkey], [("tmpf", 0)], scale=0.5)
                    for k2 in range(2):
                        mm(pL0[:, 0:512], pT[:, k2, j * 128:(j + 1) * 128], pp[:, k2, n * 512:(n + 1) * 512], k2 == 0, k2 == 1,
                           [ppk, ("pT", j)], [("ps", "L0")])
                    stt(sg, sg, 1.0, pL0[:, 0:512], ALU.add, ALU.mult, [("tmpf", 0), ("ps", "L0")], [("tmpf", 0)])
                    stt(xres[:, j, n * 512:(n + 1) * 512], sg, 0.5, xres[:, j, n * 512:(n + 1) * 512], ALU.mult, ALU.add,
                        [("tmpf", 0), ("xres", j)], [("xres", j)])
            W["hold"] = None
            if b >= FIRST_OWN:
                for j, g in enumerate(tiles):
                    S.dma("pool", out[(g - NCT) * 128:(g - NCT + 1) * 128, :], xres[:, j, :], reads=[("xres", j)], writes=[("out", g)])

        HOFF = 14 * T
        assert HOFF + 8 * T <= ARN

        OB = [(pOA, "OA", 0), (pOA, "OA", 1), (pOA, "OA", 2), (pOB, "OB", 0), (pOB, "OB", 1), (pOB, "OB", 2), (pOC, "OC", 0), (pOC, "OC", 1)]

        def attention(j, g):
            L = (g + 1) * 128
            nch = (L + 511) // 512
            js = slice(j * 128, (j + 1) * 128)
            S.phase = "A.idx"
            for ch in range(nch):
                n = min(512, L - ch * 512)
                sc = score[:, ch * 512:ch * 512 + n]
                sk = [("sc", ch)]
                kik = [("ki", kt) for kt in range(ch * 4, ch * 4 + n // 128)]
                for h in range(8):
                    bank, bkey = nextbank_idx()
                    p0 = 64 * (h % 2)
                    mm(bank[:, 0:n], qiT[p0:p0 + 64, h // 2, js], kiT[p0:p0 + 64, ch * 512:ch * 512 + n], True, True,
                       ["qiT"] + kik, [bkey])
                    act(rl[:, h % 2, 0:n], bank[:, 0:n], AF.Relu, [bkey], [("tmpf", h % 2)])
                    if h == 0:
                        ts(sc, rl[:, 0, 0:n], weff[:, j, 0:1], None, ALU.mult, None, [("tmpf", 0), ("weff", j)], sk)
                    else:
                        stt(sc, rl[:, h % 2, 0:n], weff[:, j, h:h + 1], sc, ALU.mult, ALU.add, [("tmpf", h % 2), ("weff", j)] + sk, sk)
            allk = sck(0, L)
            S.phase = "A.bis"
            S.op("dve", lambda en: en.tensor_reduce(out=sm[:, 20:21], in_=score[:, 0:L], axis=AX.X, op=ALU.max), allk, ["rmax"])
            S.op("dve", lambda en: en.tensor_reduce(out=sm[:, 21:22], in_=score[:, 0:L], axis=AX.X, op=ALU.min), allk, ["rmin"])
            tt(sm[:, 22:23], sm[:, 21:22], sm[:, 20:21], ALU.subtract, ["rmin", "rmax"], ["dd"])
            lo = sm[:, 23:24]
            stt(lo, sm[:, 22:23], 1.0 / 64, sm[:, 21:22], ALU.mult, ALU.add, ["dd", "rmin"], ["lo"])
            tt(sm[:, 24:25], sm[:, 20:21], lo, ALU.subtract, ["rmax", "lo"], ["w0"])
            ts(hw[:], cv(C_POW, NIT), sm[:, 24:25], None, ALU.mult, None, ["cvec", "w0"], ["hw"])
            dg = score[:, g * 128:(g + 1) * 128]
            tt(dg, dg, tri[:], ALU.add, [("sc", g // 4), "tri"], [("sc", g // 4)])
            cend = min(L, CTXK)
            nA = min(cend, 3072)
            act_pieces = []
            if nA > 0:
                n0 = min(nA, 2048)
                act_pieces.append((0, n0, junk2[:, 2048:2048 + n0], "junk2b"))
                if nA > 2048:
                    act_pieces.append((2048, nA - 2048, nm[:].rearrange("p a n -> p (a n)")[:, 0:nA - 2048], ("nm", 0)))
            assert len(act_pieces) <= 2 and sum(p[1] for p in act_pieces) == nA
            dve_pieces = []
            if cend > nA:
                assert cend - nA <= 2048
                dve_pieces.append((nA, cend - nA, 0))
            col = 1
            for a in range(CTXK, L, 2048):
                dve_pieces.append((a, min(2048, L - a), col))
                col += 1
            assert col <= 3
            mid, tot, stp, kadj = sm[:, 25:26], sm[:, 27:28], sm[:, 28:29], sm[:, 30:31]
            prod = sm[:, 40:46]
            CK = [("cnt", k) for k in range(6)]
            S.op("dve", lambda en: en.memset(cntb[:, 0:6], 0.0), [], CK)
            ts(kadj, cv(C_CF), -0.5 * nA, float(TOPK), ALU.mult, ALU.add, ["cvec"], ["kadj"])
            ts(mid, lo, hw[:, 0:1], None, ALU.add, None, ["lo", "hw"], ["mid"])
            for it in range(NIT):
                for k, (a, n, dst, dkey) in enumerate(act_pieces):
                    act(dst, score[:, a:a + n], AF.Sign, sck(a, a + n) + ["mid"], [dkey, ("cnt", 3 + k)] + ([("nm", 1)] if k == 1 else []),
                        bias=mid, scale=-1.0, accum=cntb[:, 3 + k:4 + k])
                for (a, n, c) in dve_pieces:
                    ts(junk2[:, 0:n], score[:, a:a + n], mid, None, ALU.is_gt, ALU.add,
                       sck(a, a + n) + ["mid"], ["junk2", ("cnt", c)], accum=cntb[:, c:c + 1])
                tt(prod, cntb[:, 0:6], wv[:, 0:6], ALU.mult, CK + ["wv"], ["prod"])
                S.op("dve", lambda en: en.tensor_reduce(out=tot, in_=prod, axis=AX.X, op=ALU.add), ["prod"], ["tot"])
                stt(stp, tot, kadj, hw[:, it:it + 1], ALU.is_ge, ALU.mult, ["tot", "hw", "kadj"], ["stp"])
                nx = min(it + 1, NIT - 1)
                dst, dk = (mid, "mid") if it < NIT - 1 else (lo, "lo")
                stt(dst, mid, hw[:, nx:nx + 1], stp, ALU.subtract, ALU.add, ["mid", "hw", "stp"], [dk])
            loc = sm[:, 29:30]
            ts(loc, lo, cv(C_CF), cv(C_CF + 1), ALU.mult, ALU.add, ["lo", "cvec"], ["loc"])
            S.phase = "A.att"
            steps = [(kt, kv) for kt in range(g + 1) for kv in range(2)]

            def qk(i):
                kt, kv = steps[i]
                ch, off = kt // 4, (kt % 4) * 128
                if kt % 4 == 0 and kv == 0:
                    n = min(512, L - ch * 512)
                    isctx = ch * 512 < CTXK
                    ts(nm[:, ch % 2, 0:n], score[:, ch * 512:ch * 512 + n], (loc if isctx else lo), -30000.0, ALU.is_le, ALU.mult,
                       [("sc", ch), "lo", "loc"], [("nm", ch % 2)])
                near = kt >= g - 1
                lgb, lgk = (pL0, ("ps", "L0")) if kv == 0 else (pL1, ("ps", "L1"))
                lg3 = lgb[:, 0:512].rearrange("p (a t) -> p a t", a=4)
                mm(lg3, kT[:, kv, kt * 128:(kt + 1) * 128], qT[:, 4 * kv:4 * kv + 4, js], True, False,
                   [("kT", kt)] + [("qT", 4 * kv + a) for a in range(4)], [lgk])
                mm(lg3, nm[:, ch % 2, off:off + 128], ident4[:], False, not near, [("nm", ch % 2), "ident4"], [lgk])
                if near:
                    mm(lg3, ident[:], biasn[:, g - kt, 4 * kv:4 * kv + 4, :], False, True, ["ident", "biasn"], [lgk])

            def pv(i):
                kt, kv = steps[i]
                lgb, lgk = (pL0, ("ps", "L0")) if kv == 0 else (pL1, ("ps", "L1"))
                pk = ("pTb", i % 2)
                pvv = pTb[:, i % 2, :]
                act(pvv, lgb[:, 0:512], AF.Exp, [lgk], [pk])
                for a in range(4):
                    hd = 4 * kv + a
                    ob, obk, sl = OB[hd]
                    mm(ob[:, sl * 129:(sl + 1) * 129], pvv[:, a * 128:(a + 1) * 128], Vt[:, kt, kv, :], (kt == 0 and sl == 0), kt == g,
                       [pk, ("V", kt), "Vones"], [("ps", obk)], skip=True)

            qk(0)
            qk(1)
            for i in range(len(steps)):
                pv(i)
                if i + 2 < len(steps):
                    qk(i + 2)
            S.phase = "A.fin"
            for (ob, obk, nh, h0) in ((pOA, "OA", 3, 0), (pOB, "OB", 3, 3), (pOC, "OC", 2, 6)):
                o3 = ob[:, 0:nh * 129].rearrange("p (h d) -> p h d", d=129)
                ts(den[:, h0:h0 + nh], o3[:, :, 128], 1e-30, None, ALU.max, None, [("ps", obk)], ["den"])
            S.op("dve", lambda en: en.reciprocal(out=rden[:], in_=den[:]), ["den"], ["rden"])
            for (ob, obk, nh, h0) in ((pOA, "OA", 3, 0), (pOB, "OB", 3, 3), (pOC, "OC", 2, 6)):
                o3 = ob[:, 0:nh * 129].rearrange("p (h d) -> p h d", d=129)
                tt(attn_tm[:, h0 * 128:(h0 + nh) * 128].rearrange("p (h d) -> p h d", d=128), o3[:, :, 0:128],
                   rden[:, h0:h0 + nh].unsqueeze(2).to_broadcast([128, nh, 128]), ALU.mult, [("ps", obk), "rden"], ["xn_tm"])
            for hd in range(8):
                tr(pT_ps[:, hd * 128:(hd + 1) * 128], attn_tm[:, hd * 128:(hd + 1) * 128], ["xn_tm"], [("ps", "T")])
            cp(attnT[:, :, js], pT_ps[:, 0:1024].rearrange("p (h t) -> p h t", h=8), [("ps", "T")], [("attnT", j)])

        for b in range(NBLK):
            block(b, b >= FIRST_FULL)
        S.final_wait("sp", [("out", g) for g in range(NCT, NT)])
        S.final_wait("pool", [("out", g) for g in range(NCT, NT)])
    print("ops", S.nops, {e: S.cnt[e] for e in S.cnt})
    return nc


def _bucket(dist):
    d_f = np.maximum(dist, 1).astype(np.float32)
    large = 16 + (np.log(d_f / np.float32(16)) / np.float32(math.log(128 / 16)) * np.float32(16)).astype(np.int32)
    large = np.minimum(large, 31)
    return np.where(dist < 16, dist, large)


def make_inputs(inp, NT, NIT=20):
    f32 = np.float32
    x = np.asarray(inp["x"], f32)
    p = np.asarray(inp["p"], f32)[0]
    Bn, SEQ, _ = x.shape
    half = SEQ // 2
    assert NT * 128 == SEQ

    def col(v, nch):
        return np.ascontiguousarray(np.asarray(v, f32).reshape(nch, 128).T)

    cvec = np.zeros((128, C_POW + NIT), f32)
    cvec[:, C_MIXG:C_MIXG + 8] = col(inp["mix_norm"][0], 8)
    cvec[:, C_FFNG:C_FFNG + 8] = col(inp["ffn_norm"][0], 8)
    cvec[:, C_PLEG:C_PLEG + 8] = col(inp["ple_norm"][0], 8)
    lcw = np.asarray(inp["lru_conv_w"], f32)[0]
    cvec[:, C_LCW:C_LCW + 32] = lcw.reshape(4, 8, 128).transpose(2, 1, 0).reshape(128, 32)
    cvec[:, C_LCB:C_LCB + 8] = col(inp["lru_conv_b"][0], 8)
    cvec[:, C_LBA:C_LBA + 8] = col(inp["lru_ba"][0], 8)
    cvec[:, C_LBX:C_LBX + 8] = col(inp["lru_bx"][0], 8)
    cvec[:, C_LAM:C_LAM + 8] = col(inp["lru_lambda"][0], 8)
    cvec[:, C_GQ] = np.asarray(inp["q_norm"], f32)[0]
    cvec[:, C_GK] = np.asarray(inp["k_norm"], f32)[0]
    fcw = np.asarray(inp["ffn_conv_w"], f32)[0]
    cvec[:, C_FCW:C_FCW + 72] = fcw.reshape(3, 24, 128).transpose(2, 1, 0).reshape(128, 72)
    cvec[:, C_FCB:C_FCB + 24] = col(inp["ffn_conv_b"][0], 24)
    cvec[:, C_EPS] = EPS
    cvec[:, C_ONE] = 1.0
    cvec[:, C_POW:C_POW + NIT] = (0.5 ** np.arange(1, NIT + 1, dtype=np.float64)).astype(f32)[None, :]

    tq = np.arange(128)[:, None]
    sk = np.arange(128)[None, :]
    tri = np.where(sk <= tq, 0.0, -1e30).astype(f32)
    ident = np.eye(128, dtype=f32)
    rb = np.asarray(inp["rel_bias"], f32)
    ss = np.arange(128)[:, None, None]
    oo = np.arange(2)[None, :, None]
    tt_ = np.arange(128)[None, None, :]
    dist = np.maximum(tt_ - ss + 128 * oo, 0).astype(np.int32)
    bidx = _bucket(dist)
    bn = rb[bidx]
    bn = np.ascontiguousarray(bn.transpose(0, 1, 3, 2)).reshape(128, 2 * 8 * 128)
    b31 = np.ascontiguousarray(rb[np.full_like(bidx, 31)].transpose(0, 1, 3, 2)).reshape(128, 2 * 8 * 128)

    shared = {
        "w_in": np.ascontiguousarray(inp["w_in"][0], f32), "w_lru_br": np.ascontiguousarray(inp["w_lru_br"][0], f32),
        "w_attn_br": np.ascontiguousarray(inp["w_attn_br"][0], f32), "w_out": np.ascontiguousarray(inp["w_out"][0], f32),
        "w_up": np.ascontiguousarray(inp["w_up"][0], f32), "w_down": np.ascontiguousarray(inp["w_down"][0], f32),
        "w_ple_gate": np.ascontiguousarray(inp["w_ple_gate"][0], f32), "w_ple_proj": np.ascontiguousarray(inp["w_ple_proj"][0], f32),
        "lru_wa": np.ascontiguousarray(inp["lru_wa"][0], f32), "lru_wx": np.ascontiguousarray(inp["lru_wx"][0], f32),
        "tri": tri, "ident": ident, "bias_near": bn, "bias_far": b31,
    }
    maps = []
    for c in range(2 * Bn):
        b, h = c // 2, c % 2
        cv_c = cvec.copy()
        if h == 1:
            xb, pb = x[b], p[b]
            cv_c[:, C_CF], cv_c[:, C_CF + 1] = 1.0, 0.0
        else:
            xb = np.concatenate([np.zeros((half, D), f32), x[b, :half]], axis=0)
            pb = np.concatenate([np.zeros((half, 256), f32), p[b, :half]], axis=0)
            cv_c[:, C_CF], cv_c[:, C_CF + 1] = 0.0, 1e30
        m = dict(shared)
        m["xbuf"] = np.ascontiguousarray(xb)
        m["pbuf"] = np.ascontiguousarray(pb)
        m["cvec"] = cv_c
        maps.append(m)
    return maps


def run(inp, NT, NIT=20):
    x = np.asarray(inp["x"])
    Bn, SEQ, _ = x.shape
    half = SEQ // 2
    nc = build(NT, NIT=NIT)
    maps = make_inputs(inp, NT, NIT=NIT)
    res = run_bass_kernel_spmd(nc, maps, core_ids=list(range(2 * Bn)))
    outp = np.empty((Bn, SEQ, D), np.float32)
    for c in range(2 * Bn):
        b, h = c // 2, c % 2
        outp[b, h * half:(h + 1) * half] = res.results[c]["out"]
    return outp


def kernel(**inputs):
    return run(inputs, 64, NIT=20)
```

```python
import math
from contextlib import ExitStack

import numpy as np
import concourse.bass as bass
import concourse.mybir as mybir
from concourse.bass_utils import run_bass_kernel_spmd

F32 = mybir.dt.float32
BF16 = mybir.dt.bfloat16
AF = mybir.ActivationFunctionType
ALU = mybir.AluOpType
AX = mybir.AxisListType

D = 1024
TB = 2
T = TB * 128
NSLOT = 4
EPS = 1e-6

C_MIXG, C_FFNG, C_PLEG = 0, 8, 16
C_LCW, C_LCB, C_LBA, C_LBX, C_LAM = 24, 56, 64, 72, 80
C_GQ, C_GK = 88, 89
C_FCW, C_FCB = 90, 162
C_CF = 186
C_EPS, C_ONE = 188, 189
C_POW = 190


class Sched:
    def __init__(self, nc, ndma=24):
        self.nc = nc
        self.eng = {"pe": nc.tensor, "act": nc.scalar, "dve": nc.vector, "pool": nc.gpsimd, "sp": nc.sync}
        self.sems = {}
        self.cnt = {}
        self.waited = {e: {} for e in self.eng}
        self.bufs = {}
        self.ndma = ndma
        self.dma_i = 0
        self.pool_i = 0
        self.nops = 0
        self.phase = None

    def setup(self, stack):
        for e in self.eng:
            self.sems[e] = stack.enter_context(self.nc.semaphore("s_" + e))
            self.cnt[e] = 0
        self.dsems = [stack.enter_context(self.nc.semaphore("d%d" % i)) for i in range(self.ndma)]
        self.dcnt = [0] * self.ndma
        self.semobj = dict(self.sems)
        for i, s in enumerate(self.dsems):
            self.semobj["d%d" % i] = s

    def _deps(self, reads, writes):
        deps = {}

        def add(k, v):
            if deps.get(k, 0) < v:
                deps[k] = v
        for b in reads:
            st = self.bufs.get(b)
            if st and st["w"]:
                add(*st["w"])
        for b in writes:
            st = self.bufs.get(b)
            if st:
                if st["w"]:
                    add(*st["w"])
                for k, v in st["r"].items():
                    add(k, v)
        return deps

    def _wait(self, e, deps, keep_one=False):
        eng = self.eng[e]
        need = [(k, v) for k, v in deps.items() if self.waited[e].get(k, 0) < v]
        inline = None
        if keep_one and need:
            inline = need.pop()
        for k, v in need:
            eng.wait_ge(self.semobj[k], v)
            self.waited[e][k] = v
        if inline is not None:
            self.waited[e][inline[0]] = inline[1]
            return (self.semobj[inline[0]], inline[1])
        return None

    def _update(self, ev, reads, writes):
        for b in reads:
            st = self.bufs.setdefault(b, {"w": None, "r": {}})
            if st["r"].get(ev[0], 0) < ev[1]:
                st["r"][ev[0]] = ev[1]
        for b in writes:
            self.bufs[b] = {"w": ev, "r": {}}

    def op(self, e, fn, reads=(), writes=()):
        deps = self._deps(reads, writes)
        if e == "pe":
            deps.pop("pe", None)
        inl = self._wait(e, deps, keep_one=True)
        ins = fn(self.eng[e])
        if inl is not None:
            ins._wait_ge(inl[0], inl[1])
        if self.phase is not None:
            ins.annotate(self.phase)
        self.cnt[e] += 1
        ins.then_inc(self.sems[e], 1)
        self._update((e, self.cnt[e]), reads, writes)
        self.nops += 1
        return ins

    def dma(self, e, out, in_, reads=(), writes=()):
        if e == "pool":
            i = self.pool_i % 3
            self.pool_i += 1
        else:
            i = 3 + self.dma_i % (self.ndma - 3)
            self.dma_i += 1
        key = "d%d" % i
        deps = self._deps(reads, writes)
        if self.dcnt[i] > 0:
            deps[key] = max(deps.get(key, 0), self.dcnt[i])
        self._wait(e, deps)
        ins = self.eng[e].dma_start(out=out, in_=in_)
        self.dcnt[i] += 16
        ins.then_inc(self.dsems[i], 16)
        self._update((key, self.dcnt[i]), reads, writes)
        self.nops += 1
        return ins

    def final_wait(self, e, bufs):
        self._wait(e, self._deps(bufs, bufs))


def build(NT, NIT=20, TOPK=256):
    NCT = NT // 2
    NTOK = NT * 128
    NBLK = NT // TB
    FIRST_OWN = NCT // TB
    FIRST_FULL = FIRST_OWN - 1
    CTXK = NCT * 128
    NOWN = (NT - NCT) * 128

    nc = bass.Bass("TRN2", target_bir_lowering=False)

    def din(name, shape, dt=F32):
        return nc.dram_tensor(name, shape, dt, kind="ExternalInput").ap()

    def dscr(name, shape, dt=BF16):
        return nc.dram_tensor(name, shape, dt, kind="Internal").ap()

    xbuf = din("xbuf", [NTOK, D])
    pbuf = din("pbuf", [NTOK, 256])
    w_in = din("w_in", [D, 6216])
    w_lbr = din("w_lru_br", [D, D])
    w_abr = din("w_attn_br", [D, D])
    w_out = din("w_out", [D, D])
    w_up = din("w_up", [D, 6144])
    w_down = din("w_down", [3072, D])
    w_pg = din("w_ple_gate", [D, D])
    w_pp = din("w_ple_proj", [256, D])
    lru_wa = din("lru_wa", [8, 128, 128])
    lru_wx = din("lru_wx", [8, 128, 128])
    cvec_d = din("cvec", [128, C_POW + NIT])
    tri_d = din("tri", [128, 128])
    ident_d = din("ident", [128, 128])
    bn_d = din("bias_near", [128, 2 * 8 * 128])
    b31_d = din("bias_far", [128, 2 * 8 * 128])
    out = nc.dram_tensor("out", [NOWN, D], F32, kind="ExternalOutput").ap()

    S = Sched(nc)
    with ExitStack() as st:
        S.setup(st)

        def sb(name, shape, dt=F32):
            return st.enter_context(nc.sbuf_tensor(name, shape, dt))

        kT = sb("kT", [128, 2, NTOK], BF16)
        Vt = sb("Vt", [128, NT, 2, 129], BF16)
        kiT = sb("kiT", [128, NTOK], BF16)
        wsl = sb("wsl", [128, NSLOT, 4096], BF16)
        lhalo = sb("lhalo", [128, 8, 3])
        cvec = sb("cvec_s", [128, C_POW + NIT])
        tri = sb("tri_s", [128, 128])
        ident_f = sb("ident_f", [128, 128])
        ident = sb("ident_b", [128, 128], BF16)
        ident4 = sb("ident4", [128, 4, 128], BF16)
        ones_b = sb("ones_b", [128, 128], BF16)
        biasn = sb("biasn", [128, 2, 8, 128], BF16)
        s8h = sb("s8h", [128, 8])
        s16h = sb("s16h", [128, 8])
        hba = sb("hba", [128, 8])
        hbx = sb("hbx", [128, 8])
        gqs = sb("gqs", [128, 1])
        hstate = sb("hstate", [128, 8])
        fhalo = sb("fhalo", [128, 24, 2])
        xres = sb("xres", [128, TB, D])
        xnT = sb("xnT", [128, 8, T], BF16)
        xn_tm = sb("xn_tm", [128, D], BF16)
        qT = sb("qT", [128, 8, T], BF16)
        qiT = sb("qiT", [128, 4, T], BF16)
        weff = sb("weff", [128, TB, 8])
        hgT = sb("hgT", [128, 8, T], BF16)
        attnT = sb("attnT", [128, 8, T], BF16)
        attn_tm = xn_tm
        mergedT = qT
        actT = hgT
        pT = sb("pT", [128, 2, T], BF16)
        p_tm = sb("p_tm", [128, 256], BF16)
        sm = sb("sm", [128, 64])
        hw = sb("hw", [128, NIT])
        cntb = sb("cntb", [128, 8])
        wv = sb("wv", [128, 8])
        den = sb("den", [128, 8])
        rden = sb("rden", [128, 8])
        uab = sb("uab", [128, 2, T], BF16)
        qsq = sb("qsq", [128, T], BF16)
        tmpf = sb("tmpf", [128, 2, 512])
        gbuf = sb("gbuf", [128, 2, T + 2])
        gcb = sb("gcb", [128, 2, T])
        rl = tmpf
        nm = sb("nm", [128, 2, 512], BF16)
        pTb = sb("pTb", [128, 2, 512], BF16)
        waxs = sb("waxs", [128, 8, 256], BF16)
        junk2 = sb("junk2", [128, 4096], BF16)
        ARN = max(NTOK, 32 * T + 24)
        score = sb("score", [128, ARN])
        LOFF = 24 * T
        lrux = score[:, LOFF:LOFF + 8 * (T + 3)].rearrange("p (c t) -> p c t", c=8)
        LXK = [("sc", c) for c in range(LOFF // 512, (LOFF + 8 * (T + 3) - 1) // 512 + 1)]
        pA = st.enter_context(nc.psum_tensor("pA", [128, 512], F32))
        pB = st.enter_context(nc.psum_tensor("pB", [128, 512], F32))
        pL0 = st.enter_context(nc.psum_tensor("pL0", [128, 512], F32))
        pL1 = st.enter_context(nc.psum_tensor("pL1", [128, 512], F32))
        pOA = st.enter_context(nc.psum_tensor("pOA", [128, 512], F32))
        pOB = st.enter_context(nc.psum_tensor("pOB", [128, 512], F32))
        pOC = st.enter_context(nc.psum_tensor("pOC", [128, 512], F32))
        pT_ps = st.enter_context(nc.psum_tensor("pTp", [128, 1024], BF16))
        st.enter_context(nc.Block())

        banks = {"A": pA, "B": pB, "OA": pOA, "OB": pOB, "OC": pOC, "L0": pL0, "L1": pL1}
        rot = {"n": 0, "i": 0}
        DENSE_ROT = ["A", "B"]

        ROT4 = ["A", "B", "L0", "L1"]

        def nextbank(rot4=False):
            r = ROT4 if rot4 else DENSE_ROT
            k = r[rot["n"] % len(r)]
            rot["n"] += 1
            return banks[k], ("ps", k)

        def nextbank_idx():
            k = "AB"[rot["i"] % 2]
            rot["i"] += 1
            return banks[k], ("ps", k)

        def cv(c0, n=1):
            return cvec[:, c0:c0 + n]

        def sck(a, b):
            return [("sc", c) for c in range(a // 512, (b - 1) // 512 + 1)]

        def arena(a, n):
            return score[:, a:a + n], sck(a, a + n)

        def act(out, in_, func, reads, writes, bias=None, scale=None, accum=None, e="act"):
            kw = {}
            if bias is not None:
                kw["bias"] = bias
            if scale is not None:
                kw["scale"] = scale
            if accum is not None:
                kw["accum_out"] = accum
            return S.op(e, lambda en: en.activation(out=out, in_=in_, func=func, **kw), reads, writes)

        def ts(out, in0, s1, s2, op0, op1, reads, writes, accum=None, e="dve"):
            kw = {}
            if accum is not None:
                kw["accum_out"] = accum
            if op1 is None:
                return S.op(e, lambda en: en.tensor_scalar(out=out, in0=in0, scalar1=s1, scalar2=s2, op0=op0, **kw), reads, writes)
            return S.op(e, lambda en: en.tensor_scalar(out=out, in0=in0, scalar1=s1, scalar2=s2, op0=op0, op1=op1, **kw), reads, writes)

        def stt(out, in0, scalar, in1, op0, op1, reads, writes, e="dve"):
            return S.op(e, lambda en: en.scalar_tensor_tensor(out=out, in0=in0, scalar=scalar, in1=in1, op0=op0, op1=op1), reads, writes)

        def tt(out, in0, in1, op, reads, writes, e="dve"):
            return S.op(e, lambda en: en.tensor_tensor(out=out, in0=in0, in1=in1, op=op), reads, writes)

        def cp(out, in_, reads, writes, e="act"):
            if e == "act":
                return S.op(e, lambda en: en.activation(out=out, in_=in_, func=AF.Copy), reads, writes)
            return S.op(e, lambda en: en.tensor_copy(out=out, in_=in_), reads, writes)

        def mm(out, lhsT, rhs, start, stop, reads, writes, skip=False):
            if skip:
                return S.op("pe", lambda en: en.matmul(out, lhsT=lhsT, rhs=rhs, start=start, stop=stop, skip_group_check=True), reads, writes)
            return S.op("pe", lambda en: en.matmul(out, lhsT=lhsT, rhs=rhs, start=start, stop=stop), reads, writes)

        def tr(out, in_, reads, writes):
            return S.op("pe", lambda en: en.transpose(out, in_, ident[:]), reads + ["ident"], writes)

        S.dma("sp", cvec[:], cvec_d, writes=["cvec"])
        S.dma("sp", tri[:], tri_d, writes=["tri"])
        S.dma("sp", ident_f[:], ident_d, writes=["identf"])
        bn_t, bn_k = arena(0, 2048)
        b31_t, b31_k = arena(2048, 2048)
        S.dma("sp", bn_t, bn_d, writes=bn_k)
        S.dma("sp", b31_t, b31_d, writes=b31_k)

        units = {}
        unit_order = []

        def unit(name, kc, ncols, parts):
            scr = nc.dram_tensor("u_" + name, [128, kc * ncols], BF16, kind="Internal").ap()
            units[name] = (scr, kc, ncols)
            unit_order.append(name)
            d3 = scr.rearrange("p (k n) -> p k n", k=kc)
            for (dc, src3) in parts:
                w = src3.shape[2]
                S.dma("pool", d3[:, :, dc:dc + w], src3, writes=[("u", name)])

        def kp(ap, r0, nr, c0, ncol):
            return ap[r0:r0 + nr, c0:c0 + ncol].rearrange("(k p) n -> p k n", p=128)

        unit("lrux0", 8, 512, [(0, kp(w_in, 0, D, 0, 512))])
        unit("lrux1", 8, 512, [(0, kp(w_in, 0, D, 512, 512))])
        unit("wax", 8, 256, [(0, lru_wa.rearrange("n c d -> c n d")), (128, lru_wx.rearrange("n c d -> c n d"))])
        unit("kk", 8, 384, [(0, kp(w_in, 0, D, 3072, 256)), (256, kp(w_in, 0, D, 4096, 64)), (320, kp(w_in, 0, D, 4096, 64))])
        unit("vw", 8, 264, [(0, kp(w_in, 0, D, 3328, 256)), (256, kp(w_in, 0, D, 4160, 8))])
        unit("q0", 8, 512, [(0, kp(w_in, 0, D, 2048, 512))])
        unit("q1", 8, 512, [(0, kp(w_in, 0, D, 2560, 512))])
        unit("qi", 8, 512, [(0, kp(w_in, 0, D, 3584, 512))])
        unit("lg0", 8, 512, [(0, kp(w_in, 0, D, 1024, 512))])
        unit("lg1", 8, 512, [(0, kp(w_in, 0, D, 1536, 512))])
        for hh in range(2):
            unit("lbr%d" % hh, 8, 512, [(0, kp(w_lbr, 0, D, hh * 512, 512))])
        for hh in range(2):
            unit("ga%d" % hh, 8, 512, [(0, kp(w_in, 0, D, 4168 + hh * 512, 512))])
        for hh in range(2):
            unit("abr%d" % hh, 8, 512, [(0, kp(w_abr, 0, D, hh * 512, 512))])
        for hh in range(2):
            unit("gb%d" % hh, 8, 512, [(0, kp(w_in, 0, D, 5192 + hh * 512, 512))])
        for hh in range(2):
            unit("wo%d" % hh, 8, 512, [(0, kp(w_out, 0, D, hh * 512, 512))])
        for fg in range(3):
            for hh, ab in enumerate("ab"):
                unit("upg%d%s" % (fg, ab), 8, 512, [(0, kp(w_up, 0, D, fg * 1024 + hh * 512, 512))])
            for hh, ab in enumerate("ab"):
                unit("upv%d%s" % (fg, ab), 8, 512, [(0, kp(w_up, 0, D, 3072 + fg * 1024 + hh * 512, 512))])
            for hh, ab in enumerate("ab"):
                unit("dn%d%s" % (fg, ab), 8, 512, [(0, kp(w_down, fg * 1024, 1024, hh * 512, 512))])
        unit("pp", 2, 1024, [(0, kp(w_pp, 0, 256, 0, 1024))])
        for hh in range(2):
            unit("pg%d" % hh, 8, 512, [(0, kp(w_pg, 0, D, hh * 512, 512))])

        cp(ident[:], ident_f[:], ["identf"], ["ident"], e="dve")
        for a4 in range(4):
            cp(ident4[:, a4, :], ident_f[:], ["identf"], ["ident4"], e="dve")
        S.op("dve", lambda en: en.memset(ones_b[:], 1.0), [], ["ones"])
        S.op("dve", lambda en: en.memset(Vt[:].rearrange("p n k d -> p (n k) d")[:, :, 128:129], 1.0), [], ["Vones"])
        S.op("dve", lambda en: en.memset(lhalo[:], 0.0), [], ["lhalo"])
        S.op("dve", lambda en: en.memset(hstate[:], 0.0), [], ["hstate"])
        S.op("dve", lambda en: en.memset(fhalo[:], 0.0), [], ["fhalo"])
        tt(biasn[:].rearrange("p a h t -> p (a h t)"), bn_t, b31_t, ALU.subtract, bn_k + b31_k, ["biasn"])
        act(sm[:, 0:8], cv(C_LAM, 8), AF.Exp, ["cvec"], ["sm0"], scale=-1.0)
        act(sm[:, 8:16], sm[:, 0:8], AF.Ln, ["sm0"], ["sm1"], bias=cv(C_ONE))
        ts(s8h[:], sm[:, 8:16], -4.0, None, ALU.mult, None, ["sm1"], ["s8"])
        ts(s16h[:], sm[:, 8:16], -8.0, None, ALU.mult, None, ["sm1"], ["s16"])
        ts(hba[:], cv(C_LBA, 8), 0.5, None, ALU.mult, None, ["cvec"], ["hb"])
        ts(hbx[:], cv(C_LBX, 8), 0.5, None, ALU.mult, None, ["cvec"], ["hb"])
        ts(gqs[:], cv(C_GQ), float(128 ** -0.5), None, ALU.mult, None, ["cvec"], ["gqs"])
        S.op("dve", lambda en: en.memset(wv[:], 0.0), [], ["wv"])
        S.op("dve", lambda en: en.memset(wv[:, 1:3], 1.0), ["wv"], ["wv"])
        ts(wv[:, 0:1], cv(C_CF), 1.0, None, ALU.mult, None, ["cvec", "wv"], ["wv"])
        ts(wv[:, 3:4], cv(C_CF), -0.5, None, ALU.mult, None, ["cvec", "wv"], ["wv"])
        ts(wv[:, 4:5], cv(C_CF), -0.5, None, ALU.mult, None, ["cvec", "wv"], ["wv"])
        ts(wv[:, 5:6], cv(C_CF), -0.5, None, ALU.mult, None, ["cvec", "wv"], ["wv"])

        def plan_block(full):
            pl = ["lrux0", "lrux1", "kk", "vw"]
            if not full:
                return pl
            pl += ["q0", "q1", "qi", "lg0", "lg1", "lbr0", "lbr1", "ga0", "ga1", "abr0", "abr1", "gb0", "gb1", "wo0", "wo1"]
            for fg in range(3):
                pl += ["upg%da" % fg, "upg%db" % fg, "upv%da" % fg, "upv%db" % fg, "dn%da" % fg, "dn%db" % fg]
            pl += ["pp", "pg0", "pg1"]
            return pl

        plan = []
        for b in range(NBLK):
            plan += plan_block(b >= FIRST_FULL)
        W = {"issued": 0, "consumed": 0, "hold": None}

        def slotview(i):
            scr, k, n = units[plan[i]]
            s = i % NSLOT
            return wsl[:, s, 0:k * n].rearrange("p (k n) -> p k n", k=k), ("ws", s)

        def wget(name):
            n = W["consumed"]
            assert plan[n] == name, (plan[n], name)
            retired = n if W["hold"] is None else min(n, W["hold"])
            while W["issued"] < min(len(plan), retired + NSLOT):
                i = W["issued"]
                v, key = slotview(i)
                scr, k_, n_ = units[plan[i]]
                S.dma("sp", wsl[:, i % NSLOT, 0:k_ * n_], scr, reads=[("u", plan[i])], writes=[key])
                W["issued"] += 1
            assert W["issued"] > n, name
            W["consumed"] += 1
            return slotview(n)

        def norm_transpose(j, gcol):
            src = xres[:, j, :]
            act(junk2[:, 0:D], src, AF.Square, [("xres", j)], ["junk2", "ssq"], accum=sm[:, 16:17])
            act(sm[:, 17:18], sm[:, 16:17], AF.Sqrt, ["ssq"], ["rstd0"], bias=cv(C_EPS), scale=1.0 / D)
            S.op("dve", lambda en: en.reciprocal(out=sm[:, 18:19], in_=sm[:, 17:18]), ["rstd0"], ["rstd"])
            ts(xn_tm[:], src, sm[:, 18:19], None, ALU.mult, None, [("xres", j), "rstd"], ["xn_tm"])
            for kc in range(8):
                tr(pT_ps[:, kc * 128:(kc + 1) * 128], xn_tm[:, kc * 128:(kc + 1) * 128], ["xn_tm"], [("ps", "T")])
            tt(xnT[:, :, j * 128:(j + 1) * 128], pT_ps[:, 0:1024].rearrange("p (k t) -> p k t", k=8),
               cv(gcol, 8).unsqueeze(2).to_broadcast([128, 8, 128]), ALU.mult, [("ps", "T"), "cvec"], [("xnT", j)])

        XNK = [("xnT", j) for j in range(TB)]

        def proj_fm(wname, ncols, evac, m=128, rhsT=None, rkeys=None, rot4=True):
            wv, wkey = wget(wname)
            rhsT = xnT if rhsT is None else rhsT
            rkeys = XNK if rkeys is None else rkeys
            for ci in range(ncols // m):
                bank, bkey = nextbank(rot4)
                for kc in range(8):
                    mm(bank[0:m, 0:T], wv[:, kc, ci * m:(ci + 1) * m], rhsT[:, kc, :], kc == 0, kc == 7,
                       [wkey] + rkeys, [bkey])
                evac(ci, bank, bkey)

        def proj_tasks(wname, ncols, evac, m=128):
            st8 = {}

            def mk(ci):
                def f():
                    if ci == 0:
                        st8["w"] = wget(wname)
                    wv, wkey = st8["w"]
                    bank, bkey = nextbank()
                    for kc in range(8):
                        mm(bank[0:m, 0:T], wv[:, kc, ci * m:(ci + 1) * m], xnT[:, kc, :], kc == 0, kc == 7, [wkey] + XNK, [bkey])
                    evac(ci, bank, bkey)
                return f
            return [mk(ci) for ci in range(ncols // m)]

        def headnorm(bank, bkey, gcol_ap, gkey, out_ap, okeys):
            act(qsq[:], bank[:, 0:T], AF.Square, [bkey], ["qsq"])
            mm(pL1[:, 0:T], ones_b[:], qsq[:], True, True, ["ones", "qsq"], [("ps", "L1")])
            sd0, sd0k = arena(22 * T, T)
            sd1, sd1k = arena(23 * T, T)
            act(sd0, pL1[:, 0:T], AF.Sqrt, [("ps", "L1"), "cvec"], sd0k, bias=cv(C_EPS), scale=1.0 / 128)
            S.op("dve", lambda en: en.reciprocal(out=sd1, in_=sd0), sd0k, sd1k)
            stt(out_ap, bank[:, 0:T], gcol_ap, sd1, ALU.mult, ALU.mult, [bkey, gkey] + sd1k, okeys)

        def block(b, full):
            t0 = b * T
            tiles = [b * TB + j for j in range(TB)]
            S.phase = "P1.norm"
            for j, g in enumerate(tiles):
                S.dma("sp", xres[:, j, :], xbuf[g * 128:(g + 1) * 128, :], writes=[("xres", j)])
            for j in range(TB):
                norm_transpose(j, C_MIXG)
            S.phase = "P1.lru"
            cp(lrux[:, :, 0:3], lhalo[:], ["lhalo"], LXK, e="pool")

            def ev_lrux(base):
                def f(ci, bank, bkey):
                    cp(lrux[:, base + ci, 3:3 + T], bank[:, 0:T], [bkey], LXK)
                return f
            proj_fm("lrux0", 512, ev_lrux(0), rot4=False)
            proj_fm("lrux1", 512, ev_lrux(4), rot4=False)
            S.dma("sp", waxs[:].rearrange("p n d -> p (n d)"), units["wax"][0], reads=[("u", "wax")], writes=["waxs"])
            wax, waxk = waxs, "waxs"
            side = []

            def ev_kk(ci, bank, bkey):
                if ci < 2:
                    headnorm(bank, bkey, cv(C_GK), "cvec", kT[:, ci, t0:t0 + T], [("kT", g) for g in tiles])
                else:
                    cp(kiT[:, t0:t0 + T], bank[:, 0:T], [bkey], [("ki", g) for g in tiles])
            side += proj_tasks("kk", 384, ev_kk)
            vst = {}

            def vw_task(j, g):
                def f():
                    if j == 0:
                        vst["w"] = wget("vw")
                    vw, vwk = vst["w"]
                    bank, bkey = nextbank()
                    for kc in range(8):
                        mm(bank[:, 0:264], xnT[:, kc, j * 128:(j + 1) * 128], vw[:, kc, :], kc == 0, kc == 7, [vwk, ("xnT", j)], [bkey])
                    cp(Vt[:, g, :, 0:128], bank[:, 0:256].rearrange("p (a d) -> p a d", a=2), [bkey], [("V", g)])
                    if full:
                        ts(weff[:, j, :], bank[:, 256:264], float(8 ** -0.5 * 64 ** -0.5), None, ALU.mult, None, [bkey], [("weff", j)])
                return f
            side += [vw_task(j, g) for j, g in enumerate(tiles)]
            if full:
                def ev_q(base):
                    def f(ci, bank, bkey):
                        headnorm(bank, bkey, gqs[:, 0:1], "gqs", qT[:, base + ci, :], [("qT", base + ci)])
                    return f
                side += proj_tasks("q0", 512, ev_q(0))
                side += proj_tasks("q1", 512, ev_q(4))

                def ev_qi(ci, bank, bkey):
                    cp(qiT[:, ci, :], bank[:, 0:T], [bkey], ["qiT"])
                side += proj_tasks("qi", 512, ev_qi)
            per = (len(side) + 3) // 4
            if b == FIRST_OWN:
                ts(hstate[:], hstate[:], cv(C_CF), None, ALU.mult, None, ["hstate", "cvec"], ["hstate"])
            def lru_bufs(c):
                pb = c % 2
                base = pb * 7 * T
                names = ["ua", "r", "ig", "a", "a2", "m", "u"]
                d = {n: arena(base + i * T, T) for i, n in enumerate(names)}
                d["h"] = arena(HOFF + c * T, T)
                return pb, d

            for c0 in range(0, 8, 2):
                pair = (c0, c0 + 1)
                for c in pair:
                    pb, d = lru_bufs(c)
                    ua, uak = d["ua"]
                    ts(ua, lrux[:, c, 0:T], cv(C_LCW + c * 4), cv(C_LCB + c), ALU.mult, ALU.add, LXK + ["cvec"], uak)
                    for jj in range(1, 4):
                        stt(ua, lrux[:, c, jj:jj + T], cv(C_LCW + c * 4 + jj), ua, ALU.mult, ALU.add, LXK + ["cvec"] + uak, uak)
                    cp(uab[:, pb, :], ua, uak, [("uab", pb)])
                for c in pair:
                    pb, d = lru_bufs(c)
                    (r_, rk), (ig, igk) = d["r"], d["ig"]
                    lb, lk = (pL0, ("ps", "L0")) if pb == 0 else (pL1, ("ps", "L1"))
                    mm(lb[:, 0:T], wax[:, c, 0:128], uab[:, pb, :], True, True, [waxk, ("uab", pb)], [lk])
                    act(r_, lb[:, 0:T], AF.Tanh, [lk, "hb"], rk, bias=hba[:, c:c + 1], scale=0.5)
                    mm(lb[:, 0:T], wax[:, c, 128:256], uab[:, pb, :], True, True, [waxk, ("uab", pb)], [lk])
                    act(ig, lb[:, 0:T], AF.Tanh, [lk, "hb"], igk, bias=hbx[:, c:c + 1], scale=0.5)
                for c in pair:
                    pb, d = lru_bufs(c)
                    (r_, rk), (a_, ak), (a2, a2k) = d["r"], d["a"], d["a2"]
                    act(a_, r_, AF.Exp, rk + ["s8"], ak, bias=s8h[:, c:c + 1], scale=s8h[:, c:c + 1])
                    act(a2, r_, AF.Exp, rk + ["s16"], a2k, bias=s16h[:, c:c + 1], scale=s16h[:, c:c + 1])
                for c in pair:
                    pb, d = lru_bufs(c)
                    (a2, a2k), (m_, mk) = d["a2"], d["m"]
                    act(m_, a2, AF.Sqrt, a2k + ["cvec"], mk, bias=cv(C_ONE), scale=-1.0)
                for c in pair:
                    pb, d = lru_bufs(c)
                    (ua, uak), (ig, igk), (m_, mk), (u_, uk), (a_, ak), (hc, hck) = d["ua"], d["ig"], d["m"], d["u"], d["a"], d["h"]
                    stt(u_, ig, 1.0, ua, ALU.add, ALU.mult, igk + uak, uk)
                    stt(u_, u_, 0.5, m_, ALU.mult, ALU.mult, uk + mk, uk)
                    S.op("dve", lambda en: en.tensor_tensor_scan(out=hc, data0=a_, data1=u_, initial=hstate[:, c:c + 1],
                                                                 op0=ALU.mult, op1=ALU.add), ak + uk + ["hstate"], hck)
                    cp(hstate[:, c:c + 1], hc[:, T - 1:T], hck, ["hstate"], e="dve")
                S.phase = "P1.side"
                for _ in range(per):
                    if side:
                        side.pop(0)()
                S.phase = "P1.lru"
            while side:
                side.pop(0)()
            cp(lhalo[:], lrux[:, :, T:T + 3], LXK, ["lhalo"], e="pool")

            if not full:
                return

            S.phase = "P1.q"
            def ev_lg(base):
                def f(ci, bank, bkey):
                    c = base + ci
                    hc, hck = arena(HOFF + c * T, T)
                    gt = tmpf[:, ci % 2, 0:T]
                    act(gt, bank[:, 0:T], AF.Gelu_apprx_tanh, [bkey], [("tmpf", ci % 2)])
                    tt(hgT[:, c, :], gt, hc, ALU.mult, [("tmpf", ci % 2)] + hck, [("hgT", c)])
                return f
            proj_fm("lg0", 512, ev_lg(0))
            proj_fm("lg1", 512, ev_lg(4))

            for j, g in enumerate(tiles):
                attention(j, g)

            S.phase = "M.merge"
            ya_t = lambda m: arena(m * T, T)
            yb_t = lambda m: arena(8 * T + m * T, T)

            def ev_ya(base):
                def f(ci, bank, bkey):
                    v, k = ya_t(base + ci)
                    act(v, bank[:, 0:T], AF.Copy, [bkey], k, scale=0.5)
                return f
            HGK = [("hgT", c) for c in range(8)]
            proj_fm("lbr0", 512, ev_ya(0), rhsT=hgT, rkeys=HGK)
            proj_fm("lbr1", 512, ev_ya(4), rhsT=hgT, rkeys=HGK)

            def ev_ga(base):
                def f(ci, bank, bkey):
                    v, k = ya_t(base + ci)
                    sgt = tmpf[:, ci % 2, 0:T]
                    act(sgt, bank[:, 0:T], AF.Tanh, [bkey], [("tmpf", ci % 2)], scale=0.5)
                    stt(v, sgt, 1.0, v, ALU.add, ALU.mult, k + [("tmpf", ci % 2)], k)
                return f
            proj_fm("ga0", 512, ev_ga(0))
            proj_fm("ga1", 512, ev_ga(4))

            def ev_yb(base):
                def f(ci, bank, bkey):
                    v, k = yb_t(base + ci)
                    act(v, bank[:, 0:T], AF.Copy, [bkey], k, scale=0.5)
                return f
            ATK = [("attnT", jj) for jj in range(TB)]
            proj_fm("abr0", 512, ev_yb(0), rhsT=attnT, rkeys=ATK)
            proj_fm("abr1", 512, ev_yb(4), rhsT=attnT, rkeys=ATK)

            def ev_gb(base):
                def f(ci, bank, bkey):
                    m = base + ci
                    va, ka = ya_t(m)
                    vb, kb = yb_t(m)
                    sgt = tmpf[:, ci % 2, 0:T]
                    act(sgt, bank[:, 0:T], AF.Tanh, [bkey], [("tmpf", ci % 2)], scale=0.5)
                    stt(vb, sgt, 1.0, vb, ALU.add, ALU.mult, kb + [("tmpf", ci % 2)], kb)
                    tt(mergedT[:, m, :], vb, va, ALU.add, ka + kb, [("qT", m)])
                return f
            proj_fm("gb0", 512, ev_gb(0))
            proj_fm("gb1", 512, ev_gb(4))

            def tm_accum(wname, lhs, lkeys):
                wv, wkey = wget(wname)
                n = int(wname[-1] in "1b")
                for j in range(TB):
                    bank, bkey = nextbank(True)
                    for kc in range(8):
                        mm(bank[:, 0:512], lhs[:, kc, j * 128:(j + 1) * 128], wv[:, kc, :], kc == 0, kc == 7, [wkey] + lkeys, [bkey])
                    tt(xres[:, j, n * 512:(n + 1) * 512], xres[:, j, n * 512:(n + 1) * 512], bank[:, 0:512], ALU.add,
                       [bkey, ("xres", j)], [("xres", j)])
            MK = [("qT", m) for m in range(8)]
            tm_accum("wo0", mergedT, MK)
            tm_accum("wo1", mergedT, MK)

            S.phase = "F.ffn"
            for j in range(TB):
                norm_transpose(j, C_FFNG)
            for fg in range(3):
                def ev_gate(base):
                    def f(ci, bank, bkey):
                        c = base + ci
                        fc = fg * 8 + c
                        pb = c % 2
                        if b == FIRST_OWN:
                            ts(gbuf[:, pb, 0:2], fhalo[:, fc, :], cv(C_CF), None, ALU.mult, None, ["fhalo", "cvec"], [("gbuf", pb)], e="pool")
                        else:
                            cp(gbuf[:, pb, 0:2], fhalo[:, fc, :], ["fhalo"], [("gbuf", pb)], e="pool")
                        cp(gbuf[:, pb, 2:2 + T], bank[:, 0:T], [bkey], [("gbuf", pb)])
                        cp(fhalo[:, fc, :], gbuf[:, pb, T:T + 2], [("gbuf", pb)], ["fhalo"], e="pool")
                        gc = gcb[:, pb, :]
                        ts(gc, gbuf[:, pb, 0:T], cv(C_FCW + fc * 3), cv(C_FCB + fc), ALU.mult, ALU.add, [("gbuf", pb), "cvec"], [("gcb", pb)])
                        for jj in (1, 2):
                            stt(gc, gbuf[:, pb, jj:jj + T], cv(C_FCW + fc * 3 + jj), gc, ALU.mult, ALU.add,
                                [("gbuf", pb), "cvec", ("gcb", pb)], [("gcb", pb)])
                        glv, glk = arena(c * T, T)
                        act(glv, gc, AF.Gelu_apprx_tanh, [("gcb", pb)], glk)
                    return f
                proj_fm("upg%da" % fg, 512, ev_gate(0))
                proj_fm("upg%db" % fg, 512, ev_gate(4))

                def ev_val(base):
                    def f(ci, bank, bkey):
                        c = base + ci
                        glv, glk = arena(c * T, T)
                        tt(actT[:, c, :], glv, bank[:, 0:T], ALU.mult, glk + [bkey], [("hgT", c)])
                    return f
                proj_fm("upv%da" % fg, 512, ev_val(0))
                proj_fm("upv%db" % fg, 512, ev_val(4))
                AK = [("hgT", c) for c in range(8)]
                tm_accum("dn%da" % fg, actT, AK)
                tm_accum("dn%db" % fg, actT, AK)

            S.phase = "E.ple"
            for j in range(TB):
                norm_transpose(j, C_PLEG)
            for j, g in enumerate(tiles):
                S.dma("pool", p_tm[:], pbuf[g * 128:(g + 1) * 128, :], writes=["p_tm"])
                for k2 in range(2):
                    tr(pT_ps[:, k2 * 128:(k2 + 1) * 128], p_tm[:, k2 * 128:(k2 + 1) * 128], ["p_tm"], [("ps", "T")])
                cp(pT[:, :, j * 128:(j + 1) * 128], pT_ps[:, 0:256].rearrange("p (k t) -> p k t", k=2), [("ps", "T")], [("pT", j)])
            pp, ppk = wget("pp")
            W["hold"] = W["consumed"] - 1
            for n in range(2):
                pg, pgk = wget("pg%d" % n)
                for j, g in enumerate(tiles):
                    bank, bkey = nextbank()
                    for kc in range(8):
                        mm(bank[:, 0:512], xnT[:, kc, j * 128:(j + 1) * 128], pg[:, kc, :], kc == 0, kc == 7, [pgk, ("xnT", j)], [bkey])
                    sg = tmpf[:, 0, :]
                    act(sg, bank[:, 0:512], AF.Tanh, [bkey], [("tmpf", 0)], scale=0.5)
                    for k2 in range(2):
                        mm(pL0[:, 0:512], pT[:, k2, j * 128:(j + 1) * 128], pp[:, k2, n * 512:(n + 1) * 512], k2 == 0, k2 == 1,
                           [ppk, ("pT", j)], [("ps", "L0")])
                    stt(sg, sg, 1.0, pL0[:, 0:512], ALU.add, ALU.mult, [("tmpf", 0), ("ps", "L0")], [("tmpf", 0)])
                    stt(xres[:, j, n * 512:(n + 1) * 512], sg, 0.5, xres[:, j, n * 512:(n + 1) * 512], ALU.mult, ALU.add,
                        [("tmpf", 0), ("xres", j)], [("xres", j)])
            W["hold"] = None
            if b >= FIRST_OWN:
                for j, g in enumerate(tiles):
                    S.dma("pool", out[(g - NCT) * 128:(g - NCT + 1) * 128, :], xres[:, j, :], reads=[("xres", j)], writes=[("out", g)])

        HOFF = 14 * T
        assert HOFF + 8 * T <= ARN

        OB = [(pOA, "OA", 0), (pOA, "OA", 1), (pOA, "OA", 2), (pOB, "OB", 0), (pOB, "OB", 1), (pOB, "OB", 2), (pOC, "OC", 0), (pOC, "OC", 1)]

        def attention(j, g):
            L = (g + 1) * 128
            nch = (L + 511) // 512
            js = slice(j * 128, (j + 1) * 128)
            S.phase = "A.idx"
            for ch in range(nch):
                n = min(512, L - ch * 512)
                sc = score[:, ch * 512:ch * 512 + n]
                sk = [("sc", ch)]
                kik = [("ki", kt) for kt in range(ch * 4, ch * 4 + n // 128)]
                for h in range(8):
                    bank, bkey = nextbank_idx()
                    p0 = 64 * (h % 2)
                    mm(bank[:, 0:n], qiT[p0:p0 + 64, h // 2, js], kiT[p0:p0 + 64, ch * 512:ch * 512 + n], True, True,
                       ["qiT"] + kik, [bkey])
                    act(rl[:, h % 2, 0:n], bank[:, 0:n], AF.Relu, [bkey], [("tmpf", h % 2)])
                    if h == 0:
                        ts(sc, rl[:, 0, 0:n], weff[:, j, 0:1], None, ALU.mult, None, [("tmpf", 0), ("weff", j)], sk)
                    else:
                        stt(sc, rl[:, h % 2, 0:n], weff[:, j, h:h + 1], sc, ALU.mult, ALU.add, [("tmpf", h % 2), ("weff", j)] + sk, sk)
            allk = sck(0, L)
            S.phase = "A.bis"
            S.op("dve", lambda en: en.tensor_reduce(out=sm[:, 20:21], in_=score[:, 0:L], axis=AX.X, op=ALU.max), allk, ["rmax"])
            S.op("dve", lambda en: en.tensor_reduce(out=sm[:, 21:22], in_=score[:, 0:L], axis=AX.X, op=ALU.min), allk, ["rmin"])
            tt(sm[:, 22:23], sm[:, 21:22], sm[:, 20:21], ALU.subtract, ["rmin", "rmax"], ["dd"])
            lo = sm[:, 23:24]
            stt(lo, sm[:, 22:23], 1.0 / 64, sm[:, 21:22], ALU.mult, ALU.add, ["dd", "rmin"], ["lo"])
            tt(sm[:, 24:25], sm[:, 20:21], lo, ALU.subtract, ["rmax", "lo"], ["w0"])
            ts(hw[:], cv(C_POW, NIT), sm[:, 24:25], None, ALU.mult, None, ["cvec", "w0"], ["hw"])
            dg = score[:, g * 128:(g + 1) * 128]
            tt(dg, dg, tri[:], ALU.add, [("sc", g // 4), "tri"], [("sc", g // 4)])
            cend = min(L, CTXK)
            nA = min(cend, 3072 if L < 6656 else 4096)
            act_pieces = []
            if nA > 0:
                n0 = min(nA, 2048)
                act_pieces.append((0, n0, junk2[:, 2048:2048 + n0], "junk2b"))
                if nA > 2048:
                    n1 = min(nA, 3072) - 2048
                    act_pieces.append((2048, n1, nm[:].rearrange("p a n -> p (a n)")[:, 0:n1], ("nm", 0)))
                if nA > 3072:
                    act_pieces.append((3072, nA - 3072, pTb[:].rearrange("p a n -> p (a n)")[:, 0:nA - 3072], ("pTb", 0)))
            assert len(act_pieces) <= 3 and sum(p[1] for p in act_pieces) == nA
            dve_pieces = []
            if cend > nA:
                assert cend - nA <= 2048
                dve_pieces.append((nA, cend - nA, 0))
            col = 1
            for a in range(CTXK, L, 2048):
                dve_pieces.append((a, min(2048, L - a), col))
                col += 1
            assert col <= 3
            mid, tot, stp, kadj = sm[:, 25:26], sm[:, 27:28], sm[:, 28:29], sm[:, 30:31]
            prod = sm[:, 40:46]
            CK = [("cnt", k) for k in range(6)]
            S.op("dve", lambda en: en.memset(cntb[:, 0:6], 0.0), [], CK)
            ts(kadj, cv(C_CF), -0.5 * nA, float(TOPK), ALU.mult, ALU.add, ["cvec"], ["kadj"])
            ts(mid, lo, hw[:, 0:1], None, ALU.add, None, ["lo", "hw"], ["mid"])
            for it in range(NIT):
                for k, (a, n, dst, dkey) in enumerate(act_pieces):
                    act(dst, score[:, a:a + n], AF.Sign, sck(a, a + n) + ["mid"], [dkey, ("cnt", 3 + k)] + ([("nm", 1)] if k == 1 else []) + ([("pTb", 1)] if k == 2 else []),
                        bias=mid, scale=-1.0, accum=cntb[:, 3 + k:4 + k])
                for (a, n, c) in dve_pieces:
                    ts(junk2[:, 0:n], score[:, a:a + n], mid, None, ALU.is_gt, ALU.add,
                       sck(a, a + n) + ["mid"], ["junk2", ("cnt", c)], accum=cntb[:, c:c + 1])
                tt(prod, cntb[:, 0:6], wv[:, 0:6], ALU.mult, CK + ["wv"], ["prod"])
                S.op("dve", lambda en: en.tensor_reduce(out=tot, in_=prod, axis=AX.X, op=ALU.add), ["prod"], ["tot"])
                stt(stp, tot, kadj, hw[:, it:it + 1], ALU.is_ge, ALU.mult, ["tot", "hw", "kadj"], ["stp"])
                nx = min(it + 1, NIT - 1)
                dst, dk = (mid, "mid") if it < NIT - 1 else (lo, "lo")
                stt(dst, mid, hw[:, nx:nx + 1], stp, ALU.subtract, ALU.add, ["mid", "hw", "stp"], [dk])
            loc = sm[:, 29:30]
            ts(loc, lo, cv(C_CF), cv(C_CF + 1), ALU.mult, ALU.add, ["lo", "cvec"], ["loc"])
            S.phase = "A.att"
            steps = [(kt, kv) for kt in range(g + 1) for kv in range(2)]

            def qk(i):
                kt, kv = steps[i]
                ch, off = kt // 4, (kt % 4) * 128
                if kt % 4 == 0 and kv == 0:
                    n = min(512, L - ch * 512)
                    isctx = ch * 512 < CTXK
                    ts(nm[:, ch % 2, 0:n], score[:, ch * 512:ch * 512 + n], (loc if isctx else lo), -30000.0, ALU.is_le, ALU.mult,
                       [("sc", ch), "lo", "loc"], [("nm", ch % 2)])
                near = kt >= g - 1
                lgb, lgk = (pL0, ("ps", "L0")) if kv == 0 else (pL1, ("ps", "L1"))
                lg3 = lgb[:, 0:512].rearrange("p (a t) -> p a t", a=4)
                mm(lg3, kT[:, kv, kt * 128:(kt + 1) * 128], qT[:, 4 * kv:4 * kv + 4, js], True, False,
                   [("kT", kt)] + [("qT", 4 * kv + a) for a in range(4)], [lgk])
                mm(lg3, nm[:, ch % 2, off:off + 128], ident4[:], False, not near, [("nm", ch % 2), "ident4"], [lgk])
                if near:
                    mm(lg3, ident[:], biasn[:, g - kt, 4 * kv:4 * kv + 4, :], False, True, ["ident", "biasn"], [lgk])

            def pv(i):
                kt, kv = steps[i]
                lgb, lgk = (pL0, ("ps", "L0")) if kv == 0 else (pL1, ("ps", "L1"))
                pk = ("pTb", i % 2)
                pvv = pTb[:, i % 2, :]
                act(pvv, lgb[:, 0:512], AF.Exp, [lgk], [pk])
                for a in range(4):
                    hd = 4 * kv + a
                    ob, obk, sl = OB[hd]
                    mm(ob[:, sl * 129:(sl + 1) * 129], pvv[:, a * 128:(a + 1) * 128], Vt[:, kt, kv, :], (kt == 0 and sl == 0), kt == g,
                       [pk, ("V", kt), "Vones"], [("ps", obk)], skip=True)

            qk(0)
            qk(1)
            for i in range(len(steps)):
                pv(i)
                if i + 2 < len(steps):
                    qk(i + 2)
            S.phase = "A.fin"
            for (ob, obk, nh, h0) in ((pOA, "OA", 3, 0), (pOB, "OB", 3, 3), (pOC, "OC", 2, 6)):
                o3 = ob[:, 0:nh * 129].rearrange("p (h d) -> p h d", d=129)
                ts(den[:, h0:h0 + nh], o3[:, :, 128], 1e-30, None, ALU.max, None, [("ps", obk)], ["den"])
            S.op("dve", lambda en: en.reciprocal(out=rden[:], in_=den[:]), ["den"], ["rden"])
            for (ob, obk, nh, h0) in ((pOA, "OA", 3, 0), (pOB, "OB", 3, 3), (pOC, "OC", 2, 6)):
                o3 = ob[:, 0:nh * 129].rearrange("p (h d) -> p h d", d=129)
                tt(attn_tm[:, h0 * 128:(h0 + nh) * 128].rearrange("p (h d) -> p h d", d=128), o3[:, :, 0:128],
                   rden[:, h0:h0 + nh].unsqueeze(2).to_broadcast([128, nh, 128]), ALU.mult, [("ps", obk), "rden"], ["xn_tm"])
            for hd in range(8):
                tr(pT_ps[:, hd * 128:(hd + 1) * 128], attn_tm[:, hd * 128:(hd + 1) * 128], ["xn_tm"], [("ps", "T")])
            cp(attnT[:, :, js], pT_ps[:, 0:1024].rearrange("p (h t) -> p h t", h=8), [("ps", "T")], [("attnT", j)])

        for b in range(NBLK):
            block(b, b >= FIRST_FULL)
        S.final_wait("sp", [("out", g) for g in range(NCT, NT)])
        S.final_wait("pool", [("out", g) for g in range(NCT, NT)])
    print("ops", S.nops, {e: S.cnt[e] for e in S.cnt})
    return nc


def _bucket(dist):
    d_f = np.maximum(dist, 1).astype(np.float32)
    large = 16 + (np.log(d_f / np.float32(16)) / np.float32(math.log(128 / 16)) * np.float32(16)).astype(np.int32)
    large = np.minimum(large, 31)
    return np.where(dist < 16, dist, large)


def make_inputs(inp, NT, NIT=20):
    f32 = np.float32
    x = np.asarray(inp["x"], f32)
    p = np.asarray(inp["p"], f32)[0]
    Bn, SEQ, _ = x.shape
    half = SEQ // 2
    assert NT * 128 == SEQ

    def col(v, nch):
        return np.ascontiguousarray(np.asarray(v, f32).reshape(nch, 128).T)

    cvec = np.zeros((128, C_POW + NIT), f32)
    cvec[:, C_MIXG:C_MIXG + 8] = col(inp["mix_norm"][0], 8)
    cvec[:, C_FFNG:C_FFNG + 8] = col(inp["ffn_norm"][0], 8)
    cvec[:, C_PLEG:C_PLEG + 8] = col(inp["ple_norm"][0], 8)
    lcw = np.asarray(inp["lru_conv_w"], f32)[0]
    cvec[:, C_LCW:C_LCW + 32] = lcw.reshape(4, 8, 128).transpose(2, 1, 0).reshape(128, 32)
    cvec[:, C_LCB:C_LCB + 8] = col(inp["lru_conv_b"][0], 8)
    cvec[:, C_LBA:C_LBA + 8] = col(inp["lru_ba"][0], 8)
    cvec[:, C_LBX:C_LBX + 8] = col(inp["lru_bx"][0], 8)
    cvec[:, C_LAM:C_LAM + 8] = col(inp["lru_lambda"][0], 8)
    cvec[:, C_GQ] = np.asarray(inp["q_norm"], f32)[0]
    cvec[:, C_GK] = np.asarray(inp["k_norm"], f32)[0]
    fcw = np.asarray(inp["ffn_conv_w"], f32)[0]
    cvec[:, C_FCW:C_FCW + 72] = fcw.reshape(3, 24, 128).transpose(2, 1, 0).reshape(128, 72)
    cvec[:, C_FCB:C_FCB + 24] = col(inp["ffn_conv_b"][0], 24)
    cvec[:, C_EPS] = EPS
    cvec[:, C_ONE] = 1.0
    cvec[:, C_POW:C_POW + NIT] = (0.5 ** np.arange(1, NIT + 1, dtype=np.float64)).astype(f32)[None, :]

    tq = np.arange(128)[:, None]
    sk = np.arange(128)[None, :]
    tri = np.where(sk <= tq, 0.0, -1e30).astype(f32)
    ident = np.eye(128, dtype=f32)
    rb = np.asarray(inp["rel_bias"], f32)
    ss = np.arange(128)[:, None, None]
    oo = np.arange(2)[None, :, None]
    tt_ = np.arange(128)[None, None, :]
    dist = np.maximum(tt_ - ss + 128 * oo, 0).astype(np.int32)
    bidx = _bucket(dist)
    bn = rb[bidx]
    bn = np.ascontiguousarray(bn.transpose(0, 1, 3, 2)).reshape(128, 2 * 8 * 128)
    b31 = np.ascontiguousarray(rb[np.full_like(bidx, 31)].transpose(0, 1, 3, 2)).reshape(128, 2 * 8 * 128)

    shared = {
        "w_in": np.ascontiguousarray(inp["w_in"][0], f32), "w_lru_br": np.ascontiguousarray(inp["w_lru_br"][0], f32),
        "w_attn_br": np.ascontiguousarray(inp["w_attn_br"][0], f32), "w_out": np.ascontiguousarray(inp["w_out"][0], f32),
        "w_up": np.ascontiguousarray(inp["w_up"][0], f32), "w_down": np.ascontiguousarray(inp["w_down"][0], f32),
        "w_ple_gate": np.ascontiguousarray(inp["w_ple_gate"][0], f32), "w_ple_proj": np.ascontiguousarray(inp["w_ple_proj"][0], f32),
        "lru_wa": np.ascontiguousarray(inp["lru_wa"][0], f32), "lru_wx": np.ascontiguousarray(inp["lru_wx"][0], f32),
        "tri": tri, "ident": ident, "bias_near": bn, "bias_far": b31,
    }
    maps = []
    for c in range(2 * Bn):
        b, h = c // 2, c % 2
        cv_c = cvec.copy()
        if h == 1:
            xb, pb = x[b], p[b]
            cv_c[:, C_CF], cv_c[:, C_CF + 1] = 1.0, 0.0
        else:
            xb = np.concatenate([np.zeros((half, D), f32), x[b, :half]], axis=0)
            pb = np.concatenate([np.zeros((half, 256), f32), p[b, :half]], axis=0)
            cv_c[:, C_CF], cv_c[:, C_CF + 1] = 0.0, 1e30
        m = dict(shared)
        m["xbuf"] = np.ascontiguousarray(xb)
        m["pbuf"] = np.ascontiguousarray(pb)
        m["cvec"] = cv_c
        maps.append(m)
    return maps


def run(inp, NT, NIT=20):
    x = np.asarray(inp["x"])
    Bn, SEQ, _ = x.shape
    half = SEQ // 2
    nc = build(NT, NIT=NIT)
    maps = make_inputs(inp, NT, NIT=NIT)
    res = run_bass_kernel_spmd(nc, maps, core_ids=list(range(2 * Bn)))
    outp = np.empty((Bn, SEQ, D), np.float32)
    for c in range(2 * Bn):
        b, h = c // 2, c % 2
        outp[b, h * half:(h + 1) * half] = res.results[c]["out"]
    return outp


def kernel(**inputs):
    return run(inputs, 64, NIT=20)
```

```python
import math
from contextlib import ExitStack

import numpy as np
import concourse.bass as bass
import concourse.mybir as mybir
from concourse.bass_utils import run_bass_kernel_spmd

F32 = mybir.dt.float32
BF16 = mybir.dt.bfloat16
AF = mybir.ActivationFunctionType
ALU = mybir.AluOpType
AX = mybir.AxisListType

D = 1024
TB = 2
T = TB * 128
NSLOT = 4
EPS = 1e-6

C_MIXG, C_FFNG, C_PLEG = 0, 8, 16
C_LCW, C_LCB, C_LBA, C_LBX, C_LAM = 24, 56, 64, 72, 80
C_GQ, C_GK = 88, 89
C_FCW, C_FCB = 90, 162
C_CF = 186
C_EPS, C_ONE = 188, 189
C_POW = 190


class Sched:
    def __init__(self, nc, ndma=24):
        self.nc = nc
        self.eng = {"pe": nc.tensor, "act": nc.scalar, "dve": nc.vector, "pool": nc.gpsimd, "sp": nc.sync}
        self.sems = {}
        self.cnt = {}
        self.waited = {e: {} for e in self.eng}
        self.bufs = {}
        self.ndma = ndma
        self.dma_i = 0
        self.pool_i = 0
        self.nops = 0
        self.phase = None

    def setup(self, stack):
        for e in self.eng:
            self.sems[e] = stack.enter_context(self.nc.semaphore("s_" + e))
            self.cnt[e] = 0
        self.dsems = [stack.enter_context(self.nc.semaphore("d%d" % i)) for i in range(self.ndma)]
        self.dcnt = [0] * self.ndma
        self.semobj = dict(self.sems)
        for i, s in enumerate(self.dsems):
            self.semobj["d%d" % i] = s

    def _deps(self, reads, writes):
        deps = {}

        def add(k, v):
            if deps.get(k, 0) < v:
                deps[k] = v
        for b in reads:
            st = self.bufs.get(b)
            if st and st["w"]:
                add(*st["w"])
        for b in writes:
            st = self.bufs.get(b)
            if st:
                if st["w"]:
                    add(*st["w"])
                for k, v in st["r"].items():
                    add(k, v)
        return deps

    def _wait(self, e, deps, keep_one=False):
        eng = self.eng[e]
        need = [(k, v) for k, v in deps.items() if self.waited[e].get(k, 0) < v]
        inline = None
        if keep_one and need:
            inline = need.pop()
        for k, v in need:
            eng.wait_ge(self.semobj[k], v)
            self.waited[e][k] = v
        if inline is not None:
            self.waited[e][inline[0]] = inline[1]
            return (self.semobj[inline[0]], inline[1])
        return None

    def _update(self, ev, reads, writes):
        for b in reads:
            st = self.bufs.setdefault(b, {"w": None, "r": {}})
            if st["r"].get(ev[0], 0) < ev[1]:
                st["r"][ev[0]] = ev[1]
        for b in writes:
            self.bufs[b] = {"w": ev, "r": {}}

    def op(self, e, fn, reads=(), writes=()):
        deps = self._deps(reads, writes)
        if e == "pe":
            deps.pop("pe", None)
        inl = self._wait(e, deps, keep_one=True)
        ins = fn(self.eng[e])
        if inl is not None:
            ins._wait_ge(inl[0], inl[1])
        if self.phase is not None:
            ins.annotate(self.phase)
        self.cnt[e] += 1
        ins.then_inc(self.sems[e], 1)
        self._update((e, self.cnt[e]), reads, writes)
        self.nops += 1
        return ins

    def dma(self, e, out, in_, reads=(), writes=()):
        if e == "pool":
            i = self.pool_i % 3
            self.pool_i += 1
        else:
            i = 3 + self.dma_i % (self.ndma - 3)
            self.dma_i += 1
        key = "d%d" % i
        deps = self._deps(reads, writes)
        if self.dcnt[i] > 0:
            deps[key] = max(deps.get(key, 0), self.dcnt[i])
        self._wait(e, deps)
        ins = self.eng[e].dma_start(out=out, in_=in_)
        self.dcnt[i] += 16
        ins.then_inc(self.dsems[i], 16)
        self._update((key, self.dcnt[i]), reads, writes)
        self.nops += 1
        return ins

    def final_wait(self, e, bufs):
        self._wait(e, self._deps(bufs, bufs))


def build(NT, NIT=20, TOPK=256):
    NCT = NT // 2
    NTOK = NT * 128
    NBLK = NT // TB
    FIRST_OWN = NCT // TB
    FIRST_FULL = FIRST_OWN - 1
    CTXK = NCT * 128
    NOWN = (NT - NCT) * 128

    nc = bass.Bass("TRN2", target_bir_lowering=False)

    def din(name, shape, dt=F32):
        return nc.dram_tensor(name, shape, dt, kind="ExternalInput").ap()

    def dscr(name, shape, dt=BF16):
        return nc.dram_tensor(name, shape, dt, kind="Internal").ap()

    xbuf = din("xbuf", [NTOK, D])
    pbuf = din("pbuf", [NTOK, 256])
    w_in = din("w_in", [D, 6216])
    w_lbr = din("w_lru_br", [D, D])
    w_abr = din("w_attn_br", [D, D])
    w_out = din("w_out", [D, D])
    w_up = din("w_up", [D, 6144])
    w_down = din("w_down", [3072, D])
    w_pg = din("w_ple_gate", [D, D])
    w_pp = din("w_ple_proj", [256, D])
    lru_wa = din("lru_wa", [8, 128, 128])
    lru_wx = din("lru_wx", [8, 128, 128])
    cvec_d = din("cvec", [128, C_POW + NIT])
    tri_d = din("tri", [128, 128])
    ident_d = din("ident", [128, 128])
    bn_d = din("bias_near", [128, 2 * 8 * 128])
    b31_d = din("bias_far", [128, 2 * 8 * 128])
    out = nc.dram_tensor("out", [NOWN, D], F32, kind="ExternalOutput").ap()

    S = Sched(nc)
    with ExitStack() as st:
        S.setup(st)

        def sb(name, shape, dt=F32):
            return st.enter_context(nc.sbuf_tensor(name, shape, dt))

        kT = sb("kT", [128, 2, NTOK], BF16)
        Vt = sb("Vt", [128, NT, 2, 129], BF16)
        kiT = sb("kiT", [128, NTOK], BF16)
        wsl = sb("wsl", [128, NSLOT, 4096], BF16)
        lhalo = sb("lhalo", [128, 8, 3])
        cvec = sb("cvec_s", [128, C_POW + NIT])
        tri = sb("tri_s", [128, 128])
        ident_f = sb("ident_f", [128, 128])
        ident = sb("ident_b", [128, 128], BF16)
        ident4 = sb("ident4", [128, 4, 128], BF16)
        ones_b = sb("ones_b", [128, 128], BF16)
        biasn = sb("biasn", [128, 2, 8, 128], BF16)
        s8h = sb("s8h", [128, 8])
        s16h = sb("s16h", [128, 8])
        hba = sb("hba", [128, 8])
        hbx = sb("hbx", [128, 8])
        gqs = sb("gqs", [128, 1])
        hstate = sb("hstate", [128, 8])
        fhalo = sb("fhalo", [128, 24, 2])
        xres = sb("xres", [128, TB, D])
        xnT = sb("xnT", [128, 8, T], BF16)
        xn_tm = sb("xn_tm", [128, D], BF16)
        qT = sb("qT", [128, 8, T], BF16)
        qiT = sb("qiT", [128, 4, T], BF16)
        weff = sb("weff", [128, TB, 8])
        hgT = sb("hgT", [128, 8, T], BF16)
        attnT = sb("attnT", [128, 8, T], BF16)
        attn_tm = xn_tm
        mergedT = qT
        actT = hgT
        pT = sb("pT", [128, 2, T], BF16)
        p_tm = sb("p_tm", [128, 256], BF16)
        sm = sb("sm", [128, 64])
        hw = sb("hw", [128, NIT])
        cntb = sb("cntb", [128, 8])
        wv = sb("wv", [128, 8])
        den = sb("den", [128, 8])
        rden = sb("rden", [128, 8])
        uab = sb("uab", [128, 2, T], BF16)
        qsq = sb("qsq", [128, T], BF16)
        tmpf = sb("tmpf", [128, 2, 512])
        gbuf = sb("gbuf", [128, 2, T + 2])
        gcb = sb("gcb", [128, 2, T])
        rl = tmpf
        nm = sb("nm", [128, 2, 512], BF16)
        pTb = sb("pTb", [128, 2, 512], BF16)
        waxs = sb("waxs", [128, 8, 256], BF16)
        junk2 = sb("junk2", [128, 4096], BF16)
        ARN = max(NTOK, 32 * T + 24)
        score = sb("score", [128, ARN])
        LOFF = 24 * T
        lrux = score[:, LOFF:LOFF + 8 * (T + 3)].rearrange("p (c t) -> p c t", c=8)
        LXK = [("sc", c) for c in range(LOFF // 512, (LOFF + 8 * (T + 3) - 1) // 512 + 1)]
        pA = st.enter_context(nc.psum_tensor("pA", [128, 512], F32))
        pB = st.enter_context(nc.psum_tensor("pB", [128, 512], F32))
        pL0 = st.enter_context(nc.psum_tensor("pL0", [128, 512], F32))
        pL1 = st.enter_context(nc.psum_tensor("pL1", [128, 512], F32))
        pOA = st.enter_context(nc.psum_tensor("pOA", [128, 512], F32))
        pOB = st.enter_context(nc.psum_tensor("pOB", [128, 512], F32))
        pOC = st.enter_context(nc.psum_tensor("pOC", [128, 512], F32))
        pT_ps = st.enter_context(nc.psum_tensor("pTp", [128, 1024], BF16))
        st.enter_context(nc.Block())

        banks = {"A": pA, "B": pB, "OA": pOA, "OB": pOB, "OC": pOC, "L0": pL0, "L1": pL1}
        rot = {"n": 0, "i": 0}
        DENSE_ROT = ["A", "B"]

        ROT4 = ["A", "B", "L0", "L1"]

        def nextbank(rot4=False):
            r = ROT4 if rot4 else DENSE_ROT
            k = r[rot["n"] % len(r)]
            rot["n"] += 1
            return banks[k], ("ps", k)

        def nextbank_idx():
            k = "AB"[rot["i"] % 2]
            rot["i"] += 1
            return banks[k], ("ps", k)

        def cv(c0, n=1):
            return cvec[:, c0:c0 + n]

        def sck(a, b):
            return [("sc", c) for c in range(a // 512, (b - 1) // 512 + 1)]

        def arena(a, n):
            return score[:, a:a + n], sck(a, a + n)

        def act(out, in_, func, reads, writes, bias=None, scale=None, accum=None, e="act"):
            kw = {}
            if bias is not None:
                kw["bias"] = bias
            if scale is not None:
                kw["scale"] = scale
            if accum is not None:
                kw["accum_out"] = accum
            return S.op(e, lambda en: en.activation(out=out, in_=in_, func=func, **kw), reads, writes)

        def ts(out, in0, s1, s2, op0, op1, reads, writes, accum=None, e="dve"):
            kw = {}
            if accum is not None:
                kw["accum_out"] = accum
            if op1 is None:
                return S.op(e, lambda en: en.tensor_scalar(out=out, in0=in0, scalar1=s1, scalar2=s2, op0=op0, **kw), reads, writes)
            return S.op(e, lambda en: en.tensor_scalar(out=out, in0=in0, scalar1=s1, scalar2=s2, op0=op0, op1=op1, **kw), reads, writes)

        def stt(out, in0, scalar, in1, op0, op1, reads, writes, e="dve"):
            return S.op(e, lambda en: en.scalar_tensor_tensor(out=out, in0=in0, scalar=scalar, in1=in1, op0=op0, op1=op1), reads, writes)

        def tt(out, in0, in1, op, reads, writes, e="dve"):
            return S.op(e, lambda en: en.tensor_tensor(out=out, in0=in0, in1=in1, op=op), reads, writes)

        def cp(out, in_, reads, writes, e="act"):
            if e == "act":
                return S.op(e, lambda en: en.activation(out=out, in_=in_, func=AF.Copy), reads, writes)
            return S.op(e, lambda en: en.tensor_copy(out=out, in_=in_), reads, writes)

        def mm(out, lhsT, rhs, start, stop, reads, writes, skip=False):
            if skip:
                return S.op("pe", lambda en: en.matmul(out, lhsT=lhsT, rhs=rhs, start=start, stop=stop, skip_group_check=True), reads, writes)
            return S.op("pe", lambda en: en.matmul(out, lhsT=lhsT, rhs=rhs, start=start, stop=stop), reads, writes)

        def tr(out, in_, reads, writes):
            return S.op("pe", lambda en: en.transpose(out, in_, ident[:]), reads + ["ident"], writes)

        S.dma("sp", cvec[:], cvec_d, writes=["cvec"])
        S.dma("sp", tri[:], tri_d, writes=["tri"])
        S.dma("sp", ident_f[:], ident_d, writes=["identf"])
        bn_t, bn_k = arena(0, 2048)
        b31_t, b31_k = arena(2048, 2048)
        S.dma("sp", bn_t, bn_d, writes=bn_k)
        S.dma("sp", b31_t, b31_d, writes=b31_k)

        units = {}
        unit_order = []

        def unit(name, kc, ncols, parts):
            scr = nc.dram_tensor("u_" + name, [128, kc * ncols], BF16, kind="Internal").ap()
            units[name] = (scr, kc, ncols)
            unit_order.append(name)
            d3 = scr.rearrange("p (k n) -> p k n", k=kc)
            for (dc, src3) in parts:
                w = src3.shape[2]
                S.dma("pool", d3[:, :, dc:dc + w], src3, writes=[("u", name)])

        def kp(ap, r0, nr, c0, ncol):
            return ap[r0:r0 + nr, c0:c0 + ncol].rearrange("(k p) n -> p k n", p=128)

        unit("lrux0", 8, 512, [(0, kp(w_in, 0, D, 0, 512))])
        unit("lrux1", 8, 512, [(0, kp(w_in, 0, D, 512, 512))])
        unit("wax", 8, 256, [(0, lru_wa.rearrange("n c d -> c n d")), (128, lru_wx.rearrange("n c d -> c n d"))])
        unit("kk", 8, 384, [(0, kp(w_in, 0, D, 3072, 256)), (256, kp(w_in, 0, D, 4096, 64)), (320, kp(w_in, 0, D, 4096, 64))])
        unit("vw", 8, 264, [(0, kp(w_in, 0, D, 3328, 256)), (256, kp(w_in, 0, D, 4160, 8))])
        unit("q0", 8, 512, [(0, kp(w_in, 0, D, 2048, 512))])
        unit("q1", 8, 512, [(0, kp(w_in, 0, D, 2560, 512))])
        unit("qi", 8, 512, [(0, kp(w_in, 0, D, 3584, 512))])
        unit("lg0", 8, 512, [(0, kp(w_in, 0, D, 1024, 512))])
        unit("lg1", 8, 512, [(0, kp(w_in, 0, D, 1536, 512))])
        for hh in range(2):
            unit("lbr%d" % hh, 8, 512, [(0, kp(w_lbr, 0, D, hh * 512, 512))])
        for hh in range(2):
            unit("ga%d" % hh, 8, 512, [(0, kp(w_in, 0, D, 4168 + hh * 512, 512))])
        for hh in range(2):
            unit("abr%d" % hh, 8, 512, [(0, kp(w_abr, 0, D, hh * 512, 512))])
        for hh in range(2):
            unit("gb%d" % hh, 8, 512, [(0, kp(w_in, 0, D, 5192 + hh * 512, 512))])
        for hh in range(2):
            unit("wo%d" % hh, 8, 512, [(0, kp(w_out, 0, D, hh * 512, 512))])
        for fg in range(3):
            for hh, ab in enumerate("ab"):
                unit("upg%d%s" % (fg, ab), 8, 512, [(0, kp(w_up, 0, D, fg * 1024 + hh * 512, 512))])
            for hh, ab in enumerate("ab"):
                unit("upv%d%s" % (fg, ab), 8, 512, [(0, kp(w_up, 0, D, 3072 + fg * 1024 + hh * 512, 512))])
            for hh, ab in enumerate("ab"):
                unit("dn%d%s" % (fg, ab), 8, 512, [(0, kp(w_down, fg * 1024, 1024, hh * 512, 512))])
        unit("pp", 2, 1024, [(0, kp(w_pp, 0, 256, 0, 1024))])
        for hh in range(2):
            unit("pg%d" % hh, 8, 512, [(0, kp(w_pg, 0, D, hh * 512, 512))])

        cp(ident[:], ident_f[:], ["identf"], ["ident"], e="dve")
        for a4 in range(4):
            cp(ident4[:, a4, :], ident_f[:], ["identf"], ["ident4"], e="dve")
        S.op("dve", lambda en: en.memset(ones_b[:], 1.0), [], ["ones"])
        S.op("dve", lambda en: en.memset(Vt[:].rearrange("p n k d -> p (n k) d")[:, :, 128:129], 1.0), [], ["Vones"])
        S.op("dve", lambda en: en.memset(lhalo[:], 0.0), [], ["lhalo"])
        S.op("dve", lambda en: en.memset(hstate[:], 0.0), [], ["hstate"])
        S.op("dve", lambda en: en.memset(fhalo[:], 0.0), [], ["fhalo"])
        tt(biasn[:].rearrange("p a h t -> p (a h t)"), bn_t, b31_t, ALU.subtract, bn_k + b31_k, ["biasn"])
        act(sm[:, 0:8], cv(C_LAM, 8), AF.Exp, ["cvec"], ["sm0"], scale=-1.0)
        act(sm[:, 8:16], sm[:, 0:8], AF.Ln, ["sm0"], ["sm1"], bias=cv(C_ONE))
        ts(s8h[:], sm[:, 8:16], -4.0, None, ALU.mult, None, ["sm1"], ["s8"])
        ts(s16h[:], sm[:, 8:16], -8.0, None, ALU.mult, None, ["sm1"], ["s16"])
        ts(hba[:], cv(C_LBA, 8), 0.5, None, ALU.mult, None, ["cvec"], ["hb"])
        ts(hbx[:], cv(C_LBX, 8), 0.5, None, ALU.mult, None, ["cvec"], ["hb"])
        ts(gqs[:], cv(C_GQ), float(128 ** -0.5), None, ALU.mult, None, ["cvec"], ["gqs"])
        S.op("dve", lambda en: en.memset(wv[:], 0.0), [], ["wv"])
        S.op("dve", lambda en: en.memset(wv[:, 1:3], 1.0), ["wv"], ["wv"])
        ts(wv[:, 0:1], cv(C_CF), 1.0, None, ALU.mult, None, ["cvec", "wv"], ["wv"])
        ts(wv[:, 3:4], cv(C_CF), -0.5, None, ALU.mult, None, ["cvec", "wv"], ["wv"])
        ts(wv[:, 4:5], cv(C_CF), -0.5, None, ALU.mult, None, ["cvec", "wv"], ["wv"])
        ts(wv[:, 5:6], cv(C_CF), -0.5, None, ALU.mult, None, ["cvec", "wv"], ["wv"])

        def plan_block(full):
            pl = ["lrux0", "lrux1", "kk", "vw"]
            if not full:
                return pl
            pl += ["q0", "q1", "qi", "lg0", "lg1", "lbr0", "lbr1", "ga0", "ga1", "abr0", "abr1", "gb0", "gb1", "wo0", "wo1"]
            for fg in range(3):
                pl += ["upg%da" % fg, "upg%db" % fg, "upv%da" % fg, "upv%db" % fg, "dn%da" % fg, "dn%db" % fg]
            pl += ["pp", "pg0", "pg1"]
            return pl

        plan = []
        for b in range(NBLK):
            plan += plan_block(b >= FIRST_FULL)
        W = {"issued": 0, "consumed": 0, "hold": None}

        def slotview(i):
            scr, k, n = units[plan[i]]
            s = i % NSLOT
            return wsl[:, s, 0:k * n].rearrange("p (k n) -> p k n", k=k), ("ws", s)

        def wget(name):
            n = W["consumed"]
            assert plan[n] == name, (plan[n], name)
            retired = n if W["hold"] is None else min(n, W["hold"])
            while W["issued"] < min(len(plan), retired + NSLOT):
                i = W["issued"]
                v, key = slotview(i)
                scr, k_, n_ = units[plan[i]]
                S.dma("sp", wsl[:, i % NSLOT, 0:k_ * n_], scr, reads=[("u", plan[i])], writes=[key])
                W["issued"] += 1
            assert W["issued"] > n, name
            W["consumed"] += 1
            return slotview(n)

        def norm_transpose(j, gcol):
            src = xres[:, j, :]
            act(junk2[:, 0:D], src, AF.Square, [("xres", j)], ["junk2", "ssq"], accum=sm[:, 16:17])
            act(sm[:, 17:18], sm[:, 16:17], AF.Sqrt, ["ssq"], ["rstd0"], bias=cv(C_EPS), scale=1.0 / D)
            S.op("dve", lambda en: en.reciprocal(out=sm[:, 18:19], in_=sm[:, 17:18]), ["rstd0"], ["rstd"])
            ts(xn_tm[:], src, sm[:, 18:19], None, ALU.mult, None, [("xres", j), "rstd"], ["xn_tm"])
            for kc in range(8):
                tr(pT_ps[:, kc * 128:(kc + 1) * 128], xn_tm[:, kc * 128:(kc + 1) * 128], ["xn_tm"], [("ps", "T")])
            tt(xnT[:, :, j * 128:(j + 1) * 128], pT_ps[:, 0:1024].rearrange("p (k t) -> p k t", k=8),
               cv(gcol, 8).unsqueeze(2).to_broadcast([128, 8, 128]), ALU.mult, [("ps", "T"), "cvec"], [("xnT", j)])

        XNK = [("xnT", j) for j in range(TB)]

        def proj_fm(wname, ncols, evac, m=128, rhsT=None, rkeys=None, rot4=True):
            wv, wkey = wget(wname)
            rhsT = xnT if rhsT is None else rhsT
            rkeys = XNK if rkeys is None else rkeys
            for ci in range(ncols // m):
                bank, bkey = nextbank(rot4)
                for kc in range(8):
                    mm(bank[0:m, 0:T], wv[:, kc, ci * m:(ci + 1) * m], rhsT[:, kc, :], kc == 0, kc == 7,
                       [wkey] + rkeys, [bkey])
                evac(ci, bank, bkey)

        def proj_tasks(wname, ncols, evac, m=128):
            st8 = {}

            def mk(ci):
                def f():
                    if ci == 0:
                        st8["w"] = wget(wname)
                    wv, wkey = st8["w"]
                    bank, bkey = nextbank()
                    for kc in range(8):
                        mm(bank[0:m, 0:T], wv[:, kc, ci * m:(ci + 1) * m], xnT[:, kc, :], kc == 0, kc == 7, [wkey] + XNK, [bkey])
                    evac(ci, bank, bkey)
                return f
            return [mk(ci) for ci in range(ncols // m)]

        def headnorm(bank, bkey, gcol_ap, gkey, out_ap, okeys):
            act(qsq[:], bank[:, 0:T], AF.Square, [bkey], ["qsq"])
            mm(pL1[:, 0:T], ones_b[:], qsq[:], True, True, ["ones", "qsq"], [("ps", "L1")])
            sd0, sd0k = arena(22 * T, T)
            sd1, sd1k = arena(23 * T, T)
            act(sd0, pL1[:, 0:T], AF.Sqrt, [("ps", "L1"), "cvec"], sd0k, bias=cv(C_EPS), scale=1.0 / 128)
            S.op("dve", lambda en: en.reciprocal(out=sd1, in_=sd0), sd0k, sd1k)
            stt(out_ap, bank[:, 0:T], gcol_ap, sd1, ALU.mult, ALU.mult, [bkey, gkey] + sd1k, okeys)

        def block(b, full):
            t0 = b * T
            tiles = [b * TB + j for j in range(TB)]
            S.phase = "P1.norm"
            for j, g in enumerate(tiles):
                S.dma("sp", xres[:, j, :], xbuf[g * 128:(g + 1) * 128, :], writes=[("xres", j)])
            for j in range(TB):
                norm_transpose(j, C_MIXG)
            S.phase = "P1.lru"
            cp(lrux[:, :, 0:3], lhalo[:], ["lhalo"], LXK, e="pool")

            def ev_lrux(base):
                def f(ci, bank, bkey):
                    cp(lrux[:, base + ci, 3:3 + T], bank[:, 0:T], [bkey], LXK)
                return f
            proj_fm("lrux0", 512, ev_lrux(0), rot4=False)
            proj_fm("lrux1", 512, ev_lrux(4), rot4=False)
            S.dma("sp", waxs[:].rearrange("p n d -> p (n d)"), units["wax"][0], reads=[("u", "wax")], writes=["waxs"])
            wax, waxk = waxs, "waxs"
            side = []

            def ev_kk(ci, bank, bkey):
                if ci < 2:
                    headnorm(bank, bkey, cv(C_GK), "cvec", kT[:, ci, t0:t0 + T], [("kT", g) for g in tiles])
                else:
                    cp(kiT[:, t0:t0 + T], bank[:, 0:T], [bkey], [("ki", g) for g in tiles])
            side += proj_tasks("kk", 384, ev_kk)
            vst = {}

            def vw_task(j, g):
                def f():
                    if j == 0:
                        vst["w"] = wget("vw")
                    vw, vwk = vst["w"]
                    bank, bkey = nextbank()
                    for kc in range(8):
                        mm(bank[:, 0:264], xnT[:, kc, j * 128:(j + 1) * 128], vw[:, kc, :], kc == 0, kc == 7, [vwk, ("xnT", j)], [bkey])
                    cp(Vt[:, g, :, 0:128], bank[:, 0:256].rearrange("p (a d) -> p a d", a=2), [bkey], [("V", g)])
                    if full:
                        ts(weff[:, j, :], bank[:, 256:264], float(8 ** -0.5 * 64 ** -0.5), None, ALU.mult, None, [bkey], [("weff", j)])
                return f
            side += [vw_task(j, g) for j, g in enumerate(tiles)]
            if full:
                def ev_q(base):
                    def f(ci, bank, bkey):
                        headnorm(bank, bkey, gqs[:, 0:1], "gqs", qT[:, base + ci, :], [("qT", base + ci)])
                    return f
                side += proj_tasks("q0", 512, ev_q(0))
                side += proj_tasks("q1", 512, ev_q(4))

                def ev_qi(ci, bank, bkey):
                    cp(qiT[:, ci, :], bank[:, 0:T], [bkey], ["qiT"])
                side += proj_tasks("qi", 512, ev_qi)
            per = (len(side) + 3) // 4
            if b == FIRST_OWN:
                ts(hstate[:], hstate[:], cv(C_CF), None, ALU.mult, None, ["hstate", "cvec"], ["hstate"])
            def lru_bufs(c):
                pb = c % 2
                base = pb * 7 * T
                names = ["ua", "r", "ig", "a", "a2", "m", "u"]
                d = {n: arena(base + i * T, T) for i, n in enumerate(names)}
                d["h"] = arena(HOFF + c * T, T)
                return pb, d

            for c0 in range(0, 8, 2):
                pair = (c0, c0 + 1)
                for c in pair:
                    pb, d = lru_bufs(c)
                    ua, uak = d["ua"]
                    ts(ua, lrux[:, c, 0:T], cv(C_LCW + c * 4), cv(C_LCB + c), ALU.mult, ALU.add, LXK + ["cvec"], uak)
                    for jj in range(1, 4):
                        stt(ua, lrux[:, c, jj:jj + T], cv(C_LCW + c * 4 + jj), ua, ALU.mult, ALU.add, LXK + ["cvec"] + uak, uak)
                    cp(uab[:, pb, :], ua, uak, [("uab", pb)])
                for c in pair:
                    pb, d = lru_bufs(c)
                    (r_, rk), (ig, igk) = d["r"], d["ig"]
                    lb, lk = (pL0, ("ps", "L0")) if pb == 0 else (pL1, ("ps", "L1"))
                    mm(lb[:, 0:T], wax[:, c, 0:128], uab[:, pb, :], True, True, [waxk, ("uab", pb)], [lk])
                    act(r_, lb[:, 0:T], AF.Tanh, [lk, "hb"], rk, bias=hba[:, c:c + 1], scale=0.5)
                    mm(lb[:, 0:T], wax[:, c, 128:256], uab[:, pb, :], True, True, [waxk, ("uab", pb)], [lk])
                    act(ig, lb[:, 0:T], AF.Tanh, [lk, "hb"], igk, bias=hbx[:, c:c + 1], scale=0.5)
                for c in pair:
                    pb, d = lru_bufs(c)
                    (r_, rk), (a_, ak), (a2, a2k) = d["r"], d["a"], d["a2"]
                    act(a_, r_, AF.Exp, rk + ["s8"], ak, bias=s8h[:, c:c + 1], scale=s8h[:, c:c + 1])
                    act(a2, r_, AF.Exp, rk + ["s16"], a2k, bias=s16h[:, c:c + 1], scale=s16h[:, c:c + 1])
                for c in pair:
                    pb, d = lru_bufs(c)
                    (a2, a2k), (m_, mk) = d["a2"], d["m"]
                    act(m_, a2, AF.Sqrt, a2k + ["cvec"], mk, bias=cv(C_ONE), scale=-1.0)
                for c in pair:
                    pb, d = lru_bufs(c)
                    (ua, uak), (ig, igk), (m_, mk), (u_, uk), (a_, ak), (hc, hck) = d["ua"], d["ig"], d["m"], d["u"], d["a"], d["h"]
                    stt(u_, ig, 1.0, ua, ALU.add, ALU.mult, igk + uak, uk)
                    stt(u_, u_, 0.5, m_, ALU.mult, ALU.mult, uk + mk, uk)
                    S.op("dve", lambda en: en.tensor_tensor_scan(out=hc, data0=a_, data1=u_, initial=hstate[:, c:c + 1],
                                                                 op0=ALU.mult, op1=ALU.add), ak + uk + ["hstate"], hck)
                    cp(hstate[:, c:c + 1], hc[:, T - 1:T], hck, ["hstate"], e="dve")
                S.phase = "P1.side"
                for _ in range(per):
                    if side:
                        side.pop(0)()
                S.phase = "P1.lru"
            while side:
                side.pop(0)()
            cp(lhalo[:], lrux[:, :, T:T + 3], LXK, ["lhalo"], e="pool")

            if not full:
                return

            S.phase = "P1.q"
            def ev_lg(base):
                def f(ci, bank, bkey):
                    c = base + ci
                    hc, hck = arena(HOFF + c * T, T)
                    gt = tmpf[:, ci % 2, 0:T]
                    act(gt, bank[:, 0:T], AF.Gelu_apprx_tanh, [bkey], [("tmpf", ci % 2)])
                    tt(hgT[:, c, :], gt, hc, ALU.mult, [("tmpf", ci % 2)] + hck, [("hgT", c)])
                return f
            proj_fm("lg0", 512, ev_lg(0))
            proj_fm("lg1", 512, ev_lg(4))

            for j, g in enumerate(tiles):
                attention(j, g)

            S.phase = "M.merge"
            ya_t = lambda m: arena(m * T, T)
            yb_t = lambda m: arena(8 * T + m * T, T)

            def ev_ya(base):
                def f(ci, bank, bkey):
                    v, k = ya_t(base + ci)
                    act(v, bank[:, 0:T], AF.Copy, [bkey], k, scale=0.5)
                return f
            HGK = [("hgT", c) for c in range(8)]
            proj_fm("lbr0", 512, ev_ya(0), rhsT=hgT, rkeys=HGK)
            proj_fm("lbr1", 512, ev_ya(4), rhsT=hgT, rkeys=HGK)

            def ev_ga(base):
                def f(ci, bank, bkey):
                    v, k = ya_t(base + ci)
                    sgt = tmpf[:, ci % 2, 0:T]
                    act(sgt, bank[:, 0:T], AF.Tanh, [bkey], [("tmpf", ci % 2)], scale=0.5)
                    stt(v, sgt, 1.0, v, ALU.add, ALU.mult, k + [("tmpf", ci % 2)], k)
                return f
            proj_fm("ga0", 512, ev_ga(0))
            proj_fm("ga1", 512, ev_ga(4))

            def ev_yb(base):
                def f(ci, bank, bkey):
                    v, k = yb_t(base + ci)
                    act(v, bank[:, 0:T], AF.Copy, [bkey], k, scale=0.5)
                return f
            ATK = [("attnT", jj) for jj in range(TB)]
            proj_fm("abr0", 512, ev_yb(0), rhsT=attnT, rkeys=ATK)
            proj_fm("abr1", 512, ev_yb(4), rhsT=attnT, rkeys=ATK)

            def ev_gb(base):
                def f(ci, bank, bkey):
                    m = base + ci
                    va, ka = ya_t(m)
                    vb, kb = yb_t(m)
                    sgt = tmpf[:, ci % 2, 0:T]
                    act(sgt, bank[:, 0:T], AF.Tanh, [bkey], [("tmpf", ci % 2)], scale=0.5)
                    stt(vb, sgt, 1.0, vb, ALU.add, ALU.mult, kb + [("tmpf", ci % 2)], kb)
                    tt(mergedT[:, m, :], vb, va, ALU.add, ka + kb, [("qT", m)])
                return f
            proj_fm("gb0", 512, ev_gb(0))
            proj_fm("gb1", 512, ev_gb(4))

            def tm_accum(wname, lhs, lkeys):
                wv, wkey = wget(wname)
                n = int(wname[-1] in "1b")
                for j in range(TB):
                    bank, bkey = nextbank(True)
                    for kc in range(8):
                        mm(bank[:, 0:512], lhs[:, kc, j * 128:(j + 1) * 128], wv[:, kc, :], kc == 0, kc == 7, [wkey] + lkeys, [bkey])
                    tt(xres[:, j, n * 512:(n + 1) * 512], xres[:, j, n * 512:(n + 1) * 512], bank[:, 0:512], ALU.add,
                       [bkey, ("xres", j)], [("xres", j)])
            MK = [("qT", m) for m in range(8)]
            tm_accum("wo0", mergedT, MK)
            tm_accum("wo1", mergedT, MK)

            S.phase = "F.ffn"
            for j in range(TB):
                norm_transpose(j, C_FFNG)
            for fg in range(3):
                def ev_gate(base):
                    def f(ci, bank, bkey):
                        c = base + ci
                        fc = fg * 8 + c
                        pb = c % 2
                        if b == FIRST_OWN:
                            ts(gbuf[:, pb, 0:2], fhalo[:, fc, :], cv(C_CF), None, ALU.mult, None, ["fhalo", "cvec"], [("gbuf", pb)], e="pool")
                        else:
                            cp(gbuf[:, pb, 0:2], fhalo[:, fc, :], ["fhalo"], [("gbuf", pb)], e="pool")
                        cp(gbuf[:, pb, 2:2 + T], bank[:, 0:T], [bkey], [("gbuf", pb)])
                        cp(fhalo[:, fc, :], gbuf[:, pb, T:T + 2], [("gbuf", pb)], ["fhalo"], e="pool")
                        gc = gcb[:, pb, :]
                        ts(gc, gbuf[:, pb, 0:T], cv(C_FCW + fc * 3), cv(C_FCB + fc), ALU.mult, ALU.add, [("gbuf", pb), "cvec"], [("gcb", pb)])
                        for jj in (1, 2):
                            stt(gc, gbuf[:, pb, jj:jj + T], cv(C_FCW + fc * 3 + jj), gc, ALU.mult, ALU.add,
                                [("gbuf", pb), "cvec", ("gcb", pb)], [("gcb", pb)])
                        glv, glk = arena(c * T, T)
                        act(glv, gc, AF.Gelu_apprx_tanh, [("gcb", pb)], glk)
                    return f
                proj_fm("upg%da" % fg, 512, ev_gate(0))
                proj_fm("upg%db" % fg, 512, ev_gate(4))

                def ev_val(base):
                    def f(ci, bank, bkey):
                        c = base + ci
                        glv, glk = arena(c * T, T)
                        tt(actT[:, c, :], glv, bank[:, 0:T], ALU.mult, glk + [bkey], [("hgT", c)])
                    return f
                proj_fm("upv%da" % fg, 512, ev_val(0))
                proj_fm("upv%db" % fg, 512, ev_val(4))
                AK = [("hgT", c) for c in range(8)]
                tm_accum("dn%da" % fg, actT, AK)
                tm_accum("dn%db" % fg, actT, AK)

            S.phase = "E.ple"
            for j in range(TB):
                norm_transpose(j, C_PLEG)
            for j, g in enumerate(tiles):
                S.dma("pool", p_tm[:], pbuf[g * 128:(g + 1) * 128, :], writes=["p_tm"])
                for k2 in range(2):
                    tr(pT_ps[:, k2 * 128:(k2 + 1) * 128], p_tm[:, k2 * 128:(k2 + 1) * 128], ["p_tm"], [("ps", "T")])
                cp(pT[:, :, j * 128:(j + 1) * 128], pT_ps[:, 0:256].rearrange("p (k t) -> p k t", k=2), [("ps", "T")], [("pT", j)])
            pp, ppk = wget("pp")
            W["hold"] = W["consumed"] - 1
            for n in range(2):
                pg, pgk = wget("pg%d" % n)
                for j, g in enumerate(tiles):
                    bank, bkey = nextbank()
                    for kc in range(8):
                        mm(bank[:, 0:512], xnT[:, kc, j * 128:(j + 1) * 128], pg[:, kc, :], kc == 0, kc == 7, [pgk, ("xnT", j)], [bkey])
                    sg = tmpf[:, 0, :]
                    act(sg, bank[:, 0:512], AF.Tanh, [bkey], [("tmpf", 0)], scale=0.5)
                    for k2 in range(2):
                        mm(pL0[:, 0:512], pT[:, k2, j * 128:(j + 1) * 128], pp[:, k2, n * 512:(n + 1) * 512], k2 == 0, k2 == 1,
                           [ppk, ("pT", j)], [("ps", "L0")])
                    stt(sg, sg, 1.0, pL0[:, 0:512], ALU.add, ALU.mult, [("tmpf", 0), ("ps", "L0")], [("tmpf", 0)])
                    stt(xres[:, j, n * 512:(n + 1) * 512], sg, 0.5, xres[:, j, n * 512:(n + 1) * 512], ALU.mult, ALU.add,
                        [("tmpf", 0), ("xres", j)], [("xres", j)])
            W["hold"] = None
            if b >= FIRST_OWN:
                for j, g in enumerate(tiles):
                    S.dma("pool", out[(g - NCT) * 128:(g - NCT + 1) * 128, :], xres[:, j, :], reads=[("xres", j)], writes=[("out", g)])

        HOFF = 14 * T
        assert HOFF + 8 * T <= ARN

        OB = [(pOA, "OA", 0), (pOA, "OA", 1), (pOA, "OA", 2), (pOB, "OB", 0), (pOB, "OB", 1), (pOB, "OB", 2), (pOC, "OC", 0), (pOC, "OC", 1)]

        def attention(j, g):
            L = (g + 1) * 128
            nch = (L + 511) // 512
            js = slice(j * 128, (j + 1) * 128)
            S.phase = "A.idx"
            for ch in range(nch):
                n = min(512, L - ch * 512)
                sc = score[:, ch * 512:ch * 512 + n]
                sk = [("sc", ch)]
                kik = [("ki", kt) for kt in range(ch * 4, ch * 4 + n // 128)]
                for h in range(8):
                    bank, bkey = nextbank_idx()
                    p0 = 64 * (h % 2)
                    mm(bank[:, 0:n], qiT[p0:p0 + 64, h // 2, js], kiT[p0:p0 + 64, ch * 512:ch * 512 + n], True, True,
                       ["qiT"] + kik, [bkey])
                    act(rl[:, h % 2, 0:n], bank[:, 0:n], AF.Relu, [bkey], [("tmpf", h % 2)])
                    if h == 0:
                        ts(sc, rl[:, 0, 0:n], weff[:, j, 0:1], None, ALU.mult, None, [("tmpf", 0), ("weff", j)], sk)
                    else:
                        stt(sc, rl[:, h % 2, 0:n], weff[:, j, h:h + 1], sc, ALU.mult, ALU.add, [("tmpf", h % 2), ("weff", j)] + sk, sk)
            allk = sck(0, L)
            S.phase = "A.bis"
            S.op("dve", lambda en: en.tensor_reduce(out=sm[:, 20:21], in_=score[:, 0:L], axis=AX.X, op=ALU.max), allk, ["rmax"])
            S.op("dve", lambda en: en.tensor_reduce(out=sm[:, 21:22], in_=score[:, 0:L], axis=AX.X, op=ALU.min), allk, ["rmin"])
            tt(sm[:, 22:23], sm[:, 21:22], sm[:, 20:21], ALU.subtract, ["rmin", "rmax"], ["dd"])
            lo = sm[:, 23:24]
            stt(lo, sm[:, 22:23], 1.0 / 64, sm[:, 21:22], ALU.mult, ALU.add, ["dd", "rmin"], ["lo"])
            tt(sm[:, 24:25], sm[:, 20:21], lo, ALU.subtract, ["rmax", "lo"], ["w0"])
            ts(hw[:], cv(C_POW, NIT), sm[:, 24:25], None, ALU.mult, None, ["cvec", "w0"], ["hw"])
            dg = score[:, g * 128:(g + 1) * 128]
            tt(dg, dg, tri[:], ALU.add, [("sc", g // 4), "tri"], [("sc", g // 4)])
            cend = min(L, CTXK)
            nA = min(cend, 2560 if L < 5248 else (3072 if L < 6656 else 4096))
            act_pieces = []
            if nA > 0:
                n0 = min(nA, 2048)
                act_pieces.append((0, n0, junk2[:, 2048:2048 + n0], "junk2b"))
                if nA > 2048:
                    n1 = min(nA, 3072) - 2048
                    act_pieces.append((2048, n1, nm[:].rearrange("p a n -> p (a n)")[:, 0:n1], ("nm", 0)))
                if nA > 3072:
                    act_pieces.append((3072, nA - 3072, pTb[:].rearrange("p a n -> p (a n)")[:, 0:nA - 3072], ("pTb", 0)))
            assert len(act_pieces) <= 3 and sum(p[1] for p in act_pieces) == nA
            dve_pieces = []
            if cend > nA:
                assert cend - nA <= 2048
                dve_pieces.append((nA, cend - nA, 0))
            col = 1
            for a in range(CTXK, L, 2048):
                dve_pieces.append((a, min(2048, L - a), col))
                col += 1
            assert col <= 3
            mid, tot, stp, kadj = sm[:, 25:26], sm[:, 27:28], sm[:, 28:29], sm[:, 30:31]
            prod = sm[:, 40:46]
            CK = [("cnt", k) for k in range(6)]
            S.op("dve", lambda en: en.memset(cntb[:, 0:6], 0.0), [], CK)
            ts(kadj, cv(C_CF), -0.5 * nA, float(TOPK), ALU.mult, ALU.add, ["cvec"], ["kadj"])
            ts(mid, lo, hw[:, 0:1], None, ALU.add, None, ["lo", "hw"], ["mid"])
            for it in range(NIT):
                for k, (a, n, dst, dkey) in enumerate(act_pieces):
                    act(dst, score[:, a:a + n], AF.Sign, sck(a, a + n) + ["mid"], [dkey, ("cnt", 3 + k)] + ([("nm", 1)] if k == 1 else []) + ([("pTb", 1)] if k == 2 else []),
                        bias=mid, scale=-1.0, accum=cntb[:, 3 + k:4 + k])
                for (a, n, c) in dve_pieces:
                    ts(junk2[:, 0:n], score[:, a:a + n], mid, None, ALU.is_gt, ALU.add,
                       sck(a, a + n) + ["mid"], ["junk2", ("cnt", c)], accum=cntb[:, c:c + 1])
                tt(prod, cntb[:, 0:6], wv[:, 0:6], ALU.mult, CK + ["wv"], ["prod"])
                S.op("dve", lambda en: en.tensor_reduce(out=tot, in_=prod, axis=AX.X, op=ALU.add), ["prod"], ["tot"])
                stt(stp, tot, kadj, hw[:, it:it + 1], ALU.is_ge, ALU.mult, ["tot", "hw", "kadj"], ["stp"])
                nx = min(it + 1, NIT - 1)
                dst, dk = (mid, "mid") if it < NIT - 1 else (lo, "lo")
                stt(dst, mid, hw[:, nx:nx + 1], stp, ALU.subtract, ALU.add, ["mid", "hw", "stp"], [dk])
            loc = sm[:, 29:30]
            ts(loc, lo, cv(C_CF), cv(C_CF + 1), ALU.mult, ALU.add, ["lo", "cvec"], ["loc"])
            S.phase = "A.att"
            steps = [(kt, kv) for kt in range(g + 1) for kv in range(2)]

            def qk(i):
                kt, kv = steps[i]
                ch, off = kt // 4, (kt % 4) * 128
                if kt % 4 == 0 and kv == 0:
                    n = min(512, L - ch * 512)
                    isctx = ch * 512 < CTXK
                    ts(nm[:, ch % 2, 0:n], score[:, ch * 512:ch * 512 + n], (loc if isctx else lo), -30000.0, ALU.is_le, ALU.mult,
                       [("sc", ch), "lo", "loc"], [("nm", ch % 2)])
                near = kt >= g - 1
                lgb, lgk = (pL0, ("ps", "L0")) if kv == 0 else (pL1, ("ps", "L1"))
                lg3 = lgb[:, 0:512].rearrange("p (a t) -> p a t", a=4)
                mm(lg3, kT[:, kv, kt * 128:(kt + 1) * 128], qT[:, 4 * kv:4 * kv + 4, js], True, False,
                   [("kT", kt)] + [("qT", 4 * kv + a) for a in range(4)], [lgk])
                mm(lg3, nm[:, ch % 2, off:off + 128], ident4[:], False, not near, [("nm", ch % 2), "ident4"], [lgk])
                if near:
                    mm(lg3, ident[:], biasn[:, g - kt, 4 * kv:4 * kv + 4, :], False, True, ["ident", "biasn"], [lgk])

            def pv(i):
                kt, kv = steps[i]
                lgb, lgk = (pL0, ("ps", "L0")) if kv == 0 else (pL1, ("ps", "L1"))
                pk = ("pTb", i % 2)
                pvv = pTb[:, i % 2, :]
                act(pvv, lgb[:, 0:512], AF.Exp, [lgk], [pk])
                for a in range(4):
                    hd = 4 * kv + a
                    ob, obk, sl = OB[hd]
                    mm(ob[:, sl * 129:(sl + 1) * 129], pvv[:, a * 128:(a + 1) * 128], Vt[:, kt, kv, :], (kt == 0 and sl == 0), kt == g,
                       [pk, ("V", kt), "Vones"], [("ps", obk)], skip=True)

            qk(0)
            qk(1)
            for i in range(len(steps)):
                pv(i)
                if i + 2 < len(steps):
                    qk(i + 2)
            S.phase = "A.fin"
            for (ob, obk, nh, h0) in ((pOA, "OA", 3, 0), (pOB, "OB", 3, 3), (pOC, "OC", 2, 6)):
                o3 = ob[:, 0:nh * 129].rearrange("p (h d) -> p h d", d=129)
                ts(den[:, h0:h0 + nh], o3[:, :, 128], 1e-30, None, ALU.max, None, [("ps", obk)], ["den"])
            S.op("dve", lambda en: en.reciprocal(out=rden[:], in_=den[:]), ["den"], ["rden"])
            for (ob, obk, nh, h0) in ((pOA, "OA", 3, 0), (pOB, "OB", 3, 3), (pOC, "OC", 2, 6)):
                o3 = ob[:, 0:nh * 129].rearrange("p (h d) -> p h d", d=129)
                tt(attn_tm[:, h0 * 128:(h0 + nh) * 128].rearrange("p (h d) -> p h d", d=128), o3[:, :, 0:128],
                   rden[:, h0:h0 + nh].unsqueeze(2).to_broadcast([128, nh, 128]), ALU.mult, [("ps", obk), "rden"], ["xn_tm"])
            for hd in range(8):
                tr(pT_ps[:, hd * 128:(hd + 1) * 128], attn_tm[:, hd * 128:(hd + 1) * 128], ["xn_tm"], [("ps", "T")])
            cp(attnT[:, :, js], pT_ps[:, 0:1024].rearrange("p (h t) -> p h t", h=8), [("ps", "T")], [("attnT", j)])

        for b in range(NBLK):
            block(b, b >= FIRST_FULL)
        S.final_wait("sp", [("out", g) for g in range(NCT, NT)])
        S.final_wait("pool", [("out", g) for g in range(NCT, NT)])
    print("ops", S.nops, {e: S.cnt[e] for e in S.cnt})
    return nc


def _bucket(dist):
    d_f = np.maximum(dist, 1).astype(np.float32)
    large = 16 + (np.log(d_f / np.float32(16)) / np.float32(math.log(128 / 16)) * np.float32(16)).astype(np.int32)
    large = np.minimum(large, 31)
    return np.where(dist < 16, dist, large)


def make_inputs(inp, NT, NIT=20):
    f32 = np.float32
    x = np.asarray(inp["x"], f32)
    p = np.asarray(inp["p"], f32)[0]
    Bn, SEQ, _ = x.shape
    half = SEQ // 2
    assert NT * 128 == SEQ

    def col(v, nch):
        return np.ascontiguousarray(np.asarray(v, f32).reshape(nch, 128).T)

    cvec = np.zeros((128, C_POW + NIT), f32)
    cvec[:, C_MIXG:C_MIXG + 8] = col(inp["mix_norm"][0], 8)
    cvec[:, C_FFNG:C_FFNG + 8] = col(inp["ffn_norm"][0], 8)
    cvec[:, C_PLEG:C_PLEG + 8] = col(inp["ple_norm"][0], 8)
    lcw = np.asarray(inp["lru_conv_w"], f32)[0]
    cvec[:, C_LCW:C_LCW + 32] = lcw.reshape(4, 8, 128).transpose(2, 1, 0).reshape(128, 32)
    cvec[:, C_LCB:C_LCB + 8] = col(inp["lru_conv_b"][0], 8)
    cvec[:, C_LBA:C_LBA + 8] = col(inp["lru_ba"][0], 8)
    cvec[:, C_LBX:C_LBX + 8] = col(inp["lru_bx"][0], 8)
    cvec[:, C_LAM:C_LAM + 8] = col(inp["lru_lambda"][0], 8)
    cvec[:, C_GQ] = np.asarray(inp["q_norm"], f32)[0]
    cvec[:, C_GK] = np.asarray(inp["k_norm"], f32)[0]
    fcw = np.asarray(inp["ffn_conv_w"], f32)[0]
    cvec[:, C_FCW:C_FCW + 72] = fcw.reshape(3, 24, 128).transpose(2, 1, 0).reshape(128, 72)
    cvec[:, C_FCB:C_FCB + 24] = col(inp["ffn_conv_b"][0], 24)
    cvec[:, C_EPS] = EPS
    cvec[:, C_ONE] = 1.0
    cvec[:, C_POW:C_POW + NIT] = (0.5 ** np.arange(1, NIT + 1, dtype=np.float64)).astype(f32)[None, :]

    tq = np.arange(128)[:, None]
    sk = np.arange(128)[None, :]
    tri = np.where(sk <= tq, 0.0, -1e30).astype(f32)
    ident = np.eye(128, dtype=f32)
    rb = np.asarray(inp["rel_bias"], f32)
    ss = np.arange(128)[:, None, None]
    oo = np.arange(2)[None, :, None]
    tt_ = np.arange(128)[None, None, :]
    dist = np.maximum(tt_ - ss + 128 * oo, 0).astype(np.int32)
    bidx = _bucket(dist)
    bn = rb[bidx]
    bn = np.ascontiguousarray(bn.transpose(0, 1, 3, 2)).reshape(128, 2 * 8 * 128)
    b31 = np.ascontiguousarray(rb[np.full_like(bidx, 31)].transpose(0, 1, 3, 2)).reshape(128, 2 * 8 * 128)

    shared = {
        "w_in": np.ascontiguousarray(inp["w_in"][0], f32), "w_lru_br": np.ascontiguousarray(inp["w_lru_br"][0], f32),
        "w_attn_br": np.ascontiguousarray(inp["w_attn_br"][0], f32), "w_out": np.ascontiguousarray(inp["w_out"][0], f32),
        "w_up": np.ascontiguousarray(inp["w_up"][0], f32), "w_down": np.ascontiguousarray(inp["w_down"][0], f32),
        "w_ple_gate": np.ascontiguousarray(inp["w_ple_gate"][0], f32), "w_ple_proj": np.ascontiguousarray(inp["w_ple_proj"][0], f32),
        "lru_wa": np.ascontiguousarray(inp["lru_wa"][0], f32), "lru_wx": np.ascontiguousarray(inp["lru_wx"][0], f32),
        "tri": tri, "ident": ident, "bias_near": bn, "bias_far": b31,
    }
    maps = []
    for c in range(2 * Bn):
        b, h = c // 2, c % 2
        cv_c = cvec.copy()
        if h == 1:
            xb, pb = x[b], p[b]
            cv_c[:, C_CF], cv_c[:, C_CF + 1] = 1.0, 0.0
        else:
            xb = np.concatenate([np.zeros((half, D), f32), x[b, :half]], axis=0)
            pb = np.concatenate([np.zeros((half, 256), f32), p[b, :half]], axis=0)
            cv_c[:, C_CF], cv_c[:, C_CF + 1] = 0.0, 1e30
        m = dict(shared)
        m["xbuf"] = np.ascontiguousarray(xb)
        m["pbuf"] = np.ascontiguousarray(pb)
        m["cvec"] = cv_c
        maps.append(m)
    return maps


def run(inp, NT, NIT=20):
    x = np.asarray(inp["x"])
    Bn, SEQ, _ = x.shape
    half = SEQ // 2
    nc = build(NT, NIT=NIT)
    maps = make_inputs(inp, NT, NIT=NIT)
    res = run_bass_kernel_spmd(nc, maps, core_ids=list(range(2 * Bn)))
    outp = np.empty((Bn, SEQ, D), np.float32)
    for c in range(2 * Bn):
        b, h = c // 2, c % 2
        outp[b, h * half:(h + 1) * half] = res.results[c]["out"]
    return outp


def kernel(**inputs):
    return run(inputs, 64, NIT=20)
```
